# Optimizing a Trainium2 kernel written in Bass

```python
import math
import jax, jax.numpy as jnp
from jax import lax
import numpy as np


D_MODEL = 1024
BATCH = 2
SEQ = 16384
DEPTH = 2

N_A = max(1, DEPTH // 2)
N_B = DEPTH - N_A
HGRN_HEADS = 8
HGRN_DK = D_MODEL // HGRN_HEADS
HGRN_DV = D_MODEL // HGRN_HEADS
HGRN_FDIM = HGRN_HEADS * HGRN_DK
HGRN_CHUNK = 16
SB_HEADS = 8
SB_HEAD_DIM = 128
SB_DIM = SB_HEADS * SB_HEAD_DIM
SB_QBLOCK = 128
PEER_HEADS = 4
PEER_NKEYS = 128
PEER_N = PEER_NKEYS * PEER_NKEYS
PEER_DK = 256
PEER_TOPK = 16
PEER_TOKEN_BLOCK = 128
NORM_EPS = 1e-6

kernel_name = 'hgrn2_stickbreak_peer_yoco'


def rms_norm(x, g):
    xf = x.astype(jnp.float32)
    y = xf * lax.rsqrt(jnp.mean(xf * xf, axis=-1, keepdims=True) + NORM_EPS)
    return (y * g.astype(jnp.float32)).astype(x.dtype)


def modulate(xn, shift, scale):
    return xn * (1 + scale[:, None, :]) + shift[:, None, :]


def hgrn2_mixer(xn, w_in, lb, onorm_g, w_out):
    b, s, _ = xn.shape
    nc = s // HGRN_CHUNK
    proj = xn @ w_in
    q, f, i, g = jnp.split(proj, [HGRN_FDIM, 2 * HGRN_FDIM, 2 * HGRN_FDIM + D_MODEL], axis=-1)
    lbf = lb.astype(jnp.float32)
    fgate = lbf + (1 - lbf) * jax.nn.sigmoid(f.astype(jnp.float32))
    log_f = jnp.log(fgate)
    k = 1 - fgate
    q = jax.nn.silu(q.astype(jnp.float32))
    v = i.astype(jnp.float32)

    def to_chunks(t, d):
        return t.reshape(b, nc, HGRN_CHUNK, HGRN_HEADS, d).transpose(1, 0, 3, 2, 4)

    causal = jnp.tril(jnp.ones((HGRN_CHUNK, HGRN_CHUNK), dtype=bool))

    def step(state, inp):
        qc, kc, vc, lfc = inp
        cum = jnp.cumsum(lfc, axis=2)
        last = cum[:, :, -1:, :]
        qd = qc * jnp.exp(cum)
        kd = kc * jnp.exp(-cum)
        scores = jnp.where(causal, jnp.einsum('bhtk,bhsk->bhts', qd, kd), 0.0)
        o = (jnp.einsum('bhts,bhsv->bhtv', scores, vc)
             + jnp.einsum('bhtk,bhkv->bhtv', qd, state))
        new_state = (jnp.exp(last[:, :, 0, :])[..., None] * state
                     + jnp.einsum('bhsk,bhsv->bhkv', kc * jnp.exp(last - cum), vc))
        return new_state, o

    state0 = jnp.zeros((b, HGRN_HEADS, HGRN_DK, HGRN_DV), jnp.float32)
    _, o = lax.scan(step, state0, (to_chunks(q, HGRN_DK), to_chunks(k, HGRN_DK),
                                   to_chunks(v, HGRN_DV), to_chunks(log_f, HGRN_DK)))
    o = o.transpose(1, 0, 3, 2, 4).reshape(b, s, HGRN_HEADS, HGRN_DV)
    o = o * lax.rsqrt(jnp.mean(o * o, axis=-1, keepdims=True) + NORM_EPS)
    o = o.reshape(b, s, D_MODEL) * onorm_g.astype(jnp.float32) * jax.nn.silu(g.astype(jnp.float32))
    return o.astype(xn.dtype) @ w_out


def shared_kv(h, c_act, kv_ada_w, kv_ada_b, kv_norm_g, kv_w):
    b, s, _ = h.shape
    shift, scale = jnp.split(c_act @ kv_ada_w + kv_ada_b, 2, axis=-1)
    hn = modulate(rms_norm(h, kv_norm_g), shift, scale)
    kv = (hn @ kv_w).reshape(b, s, 2, SB_HEADS, SB_HEAD_DIM)
    k = kv[:, :, 0].transpose(0, 2, 1, 3)
    v = kv[:, :, 1].transpose(0, 2, 1, 3)
    return k, v


def stick_breaking_mixer(xn, k_sh, v_sh, w_q, w_out):
    b, s, _ = xn.shape
    nq = s // SB_QBLOCK
    q = (xn @ w_q).reshape(b, s, SB_HEADS, SB_HEAD_DIM).transpose(0, 2, 1, 3) * (1.0 / math.sqrt(SB_HEAD_DIM))
    loc = jnp.arange(SB_QBLOCK)
    tri = (loc[:, None] >= loc[None, :]).astype(jnp.float32)
    outs = []
    for i in range(nq):
        nk = i + 1
        qi = q[:, :, i * SB_QBLOCK:(i + 1) * SB_QBLOCK]
        kb = k_sh[:, :, :nk * SB_QBLOCK].reshape(b, SB_HEADS, nk, SB_QBLOCK, SB_HEAD_DIM)
        vb = v_sh[:, :, :nk * SB_QBLOCK].reshape(b, SB_HEADS, nk, SB_QBLOCK, SB_HEAD_DIM)
        z = jnp.einsum('bhqd,bhnsd->bhqns', qi, kb).astype(jnp.float32)
        s_pos = jnp.arange(nk * SB_QBLOCK).reshape(nk, SB_QBLOCK)
        t_pos = i * SB_QBLOCK + loc
        mask = s_pos[None, :, :] < t_pos[:, None, None]
        log_keep = jnp.where(mask, jax.nn.log_sigmoid(-z), 0.0)
        cs_in = jnp.einsum('bhqnj,js->bhqns', log_keep, tri)
        blk = cs_in[..., 0]
        after = lax.cumsum(blk, axis=3, reverse=True) - blk
        a = jnp.where(mask, jnp.exp(z + cs_in + after[..., None]), 0.0)
        outs.append(jnp.einsum('bhqns,bhnsd->bhqd', a.astype(vb.dtype), vb))
    o = jnp.concatenate(outs, axis=2)
    o = o.transpose(0, 2, 1, 3).reshape(b, s, SB_DIM)
    return o @ w_out


def peer_ffn(xn, w_q, subkeys, u, v):
    b, s, d = xn.shape
    t = b * s
    nb = t // PEER_TOKEN_BLOCK
    xt = xn.reshape(t, d)
    qr = (xt @ w_q).reshape(t, PEER_HEADS, 2, PEER_DK // 2)
    sc = jnp.einsum('thpd,hpnd->thpn', qr, subkeys).astype(jnp.float32)
    s1, i1 = lax.top_k(sc[:, :, 0], PEER_TOPK)
    s2, i2 = lax.top_k(sc[:, :, 1], PEER_TOPK)
    cand = (s1[..., :, None] + s2[..., None, :]).reshape(t, PEER_HEADS, PEER_TOPK * PEER_TOPK)
    best, flat = lax.top_k(cand, PEER_TOPK)
    e1 = jnp.take_along_axis(i1, flat // PEER_TOPK, axis=-1)
    e2 = jnp.take_along_axis(i2, flat % PEER_TOPK, axis=-1)
    experts = (e1 * PEER_NKEYS + e2).reshape(nb, PEER_TOKEN_BLOCK, PEER_HEADS * PEER_TOPK)
    gates = jax.nn.softmax(best, axis=-1).reshape(nb, PEER_TOKEN_BLOCK, PEER_HEADS * PEER_TOPK)

    def block(args):
        xc, ec, gc = args
        hid = jnp.einsum('cd,ced->ce', xc, jnp.take(u, ec, axis=0)).astype(jnp.float32)
        w = (gc * jax.nn.gelu(hid, approximate=False)).astype(xc.dtype)
        return jnp.einsum('ce,ced->cd', w, jnp.take(v, ec, axis=0))

    out = lax.map(block, (xt.reshape(nb, PEER_TOKEN_BLOCK, d), experts, gates))
    return out.reshape(b, s, d)


def setup_inputs(seed: int = 0) -> dict:
    key = jax.random.key(seed)
    ks = jax.random.split(key, 22)
    D = D_MODEL

    def nrm(k, shape, std):
        return jax.random.normal(k, shape, jnp.float32) * std

    return {
        'x': nrm(ks[0], (BATCH, SEQ, D), 1.0),
        'c': nrm(ks[1], (BATCH, D), 1.0),
        'ada_w': nrm(ks[2], (DEPTH, D, 6 * D), 0.5 * D ** -0.5),
        'ada_b': nrm(ks[3], (DEPTH, 6 * D), 0.02),
        'norm_mix_g': 1.0 + nrm(ks[4], (DEPTH, D), 0.02),
        'norm_ffn_g': 1.0 + nrm(ks[5], (DEPTH, D), 0.02),
        'hgrn_w_in': nrm(ks[6], (N_A, D, 2 * HGRN_FDIM + 2 * D), D ** -0.5),
        'hgrn_lb_logits': nrm(ks[7], (N_A + 1, HGRN_FDIM), 0.5),
        'hgrn_onorm_g': 1.0 + nrm(ks[8], (N_A, D), 0.02),
        'hgrn_w_out': nrm(ks[9], (N_A, D, D), D ** -0.5),
        'kv_ada_w': nrm(ks[10], (D, 2 * D), 0.5 * D ** -0.5),
        'kv_ada_b': nrm(ks[11], (2 * D,), 0.02),
        'kv_norm_g': 1.0 + nrm(ks[12], (D,), 0.02),
        'kv_w': nrm(ks[13], (D, 2 * SB_DIM), D ** -0.5),
        'sb_w_q': nrm(ks[14], (N_B, D, SB_DIM), D ** -0.5),
        'sb_w_out': nrm(ks[15], (N_B, SB_DIM, D), SB_DIM ** -0.5),
        'peer_w_q': nrm(ks[16], (DEPTH, D, PEER_HEADS * PEER_DK), D ** -0.5),
        'peer_subkeys': nrm(ks[17], (DEPTH, PEER_HEADS, 2, PEER_NKEYS, PEER_DK // 2), (PEER_DK // 2) ** -0.5),
        'peer_u': nrm(ks[18], (DEPTH, PEER_N, D), D ** -0.5),
        'peer_v': nrm(ks[19], (DEPTH, PEER_N, D), PEER_HEADS ** -0.5),
        'final_norm_g': 1.0 + nrm(ks[20], (D,), 0.02),
    }


def reference(x, c, ada_w, ada_b, norm_mix_g, norm_ffn_g, hgrn_w_in, hgrn_lb_logits,
              hgrn_onorm_g, hgrn_w_out, kv_ada_w, kv_ada_b, kv_norm_g, kv_w, sb_w_q,
              sb_w_out, peer_w_q, peer_subkeys, peer_u, peer_v, final_norm_g):
    c_act = jax.nn.silu(c)
    lb_all = jnp.cumsum(jax.nn.softmax(hgrn_lb_logits.astype(jnp.float32), axis=0), axis=0)
    h = x
    k_sh = None
    v_sh = None
    for l in range(DEPTH):
        mod = c_act @ ada_w[l] + ada_b[l]
        sh1, sc1, g1, sh2, sc2, g2 = jnp.split(mod, 6, axis=-1)
        hn = modulate(rms_norm(h, norm_mix_g[l]), sh1, sc1)
        if l < N_A:
            mix = hgrn2_mixer(hn, hgrn_w_in[l], lb_all[l], hgrn_onorm_g[l], hgrn_w_out[l])
        else:
            mix = stick_breaking_mixer(hn, k_sh, v_sh, sb_w_q[l - N_A], sb_w_out[l - N_A])
        h = h + g1[:, None, :] * mix
        hn = modulate(rms_norm(h, norm_ffn_g[l]), sh2, sc2)
        h = h + g2[:, None, :] * peer_ffn(hn, peer_w_q[l], peer_subkeys[l], peer_u[l], peer_v[l])
        if l == N_A - 1:
            k_sh, v_sh = shared_kv(h, c_act, kv_ada_w, kv_ada_b, kv_norm_g, kv_w)
    return rms_norm(h, final_norm_g)
```

```python
from contextlib import ExitStack
import math
import numpy as np
import ml_dtypes
import concourse.bass as bass
import concourse.mybir as mybir
from concourse.bass_utils import run_bass_kernel_spmd

F32 = mybir.dt.float32
BF16 = mybir.dt.bfloat16
ALU = mybir.AluOpType
AF = mybir.ActivationFunctionType
AX = mybir.AxisListType


class Buf:
    __slots__ = ("name", "w", "r")

    def __init__(self, name):
        self.name = name
        self.w = None
        self.r = []


class Sched:
    ENGS = ("pe", "act", "dve", "pool", "sp")
    NDMA = 8
    ROT = 20000

    def __init__(self, nc, es):
        self.nc = nc
        self.streams = {e: [] for e in self.ENGS}
        self.sem = {}
        self.cnt = {}
        for e in self.ENGS:
            self.sem[e] = es.enter_context(nc.semaphore("s_" + e))
            self.cnt[e] = 0
        self.dsem = {}
        self.dcnt = {}
        for e in ("sp", "act", "pool"):
            self.dsem[e] = [es.enter_context(nc.semaphore("d_%s%d" % (e, i))) for i in range(self.NDMA)]
            self.dcnt[e] = 0
        self.waited = {e: {} for e in self.ENGS}
        self.nbuf = 0
        self.es = es
        self.nrot = 0

    def buf(self, name=None):
        self.nbuf += 1
        return Buf(name or "b%d" % self.nbuf)

    def bufs(self, n, name="b"):
        return [self.buf("%s%d" % (name, i)) for i in range(n)]

    def _need(self, eng, reads, writes, same_ok):
        need = {}

        def add(tok):
            if tok is None:
                return
            sem, val, src = tok
            if same_ok and src == eng:
                return
            k = id(sem)
            if k not in need or need[k][1] < val:
                need[k] = (sem, val)

        for b in reads:
            add(b.w)
        for b in writes:
            add(b.w)
            for t in b.r:
                add(t)
        out = []
        wd = self.waited[eng]
        for k, (sem, val) in need.items():
            if wd.get(k, 0) >= val:
                continue
            wd[k] = val
            out.append((sem, val))
        return out

    def op(self, eng, fn, reads=(), writes=()):
        waits = self._need(eng, reads, writes, same_ok=(eng == "pe"))
        self.cnt[eng] += 1
        tok = (self.sem[eng], self.cnt[eng], eng)
        self.streams[eng].append((waits, fn, (self.sem[eng], 1)))
        for b in reads:
            b.r.append(tok)
        for b in writes:
            b.w = tok
            b.r = []
        return tok

    def dma(self, q, out, in_, reads=(), writes=(), **kw):
        j = self.dcnt[q]
        self.dcnt[q] += 1
        sem = self.dsem[q][j % self.NDMA]
        val = 16 * (j // self.NDMA + 1)
        waits = self._need(q, reads, writes, same_ok=False)
        if j >= self.NDMA:
            prev = val - 16
            wd = self.waited[q]
            if wd.get(id(sem), 0) < prev:
                wd[id(sem)] = prev
                waits.append((sem, prev))
        tok = (sem, val, "dma_" + q)
        self.streams[q].append((waits, lambda e: e.dma_start(out=out, in_=in_, **kw), (sem, 16)))
        for b in reads:
            b.r.append(tok)
        for b in writes:
            b.w = tok
            b.r = []
        return tok

    def wait_all(self, eng, bufs):
        waits = self._need(eng, bufs, (), same_ok=False)
        self.streams[eng].append((waits, None, None))

    def barrier(self):
        toks = []
        for e in self.ENGS:
            if self.cnt[e]:
                toks.append((self.sem[e], self.cnt[e]))
        for q in self.dsem:
            j = self.dcnt[q]
            for s in range(min(j, self.NDMA)):
                last = ((j - 1 - s) // self.NDMA) if j - 1 >= s else -1
                uses = (j - s + self.NDMA - 1) // self.NDMA
                toks.append((self.dsem[q][s], 16 * uses))
        for e in self.ENGS:
            waits = []
            wd = self.waited[e]
            for sem, val in toks:
                if wd.get(id(sem), 0) >= val:
                    continue
                wd[id(sem)] = val
                waits.append((sem, val))
            if waits:
                self.streams[e].append((waits, None, None))
        for e in self.ENGS:
            if self.cnt[e] > self.ROT:
                self.sem[e] = self.es.enter_context(self.nc.semaphore("s_%s_%d" % (e, self.nrot)))
                self.nrot += 1
                self.cnt[e] = 0
        for q in self.dsem:
            if 16 * (self.dcnt[q] // self.NDMA + 1) > self.ROT:
                self.dsem[q] = [self.es.enter_context(self.nc.semaphore("d_%s%d_%d" % (q, i, self.nrot))) for i in range(self.NDMA)]
                self.nrot += 1
                self.dcnt[q] = 0

    def emit(self):
        nc = self.nc
        handles = {"pe": "tensor", "act": "scalar", "dve": "vector", "pool": "gpsimd", "sp": "sync"}
        with nc.Block() as block:
            for e in self.ENGS:
                stream = self.streams[e]

                def body(eng, stream=stream):
                    for waits, fn, inc in stream:
                        for sem, val in waits:
                            eng.wait_ge(sem, val)
                        if fn is not None:
                            ins = fn(eng)
                            ins.then_inc(inc[0], inc[1])

                getattr(block, handles[e])(body)


D = 1024
NEXP = 16384
EPS = 1e-6


class Ctx:
    def __init__(self, nc, es):
        self.nc = nc
        self.es = es
        self.S = Sched(nc, es)
        self.n = 0

    def sb(self, es, shape, dt=F32, name=None):
        self.n += 1
        return es.enter_context(self.nc.sbuf_tensor(name or "t%d" % self.n, list(shape), dt))

    def ps(self, es, shape, dt=F32, name=None):
        self.n += 1
        return es.enter_context(self.nc.psum_tensor(name or "p%d" % self.n, list(shape), dt))

    def dram(self, name, shape, dt, kind="Internal"):
        return self.nc.dram_tensor(name, list(shape), dt, kind=kind).ap()


def make_consts(cx, es):
    S = cx.S
    c = {}
    identf = cx.sb(es, [128, 128], F32)
    identb = cx.sb(es, [128, 128], BF16)
    b = S.buf("ident")
    S.op("pool", lambda e: e.memset(identf[:], 1.0), writes=[b])
    S.op("pool", lambda e: e.affine_select(out=identf[:], in_=identf[:], pattern=[[-1, 128]],
                                           compare_op=ALU.is_equal, fill=0.0, base=0, channel_multiplier=1),
         reads=[b], writes=[b])
    S.op("dve", lambda e: e.tensor_copy(out=identb[:], in_=identf[:]), reads=[b], writes=[b])
    c["identf"], c["identb"], c["b_ident"] = identf, identb, b
    return c


def emit_mod(cx, cT_ap, specs, out_dram):
    S = cx.S
    with ExitStack() as es:
        cs = cx.sb(es, [128, 8])
        sg = cx.sb(es, [128, 8])
        CA = cx.sb(es, [128, 8, 128])
        wst = [cx.sb(es, [128, 2048]) for _ in range(2)]
        bwst = S.bufs(2, "wst")
        bias = cx.sb(es, [128, 2048])
        res = cx.sb(es, [128, 2048])
        pm = cx.ps(es, [128, 4, 512])
        b_cs, b_CA, b_bias, b_res, b_pm = S.buf(), S.buf(), S.buf(), S.buf(), S.buf()
        S.dma("sp", cs[:], cT_ap, writes=[b_cs])
        S.op("act", lambda e: e.activation(out=sg[:], in_=cs[:], func=AF.Sigmoid), reads=[b_cs], writes=[b_CA])
        S.op("dve", lambda e: e.tensor_tensor(out=cs[:], in0=cs[:], in1=sg[:], op=ALU.mult), reads=[b_cs, b_CA], writes=[b_cs])
        S.op("dve", lambda e: e.tensor_copy(out=CA[:], in_=cs[:].unsqueeze(2).to_broadcast([128, 8, 128])),
             reads=[b_cs], writes=[b_CA])
        it = 0
        for (W, B, col0, ncols) in specs:
            for cb in range(0, ncols, 2048):
                S.dma("pool", bias[:], B[0:1, cb:cb + 2048].partition_broadcast(128), writes=[b_bias])
                for kc in range(8):
                    w = wst[it % 2]
                    bw = bwst[it % 2]
                    it += 1
                    S.dma("sp", w[:], W[kc * 128:(kc + 1) * 128, cb:cb + 2048], writes=[bw])
                    for nb in range(4):
                        S.op("pe", lambda e, w=w, nb=nb, kc=kc: e.matmul(pm[:, nb, :], lhsT=CA[:, kc, :], rhs=w[:, nb * 512:(nb + 1) * 512],
                                                                        start=(kc == 0), stop=(kc == 7)),
                             reads=[b_CA, bw], writes=[b_pm])
                S.op("dve", lambda e: e.tensor_tensor(out=res[:], in0=pm[:].rearrange("p a b -> p (a b)"), in1=bias[:], op=ALU.add),
                     reads=[b_pm, b_bias], writes=[b_res])
                S.dma("sp", out_dram[:, col0 + cb:col0 + cb + 2048], res[:], reads=[b_res])
        S.barrier()


def emit_rstd(cx, ss_ap, rstd_ap, inv_n, b_ss, b_rstd):
    S = cx.S
    S.op("dve", lambda e: e.tensor_scalar(out=rstd_ap, in0=ss_ap, scalar1=inv_n, scalar2=EPS, op0=ALU.mult, op1=ALU.add),
         reads=[b_ss], writes=[b_rstd])
    S.op("act", lambda e: e.activation(out=rstd_ap, in_=rstd_ap, func=AF.Ln), reads=[b_rstd], writes=[b_rstd])
    S.op("act", lambda e: e.activation(out=rstd_ap, in_=rstd_ap, func=AF.Exp, scale=-0.5), reads=[b_rstd], writes=[b_rstd])


class NormT:
    def __init__(self, cx, es, consts, pt=None, b_pt=None):
        self.cx = cx
        self.c = consts
        S = cx.S
        self.junk = cx.sb(es, [128, 1024], BF16)
        self.st = cx.sb(es, [128, 4])
        self.tmp = cx.sb(es, [128, 1024])
        self.hb = cx.sb(es, [128, 1024], BF16)
        self.pt = pt if pt is not None else cx.ps(es, [128, 8, 128], BF16)
        self.b_junk, self.b_st, self.b_tmp, self.b_hb = S.buf(), S.buf(), S.buf(), S.buf()
        self.b_pt = b_pt if b_pt is not None else S.buf()

    def norm(self, x_ap, b_x, geff, shift, b_mod, out_ap, b_out, out_eng="pool"):
        S = self.cx.S
        st, tmp = self.st, self.tmp
        S.op("act", lambda e: e.activation(out=self.junk[:], in_=x_ap, func=AF.Square, accum_out=st[:, 0:1]),
             reads=[b_x], writes=[self.b_junk, self.b_st])
        emit_rstd(self.cx, st[:, 0:1], st[:, 1:2], 1.0 / D, self.b_st, self.b_st)
        S.op("dve", lambda e: e.scalar_tensor_tensor(out=tmp[:], in0=x_ap, scalar=st[:, 1:2], in1=geff, op0=ALU.mult, op1=ALU.mult),
             reads=[b_x, self.b_st, b_mod], writes=[self.b_tmp])
        S.op(out_eng, lambda e: e.tensor_tensor(out=out_ap, in0=tmp[:], in1=shift, op=ALU.add),
             reads=[self.b_tmp, b_mod], writes=[b_out])

    def transpose(self, in_bf, b_in, outT_ap, b_out):
        S = self.cx.S
        for kc in range(8):
            S.op("pe", lambda e, kc=kc: e.transpose(self.pt[:, kc, :], in_bf[:, kc * 128:(kc + 1) * 128], self.c["identb"][:]),
                 reads=[b_in, self.c["b_ident"]], writes=[self.b_pt])
        S.op("act", lambda e: e.copy(out=outT_ap, in_=self.pt[:]), reads=[self.b_pt], writes=[b_out])

    def normT(self, x_ap, b_x, geff, shift, b_mod, outT_ap, b_out):
        self.norm(x_ap, b_x, geff, shift, b_mod, self.hb[:], self.b_hb)
        self.transpose(self.hb, self.b_hb, outT_ap, b_out)


def load_cast(cx, es_tmp, q, dst_ap_fn, src_ap_fn, nchunks, shape, b_dst, cast_eng="pool"):
    S = cx.S
    stg = [cx.sb(es_tmp, shape) for _ in range(2)]
    bst = S.bufs(2, "stg")
    for i in range(nchunks):
        s, b = stg[i % 2], bst[i % 2]
        S.dma(q, s[:], src_ap_fn(i), writes=[b])
        S.op(cast_eng, lambda e, s=s, i=i: e.tensor_copy(out=dst_ap_fn(i), in_=s[:]), reads=[b], writes=[b_dst])


def emit_geff(cx, mod_dram, col, g_dram):
    S = cx.S
    with ExitStack() as es:
        a = cx.sb(es, [128, 1024])
        g = cx.sb(es, [128, 1024])
        ba, bg = S.buf(), S.buf()
        S.dma("sp", a[:], mod_dram[:, col:col + 1024], writes=[ba])
        S.dma("pool", g[:], g_dram[0:1, :].partition_broadcast(128), writes=[bg])
        S.op("dve", lambda e: e.scalar_tensor_tensor(out=a[:], in0=a[:], scalar=1.0, in1=g[:], op0=ALU.add, op1=ALU.mult),
             reads=[ba, bg], writes=[ba])
        S.dma("sp", mod_dram[:, col:col + 1024], a[:], reads=[ba])
        S.barrier()


def emit_precast(cx, src, dst, rows, cols):
    S = cx.S
    with ExitStack() as es:
        CW = 4096
        st = [cx.sb(es, [128, CW]) for _ in range(2)]
        ob = [cx.sb(es, [128, CW], BF16) for _ in range(2)]
        bs, bo = S.bufs(2), S.bufs(2)
        srcv = src.rearrange("(a p) c -> p a c", p=128)
        dstv = dst.rearrange("(a p) c -> p a c", p=128)
        i = 0
        per = max(1, CW // cols)
        cw = min(CW, cols)
        for a0 in range(0, rows // 128, per):
            for c0 in range(0, cols, cw):
                s, o = st[i % 2], ob[i % 2]
                sv = s[:].rearrange("p (a c) -> p a c", a=per)
                ov = o[:].rearrange("p (a c) -> p a c", a=per)
                S.dma("sp" if i % 2 == 0 else "act", sv, srcv[:, a0:a0 + per, c0:c0 + cw], writes=[bs[i % 2]])
                eng = ("pool", "dve")[i % 2]
                S.op(eng, lambda e, s=s, o=o: e.tensor_copy(out=o[:], in_=s[:]), reads=[bs[i % 2]], writes=[bo[i % 2]])
                S.dma("pool", dstv[:, a0:a0 + per, c0:c0 + cw], ov, reads=[bo[i % 2]])
                i += 1
        S.barrier()


def emit_peer(cx, consts, HM, bHM, mod_dram, c_sh, c_ge, c_g, wq_dram, skT_dram, uTb, vb):
    S = cx.S
    GA = 4
    NG = 128 // GA
    with ExitStack() as es:
        SH = cx.sb(es, [128, 1024]); GE = cx.sb(es, [128, 1024]); G2 = cx.sb(es, [128, 1024])
        b_mod = S.buf("mod")
        S.dma("sp", SH[:], mod_dram[:, c_sh:c_sh + 1024], writes=[b_mod])
        S.dma("sp", GE[:], mod_dram[:, c_ge:c_ge + 1024], writes=[b_mod])
        S.dma("sp", G2[:], mod_dram[:, c_g:c_g + 1024], writes=[b_mod])
        wq = cx.sb(es, [128, 8, 1024], BF16); b_wq = S.buf("wq")
        with ExitStack() as es2:
            load_cast(cx, es2, "act", lambda i: wq[:, i, :], lambda i: wq_dram[i * 128:(i + 1) * 128, :], 8, [128, 1024], b_wq)
            S.barrier()
        skT = cx.sb(es, [128, 8, 128]); b_sk = S.buf("sk")
        S.dma("sp", skT[:], skT_dram.rearrange("h d n -> d h n"), writes=[b_sk])
        PW = [cx.ps(es, [128, 8, 128], BF16) for _ in range(2)]; bPW = S.bufs(2, "PW")
        nt = NormT(cx, es, consts, pt=PW[0], b_pt=bPW[0])
        xnT = cx.sb(es, [128, 8, 512], BF16); b_xnT = S.bufs(4, "xnT")
        QA = cx.sb(es, [128, 4, 1024]); bQA = S.bufs(4, "QA")
        PSA = cx.ps(es, [128, 4, 512]); bPSA = S.bufs(4, "PSA")
        PH = cx.ps(es, [128, 2, 512]); bPH = S.bufs(2, "PH")
        Pexp = cx.sb(es, [128, 4, 8, 128]); bP = S.bufs(4, "Pexp")
        top = cx.sb(es, [128, 8, 16]); work = cx.sb(es, [128, 128]); cand = cx.sb(es, [128, 256]); work2 = cx.sb(es, [128, 256])
        c16 = cx.sb(es, [128, 16]); mx = cx.sb(es, [128, 8]); zs = cx.sb(es, [128, 4])
        TH = cx.sb(es, [128, 4, 4]); RZ = cx.sb(es, [128, 4, 4]); P1n = cx.sb(es, [128, 4, 4, 128])
        b_sm = S.buf("small"); bTH = S.bufs(4, "TH")
        UG = [cx.sb(es, [128, 8, GA * 128], BF16) for _ in range(2)]; bUG = S.bufs(2, "UG")
        VG = [cx.sb(es, [128, GA, 1024], BF16) for _ in range(2)]; bVG = S.bufs(2, "VG")
        X = [cx.sb(es, [128, GA, 128]) for _ in range(2)]; bX = S.bufs(2, "X")
        Mk = [cx.sb(es, [128, GA, 128]) for _ in range(2)]; bMk = S.bufs(2, "Mk")
        Wacc = cx.sb(es, [128, GA, 128]); bWacc = S.buf("Wacc")
        Wtm = [cx.sb(es, [128, GA, 128], BF16) for _ in range(4)]; bWtm = S.bufs(4, "Wtm")
        G = [cx.sb(es, [128, 512], BF16) for _ in range(2)]; bG = S.bufs(2, "G")
        WgT = [cx.sb(es, [128, GA, 512], BF16) for _ in range(2)]; bWg = S.bufs(2, "WgT")

        for tt in range(4):
            nt.normT(HM[:, tt, :], bHM[tt], GE[:], SH[:], b_mod, xnT[:, :, tt * 128:(tt + 1) * 128], b_xnT[tt])
        for hp in range(8):
            bank = hp % 4
            for kc in range(8):
                S.op("pe", lambda e, hp=hp, kc=kc, bank=bank: e.matmul(PSA[:, bank, :], lhsT=wq[:, kc, hp * 128:(hp + 1) * 128], rhs=xnT[:, kc, :],
                                                                       start=(kc == 0), stop=(kc == 7)),
                     reads=[b_wq] + b_xnT, writes=[bPSA[bank]])
            S.op("act" if hp % 2 else "dve", (lambda e, hp=hp, bank=bank: e.copy(out=QA[:, hp // 2, (hp % 2) * 512:(hp % 2 + 1) * 512], in_=PSA[:, bank, :])) if hp % 2 else
                 (lambda e, hp=hp, bank=bank: e.tensor_copy(out=QA[:, hp // 2, (hp % 2) * 512:(hp % 2 + 1) * 512], in_=PSA[:, bank, :])),
                 reads=[bPSA[bank]], writes=[bQA[hp // 2]])
        for tt in range(4):
            pb = 2 * (tt % 2)
            for hp in range(8):
                S.op("pe", lambda e, hp=hp, tt=tt, pb=pb: e.matmul(PSA[:, pb + hp // 4, (hp % 4) * 128:(hp % 4 + 1) * 128],
                                                                  lhsT=QA[:, hp // 2, (hp % 2) * 512 + tt * 128:(hp % 2) * 512 + (tt + 1) * 128],
                                                                  rhs=skT[:, hp, :], start=True, stop=True),
                     reads=[bQA[hp // 2], b_sk], writes=[bPSA[pb + hp // 4]])
            scv = PSA[:, pb:pb + 2, :].rearrange("p a (h n) -> p (a h) n", n=128)
            S.op("dve", lambda e, scv=scv: e.reduce_max(out=mx[:], in_=scv, axis=AX.X), reads=[bPSA[pb], bPSA[pb + 1]], writes=[b_sm])
            S.op("dve", lambda e: e.tensor_scalar(out=mx[:], in0=mx[:], scalar1=-1.0, scalar2=None, op0=ALU.mult), reads=[b_sm], writes=[b_sm])
            for hp in range(8):
                S.op("act", lambda e, hp=hp, tt=tt, pb=pb: e.activation(out=Pexp[:, tt, hp, :], in_=PSA[:, pb + hp // 4, (hp % 4) * 128:(hp % 4 + 1) * 128],
                                                                       func=AF.Exp, bias=mx[:, hp:hp + 1], scale=1.0),
                     reads=[bPSA[pb + hp // 4], b_sm], writes=[bP[tt]])
            for hp in range(8):
                S.op("dve", lambda e, hp=hp, tt=tt: e.max(out=top[:, hp, 0:8], in_=Pexp[:, tt, hp, :]), reads=[bP[tt]], writes=[b_sm])
                S.op("dve", lambda e, hp=hp, tt=tt: e.match_replace(out=work[:], in_to_replace=top[:, hp, 0:8], in_values=Pexp[:, tt, hp, :], imm_value=-1.0),
                     reads=[bP[tt], b_sm], writes=[b_sm])
                S.op("dve", lambda e, hp=hp: e.max(out=top[:, hp, 8:16], in_=work[:]), reads=[b_sm], writes=[b_sm])
            for h in range(4):
                S.op("dve", lambda e, h=h: e.tensor_tensor(out=cand[:].rearrange("p (a b) -> p a b", a=16),
                                                           in0=top[:, 2 * h, :].unsqueeze(2).to_broadcast([128, 16, 16]),
                                                           in1=top[:, 2 * h + 1, :].unsqueeze(1).to_broadcast([128, 16, 16]), op=ALU.mult),
                     reads=[b_sm], writes=[b_sm])
                S.op("dve", lambda e: e.max(out=c16[:, 0:8], in_=cand[:]), reads=[b_sm], writes=[b_sm])
                S.op("dve", lambda e: e.match_replace(out=work2[:], in_to_replace=c16[:, 0:8], in_values=cand[:], imm_value=-1.0), reads=[b_sm], writes=[b_sm])
                S.op("dve", lambda e: e.max(out=c16[:, 8:16], in_=work2[:]), reads=[b_sm], writes=[b_sm])
                S.op("dve", lambda e, h=h, tt=tt: e.tensor_copy(out=TH[:, tt, h:h + 1], in_=c16[:, 15:16]), reads=[b_sm], writes=[bTH[tt]])
                S.op("dve", lambda e, h=h: e.reduce_sum(out=zs[:, h:h + 1], in_=c16[:], axis=AX.X), reads=[b_sm], writes=[b_sm])
            S.op("dve", lambda e, tt=tt: e.reciprocal(out=RZ[:, tt, :], in_=zs[:]), reads=[b_sm], writes=[bTH[tt]])
            for h in range(4):
                S.op("dve", lambda e, tt=tt, h=h: e.tensor_scalar(out=P1n[:, tt, h, :], in0=Pexp[:, tt, 2 * h, :], scalar1=RZ[:, tt, h:h + 1], scalar2=None, op0=ALU.mult),
                     reads=[bP[tt], bTH[tt]], writes=[bTH[tt]])
        uv = uTb.rearrange("(kc p) n -> p kc n", p=128)
        vv = vb.rearrange("(a p) d -> p a d", p=128)
        for gi in range(NG):
            ug, vg, bu, bv = UG[gi % 2], VG[gi % 2], bUG[gi % 2], bVG[gi % 2]
            S.dma("sp", ug[:], uv[:, :, gi * GA * 128:(gi + 1) * GA * 128], writes=[bu])
            S.dma("act", vg[:], vv[:, gi * GA:(gi + 1) * GA, :], writes=[bv])
            for tt in range(4):
                for h in range(4):
                    k = (tt * 4 + h) % 2
                    S.op("pool", lambda e, tt=tt, h=h, k=k, gi=gi: e.tensor_tensor(
                        out=X[k][:], in0=Pexp[:, tt, 2 * h, gi * GA:(gi + 1) * GA].unsqueeze(2).to_broadcast([128, GA, 128]),
                        in1=Pexp[:, tt, 2 * h + 1, :].unsqueeze(1).to_broadcast([128, GA, 128]), op=ALU.mult),
                        reads=[bP[tt]], writes=[bX[k]])
                    S.op("pool", lambda e, tt=tt, h=h, k=k, gi=gi: e.tensor_tensor(
                        out=Mk[k][:], in0=P1n[:, tt, h, gi * GA:(gi + 1) * GA].unsqueeze(2).to_broadcast([128, GA, 128]),
                        in1=Pexp[:, tt, 2 * h + 1, :].unsqueeze(1).to_broadcast([128, GA, 128]), op=ALU.mult),
                        reads=[bP[tt], bTH[tt]], writes=[bMk[k]])
                    if h == 0:
                        S.op("dve", lambda e, tt=tt, h=h, k=k: e.scalar_tensor_tensor(out=Wacc[:], in0=X[k][:], scalar=TH[:, tt, h:h + 1], in1=Mk[k][:],
                                                                                      op0=ALU.is_ge, op1=ALU.mult),
                             reads=[bX[k], bMk[k], bTH[tt]], writes=[bWacc])
                    else:
                        S.op("dve", lambda e, tt=tt, h=h, k=k: e.scalar_tensor_tensor(out=X[k][:], in0=X[k][:], scalar=TH[:, tt, h:h + 1], in1=Mk[k][:],
                                                                                      op0=ALU.is_ge, op1=ALU.mult),
                             reads=[bX[k], bMk[k], bTH[tt]], writes=[bX[k]])
                        dst = Wtm[tt] if h == 3 else Wacc
                        bdst = bWtm[tt] if h == 3 else bWacc
                        S.op("dve", lambda e, k=k, dst=dst: e.tensor_tensor(out=dst[:], in0=X[k][:], in1=Wacc[:], op=ALU.add),
                             reads=[bX[k], bWacc], writes=[bdst])
            wg, bwg = WgT[gi % 2], bWg[gi % 2]
            for ac in range(GA):
                j = (gi * GA + ac) % 2
                for kc in range(8):
                    S.op("pe", lambda e, ac=ac, kc=kc, j=j, ug=ug: e.matmul(PH[:, j, :], lhsT=ug[:, kc, ac * 128:(ac + 1) * 128], rhs=xnT[:, kc, :],
                                                                          start=(kc == 0), stop=(kc == 7)),
                         reads=[bu] + b_xnT, writes=[bPH[j]])
                S.op("act", lambda e, j=j: e.activation(out=G[j][:], in_=PH[:, j, :], func=AF.Gelu), reads=[bPH[j]], writes=[bG[j]])
                for tt in range(4):
                    S.op("pe", lambda e, ac=ac, tt=tt, j=j: e.transpose(PW[j][:, tt, :], Wtm[tt][:, ac, :], consts["identb"][:]),
                         reads=[bWtm[tt], consts["b_ident"]], writes=[bPW[j]])
                S.op("dve", lambda e, ac=ac, j=j, wg=wg: e.tensor_tensor(out=wg[:, ac, :], in0=G[j][:], in1=PW[j][:, 0:4, :].rearrange("p a b -> p (a b)"), op=ALU.mult),
                     reads=[bG[j], bPW[j]], writes=[bwg])
            for tt in range(4):
                pb = 2 * (tt % 2)
                for ac in range(GA):
                    for half in range(2):
                        S.op("pe", lambda e, ac=ac, tt=tt, half=half, pb=pb, wg=wg, vg=vg: e.matmul(
                            PSA[:, pb + half, :], lhsT=wg[:, ac, tt * 128:(tt + 1) * 128], rhs=vg[:, ac, half * 512:(half + 1) * 512],
                            start=(ac == 0), stop=(ac == GA - 1)),
                            reads=[bwg, bv], writes=[bPSA[pb + half]])
                src = PSA[:, pb:pb + 2, :].rearrange("p a b -> p (a b)")
                if gi == 0:
                    S.op("act", lambda e, tt=tt, src=src: e.copy(out=QA[:, tt, :], in_=src), reads=[bPSA[pb], bPSA[pb + 1]], writes=[bQA[tt]])
                else:
                    S.op("dve", lambda e, tt=tt, src=src: e.tensor_tensor(out=QA[:, tt, :], in0=QA[:, tt, :], in1=src, op=ALU.add),
                         reads=[bPSA[pb], bPSA[pb + 1], bQA[tt]], writes=[bQA[tt]])
        for tt in range(4):
            S.op("pool", lambda e, tt=tt: e.tensor_tensor(out=QA[:, tt, :], in0=QA[:, tt, :], in1=G2[:], op=ALU.mult), reads=[bQA[tt], b_mod], writes=[bQA[tt]])
            S.op("dve", lambda e, tt=tt: e.tensor_tensor(out=HM[:, tt, :], in0=HM[:, tt, :], in1=QA[:, tt, :], op=ALU.add), reads=[bQA[tt], bHM[tt]], writes=[bHM[tt]])
        S.barrier()


def hgrn_consts_host():
    t = np.arange(128)
    ch = t // 16
    same = ch[:, None] == ch[None, :]
    BT = (same & (t[:, None] <= t[None, :])).astype(np.float32)
    RT = (same & (t[:, None] > t[None, :])).astype(np.float32)
    CI = (ch[:, None] == np.arange(8)[None, :]).astype(np.float32)
    hcA = np.concatenate([BT, RT, CI], axis=1)
    hcB = np.ascontiguousarray(CI.T).reshape(1, 1024)
    return hcA, hcB


def emit_hgrn(cx, consts, HM, bHM, mode, x_dram, ntiles, snap, mod_dram, c_sh, c_ge, c_g,
              win_dram, wout_dram, ongT_dram, lbl_dram, hcA_dram, hcB_dram, onehot_dram=None, m=0):
    S = cx.S
    full = (mode == "full")
    with ExitStack() as es:
        hcA = cx.sb(es, [128, 264]); CHM = cx.sb(es, [128, 8, 128]); b_hc = S.buf("hc")
        S.dma("sp", hcA[:], hcA_dram, writes=[b_hc])
        S.dma("pool", CHM[:].rearrange("p a b -> p (a b)"), hcB_dram[0:1, :].partition_broadcast(128), writes=[b_hc])
        BT, RT, CI = hcA[:, 0:128], hcA[:, 128:256], hcA[:, 256:264]
        LB = cx.sb(es, [128, 1024]); OML = cx.sb(es, [128, 1024]); GE = cx.sb(es, [128, 1024]); SH = cx.sb(es, [128, 1024])
        b_mod = S.buf("hmod")
        S.dma("sp", SH[:], mod_dram[:, c_sh:c_sh + 1024], writes=[b_mod])
        S.dma("sp", GE[:], mod_dram[:, c_ge:c_ge + 1024], writes=[b_mod])
        S.dma("pool", LB[:], lbl_dram[0:1, :].partition_broadcast(128), writes=[b_mod])
        S.dma("pool", OML[:], lbl_dram[1:2, :].partition_broadcast(128), writes=[b_mod])
        S.op("dve", lambda e: e.tensor_tensor(out=LB[:], in0=LB[:], in1=OML[:], op=ALU.subtract), reads=[b_mod], writes=[b_mod])
        S.op("act", lambda e: e.activation(out=LB[:], in_=LB[:], func=AF.Sigmoid), reads=[b_mod], writes=[b_mod])
        S.op("dve", lambda e: e.tensor_scalar(out=OML[:], in0=LB[:], scalar1=-1.0, scalar2=1.0, op0=ALU.mult, op1=ALU.add), reads=[b_mod], writes=[b_mod])
        win = cx.sb(es, [128, 8, 4096], BF16); b_win = S.buf("win")
        wout = cx.sb(es, [128, 8, 1024], BF16); b_wout = S.buf("wout")
        with ExitStack() as es2:
            load_cast(cx, es2, "act", lambda i: win[:, i // 4, (i % 4) * 1024:(i % 4 + 1) * 1024],
                      lambda i: win_dram[(i // 4) * 128:(i // 4 + 1) * 128, (i % 4) * 1024:(i % 4 + 1) * 1024], 32, [128, 1024], b_win)
            if full:
                G1 = cx.sb(es2, [128, 1024]); ongT = cx.sb(es2, [128, 8]); stg = [cx.sb(es2, [128, 1024]) for _ in range(2)]
                bg1, bst = S.buf(), S.bufs(2)
                S.dma("sp", G1[:], mod_dram[:, c_g:c_g + 1024], writes=[bg1])
                S.dma("sp", ongT[:], ongT_dram, writes=[bg1])
                for kc in range(8):
                    s, b = stg[kc % 2], bst[kc % 2]
                    S.dma("sp", s[:], wout_dram[kc * 128:(kc + 1) * 128, :], writes=[b])
                    S.op("dve", lambda e, s=s, kc=kc: e.tensor_scalar(out=s[:], in0=s[:], scalar1=ongT[:, kc:kc + 1], scalar2=None, op0=ALU.mult), reads=[b, bg1], writes=[b])
                    S.op("dve", lambda e, s=s, kc=kc: e.tensor_tensor(out=wout[:, kc, :], in0=s[:], in1=G1[:], op=ALU.mult), reads=[b, bg1], writes=[b_wout])
            S.barrier()
        nt = NormT(cx, es, consts)
        xt = [cx.sb(es, [128, 1024]) for _ in range(2)]; b_xt = S.bufs(2, "xt")
        hnT = cx.sb(es, [128, 8, 128], BF16); b_hnT = S.buf("hnT")
        R = [cx.sb(es, [128, 1024]) for _ in range(6)]; bR = S.bufs(6, "R")
        OGb = cx.sb(es, [128, 1024], BF16); b_og = S.buf("og")
        ogT = cx.sb(es, [128, 8, 128], BF16); b_ogT = S.buf("ogT")
        QKT = [cx.sb(es, [128, 2, 128]) for _ in range(2)]; bQKT = S.bufs(2, "QKT")
        SCM = [cx.sb(es, [128, 128]) for _ in range(2)]; bSCM = S.bufs(2, "SCM")
        QDM = cx.sb(es, [128, 8, 128]); bQDM = S.buf("QDM")
        KLM = cx.sb(es, [128, 8, 128]); bKLM = S.buf("KLM")
        ST = cx.sb(es, [128, 8, 2, 128]); bST = [S.bufs(2, "ST%d_" % h) for h in range(8)]
        EDEC = cx.sb(es, [128, 64]); bEDEC = S.buf("EDEC")
        ss8 = cx.sb(es, [128, 16]); b_ss8 = S.buf("ss8")
        PJ = [cx.ps(es, [128, 512]) for _ in range(3)]; bPJ = S.bufs(3, "PJ")
        PKV = [cx.ps(es, [128, 512]) for _ in range(2)]; bPKV = S.bufs(2, "PKV")
        PO = cx.ps(es, [128, 512]); bPO = S.buf("PO")
        PC = cx.ps(es, [128, 512]); bPC = S.buf("PC")
        pj_i = [0]

        def nextpj():
            k = pj_i[0] % 3
            pj_i[0] += 1
            return PJ[k], bPJ[k]

        def proj(j, nb):
            pj, b = nextpj()
            for kc in range(8):
                S.op("pe", lambda e, pj=pj, kc=kc: e.matmul(pj[:], lhsT=hnT[:, kc, :],
                                                            rhs=win[:, kc, j * 1024 + nb * 512:j * 1024 + (nb + 1) * 512],
                                                            start=(kc == 0), stop=(kc == 7)),
                     reads=[b_hnT, b_win], writes=[b])
            return pj, b

        if full:
            oh = cx.sb(es, [128, 4]); b_oh = S.buf("oh")
            S.dma("pool", oh[:], onehot_dram[0:1, :].partition_broadcast(128), writes=[b_oh])
            stv = ST[:, :, 0, :]
            allst = [bST[h][0] for h in range(8)]
            for j in range(4):
                S.dma("sp", R[5][:], snap[4 * m + j], writes=[bR[5]])
                r5 = R[5][:].rearrange("p (h v) -> p h v", h=8)
                if j == 0:
                    S.op("dve", lambda e, j=j, r5=r5: e.tensor_scalar(out=stv, in0=r5, scalar1=oh[:, j:j + 1], scalar2=None, op0=ALU.mult),
                         reads=[bR[5], b_oh], writes=allst)
                else:
                    S.op("dve", lambda e, j=j, r5=r5: e.scalar_tensor_tensor(out=stv, in0=r5, scalar=oh[:, j:j + 1], in1=stv, op0=ALU.mult, op1=ALU.add),
                         reads=[bR[5], b_oh] + allst, writes=allst)
        else:
            S.op("pool", lambda e: e.memset(ST[:].rearrange("p a b c -> p (a b c)"), 0.0), writes=[bST[h][s] for h in range(8) for s in range(2)])

        for ti in range(ntiles):
            x_t, bx = xt[ti % 2], b_xt[ti % 2]
            if (not full) and ti % 4 == 0:
                S.dma("pool", snap[ti // 4].rearrange("p (h v) -> p h v", h=8), ST[:, :, 0, :], reads=[bST[h][0] for h in range(8)])
            row0 = (m * 4 + ti) * 128 if full else ti * 128
            S.dma("sp", x_t[:], x_dram[row0:row0 + 128, :], writes=[bx])
            nt.normT(x_t[:], bx, GE[:], SH[:], b_mod, hnT[:], b_hnT)
            H2 = [slice(0, 512), slice(512, 1024)]
            for nb in range(2):
                pf, bpf = proj(1, nb)
                S.op("act", lambda e, pf=pf, nb=nb: e.activation(out=R[0][:, H2[nb]], in_=pf[:], func=AF.Sigmoid), reads=[bpf], writes=[bR[0]])
            S.op("dve", lambda e: e.tensor_tensor(out=R[0][:], in0=R[0][:], in1=OML[:], op=ALU.mult), reads=[bR[0], b_mod], writes=[bR[0]])
            S.op("pool", lambda e: e.tensor_tensor(out=R[0][:], in0=R[0][:], in1=LB[:], op=ALU.add), reads=[bR[0], b_mod], writes=[bR[0]])
            S.op("act", lambda e: e.activation(out=R[1][:], in_=R[0][:], func=AF.Ln), reads=[bR[0]], writes=[bR[1]])
            S.op("pool", lambda e: e.tensor_scalar(out=R[2][:], in0=R[0][:], scalar1=-1.0, scalar2=1.0, op0=ALU.mult, op1=ALU.add), reads=[bR[0]], writes=[bR[2]])
            for nb in range(2):
                pc, bpc = nextpj()
                S.op("pe", lambda e, pc=pc, nb=nb: e.matmul(pc[:], lhsT=BT, rhs=R[1][:, H2[nb]], start=True, stop=True),
                     reads=[b_hc, bR[1]], writes=[bpc])
                if full:
                    S.op("act", lambda e, pc=pc, nb=nb: e.activation(out=R[0][:, H2[nb]], in_=pc[:], func=AF.Exp), reads=[bpc], writes=[bR[0]])
                    S.op("act", lambda e, pc=pc, nb=nb: e.activation(out=R[3][:, H2[nb]], in_=pc[:], func=AF.Exp, scale=-1.0), reads=[bpc], writes=[bR[3]])
            for nb in range(2):
                pr, bpr = nextpj()
                S.op("pe", lambda e, pr=pr, nb=nb: e.matmul(pr[:], lhsT=RT, rhs=R[1][:, H2[nb]], start=True, stop=True),
                     reads=[b_hc, bR[1]], writes=[bpr])
                S.op("act", lambda e, pr=pr, nb=nb: e.activation(out=R[4][:, H2[nb]], in_=pr[:], func=AF.Exp), reads=[bpr], writes=[bR[4]])
            for h in range(8):
                S.op("pe", lambda e, h=h: e.matmul(PC[:, 384 + h * 8:384 + (h + 1) * 8], lhsT=R[1][:, h * 128:(h + 1) * 128], rhs=CI, start=True, stop=True),
                     reads=[b_hc, bR[1]], writes=[bPC])
            S.op("act", lambda e: e.activation(out=EDEC[:], in_=PC[:, 384:448], func=AF.Exp), reads=[bPC], writes=[bEDEC])
            S.op("dve", lambda e: e.tensor_tensor(out=R[4][:], in0=R[4][:], in1=R[2][:], op=ALU.mult), reads=[bR[4], bR[2]], writes=[bR[4]])
            if full:
                S.op("dve", lambda e: e.tensor_tensor(out=R[3][:], in0=R[3][:], in1=R[2][:], op=ALU.mult), reads=[bR[3], bR[2]], writes=[bR[3]])
            for nb in range(2):
                pi, bpi = proj(2, nb)
                S.op("act", lambda e, pi=pi, nb=nb: e.copy(out=R[1][:, H2[nb]], in_=pi[:]), reads=[bpi], writes=[bR[1]])
            if full:
                for nb in range(2):
                    pq, bpq = proj(0, nb)
                    S.op("act", lambda e, pq=pq, nb=nb: e.activation(out=R[2][:, H2[nb]], in_=pq[:], func=AF.Silu), reads=[bpq], writes=[bR[2]])
                S.op("dve", lambda e: e.tensor_tensor(out=R[0][:], in0=R[0][:], in1=R[2][:], op=ALU.mult), reads=[bR[0], bR[2]], writes=[bR[0]])
                for nb in range(2):
                    pg, bpg = proj(3, nb)
                    S.op("act", lambda e, pg=pg, nb=nb: e.activation(out=R[2][:, H2[nb]], in_=pg[:], func=AF.Silu), reads=[bpg], writes=[bR[2]])
            for h in range(8):
                j = h % 2
                hs = slice(h * 128, (h + 1) * 128)
                if full:
                    S.op("pe", lambda e, hs=hs: e.transpose(PC[:, 0:128], R[0][:, hs], consts["identf"][:]), reads=[bR[0], consts["b_ident"]], writes=[bPC])
                    S.op("pe", lambda e, hs=hs: e.transpose(PC[:, 128:256], R[3][:, hs], consts["identf"][:]), reads=[bR[3], consts["b_ident"]], writes=[bPC])
                    S.op("act", lambda e, j=j: e.copy(out=QKT[j][:].rearrange("p a b -> p (a b)"), in_=PC[:, 0:256]), reads=[bPC], writes=[bQKT[j]])
                    S.op("pe", lambda e, j=j: e.matmul(PC[:, 256:384], lhsT=QKT[j][:, 1, :], rhs=QKT[j][:, 0, :], start=True, stop=True), reads=[bQKT[j]], writes=[bPC])
                    S.op("dve", lambda e, j=j: e.tensor_tensor(out=SCM[j][:], in0=PC[:, 256:384], in1=BT, op=ALU.mult), reads=[bPC, b_hc], writes=[bSCM[j]])
                    S.op("pool", lambda e, j=j: e.tensor_tensor(out=QDM[:], in0=QKT[j][:, 0, :].unsqueeze(1).to_broadcast([128, 8, 128]), in1=CHM[:], op=ALU.mult),
                         reads=[bQKT[j], b_hc], writes=[bQDM])
                S.op("pool", lambda e, hs=hs: e.tensor_tensor(out=KLM[:], in0=R[4][:, hs].unsqueeze(1).to_broadcast([128, 8, 128]),
                                                              in1=CI.unsqueeze(2).to_broadcast([128, 8, 128]), op=ALU.mult),
                     reads=[bR[4], b_hc], writes=[bKLM])
                if full:
                    S.op("pe", lambda e, j=j, hs=hs: e.matmul(PO[:, 0:128], lhsT=SCM[j][:], rhs=R[1][:, hs], start=True, stop=False),
                         reads=[bSCM[j], bR[1]], writes=[bPO])
                for c in range(8):
                    k = c % 2
                    s_old, s_new = c % 2, (c + 1) % 2
                    S.op("pe", lambda e, c=c, k=k, hs=hs: e.matmul(PKV[k][:, 0:128], lhsT=KLM[:, c, :], rhs=R[1][:, hs], start=True, stop=True),
                         reads=[bKLM, bR[1]], writes=[bPKV[k]])
                    if full:
                        S.op("pe", lambda e, c=c, h=h, s_old=s_old: e.matmul(PO[:, 0:128], lhsT=QDM[:, c, :], rhs=ST[:, h, s_old, :],
                                                                            start=False, stop=(c == 7)),
                             reads=[bQDM, bST[h][s_old]], writes=[bPO])
                    S.op("dve", lambda e, c=c, k=k, h=h, s_old=s_old, s_new=s_new: e.scalar_tensor_tensor(
                        out=ST[:, h, s_new, :], in0=ST[:, h, s_old, :], scalar=EDEC[:, h * 8 + c:h * 8 + c + 1], in1=PKV[k][:, 0:128], op0=ALU.mult, op1=ALU.add),
                        reads=[bST[h][s_old], bEDEC, bPKV[k]], writes=[bST[h][s_new]])
                if full:
                    S.op("act", lambda e, hs=hs: e.copy(out=R[5][:, hs], in_=PO[:, 0:128]), reads=[bPO], writes=[bR[5]])
            if full:
                S.op("dve", lambda e: e.tensor_tensor(out=R[3][:], in0=R[5][:], in1=R[5][:], op=ALU.mult), reads=[bR[5]], writes=[bR[3]])
                S.op("dve", lambda e: e.reduce_sum(out=ss8[:, 0:8], in_=R[3][:].rearrange("p (h v) -> p h v", h=8), axis=AX.X), reads=[bR[3]], writes=[b_ss8])
                emit_rstd(cx, ss8[:, 0:8], ss8[:, 8:16], 1.0 / 128, b_ss8, b_ss8)
                S.op("dve", lambda e: e.tensor_tensor(out=R[5][:].rearrange("p (h v) -> p h v", h=8), in0=R[5][:].rearrange("p (h v) -> p h v", h=8),
                                                      in1=ss8[:, 8:16].unsqueeze(2).to_broadcast([128, 8, 128]), op=ALU.mult),
                     reads=[bR[5], b_ss8], writes=[bR[5]])
                S.op("pool", lambda e: e.tensor_tensor(out=OGb[:], in0=R[5][:], in1=R[2][:], op=ALU.mult), reads=[bR[5], bR[2]], writes=[b_og])
                nt.transpose(OGb, b_og, ogT[:], b_ogT)
                for nb in range(2):
                    pm, bpm = nextpj()
                    for kc in range(8):
                        S.op("pe", lambda e, pm=pm, nb=nb, kc=kc: e.matmul(pm[:], lhsT=ogT[:, kc, :], rhs=wout[:, kc, nb * 512:(nb + 1) * 512],
                                                                          start=(kc == 0), stop=(kc == 7)),
                             reads=[b_ogT, b_wout], writes=[bpm])
                    S.op("dve", lambda e, pm=pm, x_t=x_t, ti=ti, nb=nb: e.tensor_tensor(out=HM[:, ti, H2[nb]], in0=pm[:], in1=x_t[:, H2[nb]], op=ALU.add),
                         reads=[bpm, bx], writes=[bHM[ti]])
        if (not full) and ntiles % 4 == 0:
            S.dma("pool", snap[ntiles // 4].rearrange("p (h v) -> p h v", h=8), ST[:, :, 0, :], reads=[bST[h][0] for h in range(8)])
        S.barrier()


def attn_consts_host(r):
    j = np.arange(128)
    trin = -(j[:, None] >= j[None, :]).astype(np.float32)
    onesn = -np.ones((128, 128), np.float32)
    ac = np.concatenate([trin, onesn], axis=1).astype(ml_dtypes.bfloat16)
    k = np.arange(16)
    kp = k[None, :, None] * 128 + j[:, None, None]
    qp = 4 * r * 128 + np.arange(512)[None, None, :]
    mask = (kp < qp).astype(np.float32).astype(ml_dtypes.bfloat16)
    return ac, mask


def emit_attn(cx, consts, HM, bHM, m, mod_dram, c_sh, c_ge, c_g, wq_dram, wo_dram, KT_dram, V_dram, ac_dram, mask_dram):
    S = cx.S
    NB = 16 * (m + 1)
    with ExitStack() as es:
        SH = cx.sb(es, [128, 1024]); GE = cx.sb(es, [128, 1024]); b_mod = S.buf("amod")
        S.dma("sp", SH[:], mod_dram[:, c_sh:c_sh + 1024], writes=[b_mod])
        S.dma("sp", GE[:], mod_dram[:, c_ge:c_ge + 1024], writes=[b_mod])
        AC = cx.sb(es, [128, 256], BF16); MASK = cx.sb(es, [128, 16, 512], BF16); b_ac = S.buf("ac")
        S.dma("sp", AC[:], ac_dram, writes=[b_ac])
        S.dma("sp", MASK[:], mask_dram, writes=[b_ac])
        TRIN, ONESN = AC[:, 0:128], AC[:, 128:256]
        wq = cx.sb(es, [128, 8, 1024], BF16); b_wq = S.buf("awq")
        wo = cx.sb(es, [128, 8, 1024], BF16); b_wo = S.buf("awo")
        with ExitStack() as es2:
            load_cast(cx, es2, "act", lambda i: wq[:, i, :], lambda i: wq_dram[i * 128:(i + 1) * 128, :], 8, [128, 1024], b_wq)
            G1 = cx.sb(es2, [128, 1024]); stg = [cx.sb(es2, [128, 1024]) for _ in range(2)]
            bg1, bst = S.buf(), S.bufs(2)
            S.dma("sp", G1[:], mod_dram[:, c_g:c_g + 1024], writes=[bg1])
            for kc in range(8):
                s, b = stg[kc % 2], bst[kc % 2]
                S.dma("sp", s[:], wo_dram[kc * 128:(kc + 1) * 128, :], writes=[b])
                S.op("dve", lambda e, s=s, kc=kc: e.tensor_tensor(out=wo[:, kc, :], in0=s[:], in1=G1[:], op=ALU.mult), reads=[b, bg1], writes=[b_wo])
            S.barrier()
        nt = NormT(cx, es, consts)
        xnT = cx.sb(es, [128, 8, 512], BF16); b_xnT = S.bufs(4, "axnT")
        QT = cx.sb(es, [128, 8, 512], BF16); bQT = S.bufs(8, "QT")
        KTc = [cx.sb(es, [128, 2048], BF16) for _ in range(2)]; bKT = S.bufs(2, "KTc")
        Vc = [cx.sb(es, [128, 16, 128], BF16) for _ in range(2)]; bV = S.bufs(2, "Vc")
        E = [cx.sb(es, [128, 512]) for _ in range(2)]; bE = S.bufs(2, "E")
        LK = [cx.sb(es, [128, 512], BF16) for _ in range(2)]; bLK = S.bufs(2, "LK")
        LKS = [cx.sb(es, [128, 512], BF16) for _ in range(2)]; bLKS = S.bufs(2, "LKS")
        A = [cx.sb(es, [128, 512], BF16) for _ in range(2)]; bA = S.bufs(2, "A")
        OT = cx.sb(es, [128, 8, 512], BF16); bOT = S.bufs(8, "OT")
        PZ = [cx.ps(es, [128, 512]) for _ in range(2)]; bPZ = S.bufs(2, "PZ")
        PS = [cx.ps(es, [128, 512]) for _ in range(2)]; bPS = S.bufs(2, "PS")
        POUT = [cx.ps(es, [128, 512]) for _ in range(2)]; bPOUT = S.bufs(2, "POUT")
        for tt in range(4):
            nt.normT(HM[:, tt, :], bHM[tt], GE[:], SH[:], b_mod, xnT[:, :, tt * 128:(tt + 1) * 128], b_xnT[tt])
        sc = 1.0 / math.sqrt(128.0)
        for h in range(8):
            pz, bpz = (PZ, bPZ) if h % 4 < 2 else (PS, bPS)
            pz, bpz = pz[h % 2], bpz[h % 2]
            for kc in range(8):
                S.op("pe", lambda e, h=h, kc=kc, pz=pz: e.matmul(pz[:], lhsT=wq[:, kc, h * 128:(h + 1) * 128], rhs=xnT[:, kc, :], start=(kc == 0), stop=(kc == 7)),
                     reads=[b_wq] + b_xnT, writes=[bpz])
            S.op("act", lambda e, h=h, pz=pz: e.activation(out=QT[:, h, :], in_=pz[:], func=AF.Copy, scale=sc), reads=[bpz], writes=[bQT[h]])
        it = 0
        ld = 0
        for h in range(8):
            po, bpo = POUT[h % 2], bPOUT[h % 2]
            first = True
            for ci in range(m, -1, -1):
                kt, bkt, vc, bvc = KTc[ld % 2], bKT[ld % 2], Vc[ld % 2], bV[ld % 2]
                ld += 1
                S.dma("sp", kt[:], KT_dram[h, :, ci * 2048:(ci + 1) * 2048], writes=[bkt])
                S.dma("pool", vc[:], V_dram[h, :, ci * 16:(ci + 1) * 16, :], writes=[bvc])
                for kk in range(15, -1, -1):
                    n = ci * 16 + kk
                    masked = (ci == m)
                    i2 = it % 2
                    it += 1
                    ks = slice(kk * 128, (kk + 1) * 128)
                    S.op("pe", lambda e, i2=i2, kt=kt, ks=ks, h=h: e.matmul(PZ[i2][:], lhsT=kt[:, ks], rhs=QT[:, h, :], start=True, stop=True),
                         reads=[bkt, bQT[h]], writes=[bPZ[i2]])
                    S.op("act", lambda e, i2=i2: e.activation(out=E[i2][:], in_=PZ[i2][:], func=AF.Exp), reads=[bPZ[i2]], writes=[bE[i2]])
                    S.op("act", lambda e, i2=i2: e.activation(out=LK[i2][:], in_=E[i2][:], func=AF.Ln, bias=1.0, scale=1.0), reads=[bE[i2]], writes=[bLK[i2]])
                    if masked:
                        S.op("pool", lambda e, i2=i2, kk=kk: e.tensor_tensor(out=LK[i2][:], in0=LK[i2][:], in1=MASK[:, kk, :], op=ALU.mult),
                             reads=[bLK[i2], b_ac], writes=[bLK[i2]])
                    S.op("pe", lambda e, i2=i2, kt=kt, ks=ks, h=h: e.matmul(PS[i2][:], lhsT=kt[:, ks], rhs=QT[:, h, :], start=True, stop=False),
                         reads=[bkt, bQT[h]], writes=[bPS[i2]])
                    S.op("pe", lambda e, i2=i2, first=first: e.matmul(PS[i2][:], lhsT=TRIN, rhs=LK[i2][:], start=False, stop=first),
                         reads=[b_ac, bLK[i2]], writes=[bPS[i2]])
                    if not first:
                        S.op("pe", lambda e, i2=i2: e.matmul(PS[i2][:], lhsT=ONESN, rhs=LKS[i2][:], start=False, stop=True),
                             reads=[b_ac, bLKS[i2]], writes=[bPS[i2]])
                    S.op("act", lambda e, i2=i2: e.activation(out=A[i2][:], in_=PS[i2][:], func=AF.Exp), reads=[bPS[i2]], writes=[bA[i2]])
                    if masked:
                        S.op("dve", lambda e, i2=i2, kk=kk: e.tensor_tensor(out=A[i2][:], in0=A[i2][:], in1=MASK[:, kk, :], op=ALU.mult),
                             reads=[bA[i2], b_ac], writes=[bA[i2]])
                    last = (n == 0)
                    S.op("pe", lambda e, i2=i2, vc=vc, kk=kk, po=po, first=first, last=last: e.matmul(po[:], lhsT=vc[:, kk, :], rhs=A[i2][:], start=first, stop=last),
                         reads=[bvc, bA[i2]], writes=[bpo])
                    nx = it % 2
                    if first:
                        S.op("pool", lambda e, i2=i2, nx=nx: e.tensor_copy(out=LKS[nx][:], in_=LK[i2][:]), reads=[bLK[i2]], writes=[bLKS[nx]])
                    elif not last:
                        S.op("pool", lambda e, i2=i2, nx=nx: e.tensor_tensor(out=LKS[nx][:], in0=LKS[i2][:], in1=LK[i2][:], op=ALU.add),
                             reads=[bLK[i2], bLKS[i2]], writes=[bLKS[nx]])
                    first = False
            S.op("dve", lambda e, h=h, po=po: e.tensor_copy(out=OT[:, h, :], in_=po[:]), reads=[bpo], writes=[bOT[h]])
        for tt in range(4):
            for nb in range(2):
                S_ps, b_ps = PZ[nb], bPZ[nb]
                for h in range(8):
                    S.op("pe", lambda e, tt=tt, nb=nb, h=h, S_ps=S_ps: e.matmul(S_ps[:], lhsT=OT[:, h, tt * 128:(tt + 1) * 128], rhs=wo[:, h, nb * 512:(nb + 1) * 512],
                                                                             start=(h == 0), stop=(h == 7)),
                         reads=[bOT[h], b_wo], writes=[b_ps])
                S.op("dve", lambda e, tt=tt, nb=nb, S_ps=S_ps: e.tensor_tensor(out=HM[:, tt, nb * 512:(nb + 1) * 512], in0=HM[:, tt, nb * 512:(nb + 1) * 512], in1=S_ps[:], op=ALU.add),
                     reads=[b_ps, bHM[tt]], writes=[bHM[tt]])
        S.barrier()


def emit_kv(cx, consts, HM, bHM, m, mod_dram, c_sh, c_ge, kvw_dram, KTo, Vo):
    S = cx.S
    with ExitStack() as es:
        SH = cx.sb(es, [128, 1024]); GE = cx.sb(es, [128, 1024]); b_mod = S.buf("kmod")
        S.dma("sp", SH[:], mod_dram[:, c_sh:c_sh + 1024], writes=[b_mod])
        S.dma("sp", GE[:], mod_dram[:, c_ge:c_ge + 1024], writes=[b_mod])
        kvw = cx.sb(es, [128, 8, 2048], BF16); b_kvw = S.buf("kvw")
        with ExitStack() as es2:
            load_cast(cx, es2, "act", lambda i: kvw[:, i // 2, (i % 2) * 1024:(i % 2 + 1) * 1024],
                      lambda i: kvw_dram[(i // 2) * 128:(i // 2 + 1) * 128, (i % 2) * 1024:(i % 2 + 1) * 1024], 16, [128, 1024], b_kvw)
            S.barrier()
        nt = NormT(cx, es, consts)
        xnT = cx.sb(es, [128, 8, 512], BF16); b_xnT = S.bufs(4, "kxnT")
        KTs = cx.sb(es, [128, 8, 512], BF16); bKTs = S.buf("KTs")
        Vs = [cx.sb(es, [128, 1024], BF16) for _ in range(2)]; bVs = S.bufs(2, "Vs")
        PK = [cx.ps(es, [128, 512]) for _ in range(4)]; bPK = S.bufs(4, "PK")
        for tt in range(4):
            nt.normT(HM[:, tt, :], bHM[tt], GE[:], SH[:], b_mod, xnT[:, :, tt * 128:(tt + 1) * 128], b_xnT[tt])
        for h in range(8):
            pk, bpk = PK[h % 4], bPK[h % 4]
            for kc in range(8):
                S.op("pe", lambda e, h=h, kc=kc, pk=pk: e.matmul(pk[:], lhsT=kvw[:, kc, h * 128:(h + 1) * 128], rhs=xnT[:, kc, :], start=(kc == 0), stop=(kc == 7)),
                     reads=[b_kvw] + b_xnT, writes=[bpk])
            S.op("act", lambda e, h=h, pk=pk: e.copy(out=KTs[:, h, :], in_=pk[:]), reads=[bpk], writes=[bKTs])
        S.dma("sp", KTo[m].rearrange("h d t -> d h t"), KTs[:], reads=[bKTs])
        for tt in range(4):
            vs, bvs = Vs[tt % 2], bVs[tt % 2]
            for nb in range(2):
                pk, bpk = PK[(tt * 2 + nb) % 4], bPK[(tt * 2 + nb) % 4]
                for kc in range(8):
                    S.op("pe", lambda e, tt=tt, nb=nb, kc=kc, pk=pk: e.matmul(pk[:], lhsT=xnT[:, kc, tt * 128:(tt + 1) * 128],
                                                                             rhs=kvw[:, kc, 1024 + nb * 512:1024 + (nb + 1) * 512], start=(kc == 0), stop=(kc == 7)),
                         reads=[b_kvw, b_xnT[tt]], writes=[bpk])
                S.op("dve", lambda e, nb=nb, pk=pk, vs=vs: e.tensor_copy(out=vs[:, nb * 512:(nb + 1) * 512], in_=pk[:]), reads=[bpk], writes=[bvs])
            S.dma("sp", Vo[m * 512 + tt * 128:m * 512 + (tt + 1) * 128, :], vs[:], reads=[bvs])
        S.barrier()


def emit_store(cx, HM, bHM, m, out):
    S = cx.S
    for tt in range(4):
        S.dma("sp", out[m * 512 + tt * 128:m * 512 + (tt + 1) * 128, :], HM[:, tt, :], reads=[bHM[tt]])


def emit_final(cx, consts, HM, bHM, m, g_dram, out):
    S = cx.S
    with ExitStack() as es:
        G = cx.sb(es, [128, 1024]); bg = S.buf("fg")
        S.dma("pool", G[:], g_dram[0:1, :].partition_broadcast(128), writes=[bg])
        junk = cx.sb(es, [128, 1024], BF16); st = cx.sb(es, [128, 8]); bj, bst = S.buf(), S.buf()
        o = [cx.sb(es, [128, 1024]) for _ in range(2)]; bo = S.bufs(2, "fo")
        for tt in range(4):
            S.op("act", lambda e, tt=tt: e.activation(out=junk[:], in_=HM[:, tt, :], func=AF.Square, accum_out=st[:, 2 * tt:2 * tt + 1]),
                 reads=[bHM[tt]], writes=[bj, bst])
            emit_rstd(cx, st[:, 2 * tt:2 * tt + 1], st[:, 2 * tt + 1:2 * tt + 2], 1.0 / D, bst, bst)
            S.op("dve", lambda e, tt=tt: e.scalar_tensor_tensor(out=o[tt % 2][:], in0=HM[:, tt, :], scalar=st[:, 2 * tt + 1:2 * tt + 2], in1=G[:], op0=ALU.mult, op1=ALU.mult),
                 reads=[bHM[tt], bst, bg], writes=[bo[tt % 2]])
            S.dma("sp", out[m * 512 + tt * 128:m * 512 + (tt + 1) * 128, :], o[tt % 2][:], reads=[bo[tt % 2]])
        S.barrier()


def build_l1(NM, do_peer=True):
    SQ = 2048 * NM
    NQG = 4 * NM
    nc = bass.Bass("TRN2", target_bir_lowering=False)
    with ExitStack() as es:
        cx = Ctx(nc, es)
        S = cx.S
        I = lambda n, s, dt=F32: nc.dram_tensor(n, list(s), dt, kind="ExternalInput").ap()
        O = lambda n, s, dt=F32: nc.dram_tensor(n, list(s), dt, kind="ExternalOutput").ap()
        xb = I("xb", [SQ, D]); xo = I("xo", [NM * 512, D]); cT = I("cT", [128, 8])
        ada_w = I("ada_w", [D, 6 * D]); ada_b = I("ada_b", [1, 6 * D]); kva_w = I("kva_w", [D, 2 * D]); kva_b = I("kva_b", [1, 2 * D])
        gmix = I("gmix", [1, D]); gffn = I("gffn", [1, D]); gkv = I("gkv", [1, D])
        win = I("win", [D, 4 * D]); wout = I("wout", [D, D]); ongT = I("ongT", [128, 8]); lbl = I("lbl", [2, D])
        hcA = I("hcA", [128, 264]); hcB = I("hcB", [1, 1024]); oh = I("oh", [1, 4])
        kvw = I("kvw", [D, 2 * D]); pwq = I("pwq", [D, D]); skT = I("skT", [8, 128, 128])
        uT = I("uT", [D, NEXP]); v = I("v", [NEXP, D])
        h1 = O("h1", [NM * 512, D]); KTo = O("KTo", [NM, 8, 128, 512], BF16); Vo = O("Vo", [NM * 512, D], BF16)
        mod = cx.dram("mod", [128, 8 * D], F32)
        snapt = cx.dram("snap", [NQG, 128, D], F32)
        snap = [snapt[g] for g in range(NQG)]
        uTb = cx.dram("uTb", [D, NEXP], BF16); vb = cx.dram("vb", [NEXP, D], BF16)
        consts = make_consts(cx, es)
        emit_mod(cx, cT, [(ada_w, ada_b, 0, 6 * D), (kva_w, kva_b, 6 * D, 2 * D)], mod)
        emit_geff(cx, mod, 1 * D, gmix)
        emit_geff(cx, mod, 4 * D, gffn)
        emit_geff(cx, mod, 7 * D, gkv)
        if do_peer:
            emit_precast(cx, uT, uTb, D, NEXP)
            emit_precast(cx, v, vb, NEXP, D)
        HM = cx.sb(es, [128, 4, D]); bHM = S.bufs(4, "HM")
        emit_hgrn(cx, consts, HM, bHM, "state", xb, 4 * (NQG - 1), snap, mod, 0, D, 2 * D, win, wout, ongT, lbl, hcA, hcB)
        for m in range(NM):
            emit_hgrn(cx, consts, HM, bHM, "full", xo, 4, snap, mod, 0, D, 2 * D, win, wout, ongT, lbl, hcA, hcB, onehot_dram=oh, m=m)
            if do_peer:
                emit_peer(cx, consts, HM, bHM, mod, 3 * D, 4 * D, 5 * D, pwq, skT, uTb, vb)
            emit_store(cx, HM, bHM, m, h1)
            emit_kv(cx, consts, HM, bHM, m, mod, 6 * D, 7 * D, kvw, KTo, Vo)
        S.barrier()
        S.emit()
    return nc


def build_l2(NM, do_peer=True):
    SQ = 2048 * NM
    nc = bass.Bass("TRN2", target_bir_lowering=False)
    with ExitStack() as es:
        cx = Ctx(nc, es)
        S = cx.S
        I = lambda n, s, dt=F32: nc.dram_tensor(n, list(s), dt, kind="ExternalInput").ap()
        O = lambda n, s, dt=F32: nc.dram_tensor(n, list(s), dt, kind="ExternalOutput").ap()
        h1 = I("h1", [NM * 512, D]); cT = I("cT", [128, 8])
        ada_w = I("ada_w", [D, 6 * D]); ada_b = I("ada_b", [1, 6 * D])
        gmix = I("gmix", [1, D]); gffn = I("gffn", [1, D]); gfin = I("gfin", [1, D])
        sbwq = I("sbwq", [D, D]); sbwo = I("sbwo", [D, D])
        KT = I("KT", [8, 128, SQ], BF16); V = I("V", [8, 128, SQ // 128, 128], BF16)
        ac = I("ac", [128, 256], BF16); mask = I("mask", [128, 16, 512], BF16)
        pwq = I("pwq", [D, D]); skT = I("skT", [8, 128, 128]); uT = I("uT", [D, NEXP]); v = I("v", [NEXP, D])
        out = O("out", [NM * 512, D])
        mod = cx.dram("mod", [128, 6 * D], F32)
        uTb = cx.dram("uTb", [D, NEXP], BF16); vb = cx.dram("vb", [NEXP, D], BF16)
        consts = make_consts(cx, es)
        emit_mod(cx, cT, [(ada_w, ada_b, 0, 6 * D)], mod)
        emit_geff(cx, mod, 1 * D, gmix)
        emit_geff(cx, mod, 4 * D, gffn)
        if do_peer:
            emit_precast(cx, uT, uTb, D, NEXP)
            emit_precast(cx, v, vb, NEXP, D)
        HM = cx.sb(es, [128, 4, D]); bHM = S.bufs(4, "HM")
        for m in range(NM):
            for tt in range(4):
                S.dma("sp", HM[:, tt, :], h1[m * 512 + tt * 128:m * 512 + (tt + 1) * 128, :], writes=[bHM[tt]])
            emit_attn(cx, consts, HM, bHM, m, mod, 0, D, 2 * D, sbwq, sbwo, KT, V, ac, mask)
            if do_peer:
                emit_peer(cx, consts, HM, bHM, mod, 3 * D, 4 * D, 5 * D, pwq, skT, uTb, vb)
            emit_final(cx, consts, HM, bHM, m, gfin, out)
        S.barrier()
        S.emit()
    return nc


def run_model(inp, NM, do_peer=True, runner=None):
    f32 = lambda a: np.ascontiguousarray(np.asarray(a, dtype=np.float32))
    x = f32(inp["x"]); c = f32(inp["c"])
    B = x.shape[0]
    SQ = 2048 * NM
    assert x.shape == (B, SQ, D) and B == 2
    ncores = 8
    if runner is None:
        runner = lambda nc, maps: run_bass_kernel_spmd(nc, maps, core_ids=list(range(len(maps)))).results
    hcA, hcB = hgrn_consts_host()
    row = lambda a: f32(a).reshape(1, -1)
    colT = lambda a: np.ascontiguousarray(f32(a).reshape(8, 128).T)
    own = lambda b, r: np.concatenate([np.arange((4 * m + r) * 512, (4 * m + r + 1) * 512) for m in range(NM)])

    def peer_w(l):
        sk = f32(inp["peer_subkeys"][l]).reshape(8, 128, 128)
        return {"pwq": f32(inp["peer_w_q"][l]), "skT": np.ascontiguousarray(sk.transpose(0, 2, 1)),
                "uT": np.ascontiguousarray(f32(inp["peer_u"][l]).T), "v": f32(inp["peer_v"][l])}

    pw0 = peer_w(0)
    shared1 = {"ada_w": f32(inp["ada_w"][0]), "ada_b": row(inp["ada_b"][0]), "kva_w": f32(inp["kv_ada_w"]), "kva_b": row(inp["kv_ada_b"]),
               "gmix": row(inp["norm_mix_g"][0]), "gffn": row(inp["norm_ffn_g"][0]), "gkv": row(inp["kv_norm_g"]),
               "win": f32(inp["hgrn_w_in"][0]), "wout": f32(inp["hgrn_w_out"][0]), "ongT": colT(inp["hgrn_onorm_g"][0]),
               "lbl": f32(inp["hgrn_lb_logits"]), "hcA": hcA, "hcB": hcB, "kvw": f32(inp["kv_w"]), **pw0}
    maps = []
    for core in range(ncores):
        b, r = core // 4, core % 4
        oh = np.zeros((1, 4), np.float32); oh[0, r] = 1.0
        maps.append({"xb": x[b], "xo": np.ascontiguousarray(x[b][own(b, r)]), "cT": colT(c[b]), "oh": oh, **shared1})
    nc1 = build_l1(NM, do_peer)
    res1 = runner(nc1, maps)
    del maps, shared1, pw0
    KTf = np.zeros((B, 8, 128, SQ), ml_dtypes.bfloat16)
    Vf = np.zeros((B, SQ, D), ml_dtypes.bfloat16)
    for core in range(ncores):
        b, r = core // 4, core % 4
        kto = np.asarray(res1[core]["KTo"]); vo = np.asarray(res1[core]["Vo"])
        for m in range(NM):
            g = 4 * m + r
            KTf[b, :, :, g * 512:(g + 1) * 512] = kto[m]
            Vf[b, g * 512:(g + 1) * 512] = vo[m * 512:(m + 1) * 512]
    Vl = np.ascontiguousarray(Vf.reshape(B, SQ // 128, 128, 8, 128).transpose(0, 3, 2, 1, 4))
    pw1 = peer_w(1)
    shared2 = {"ada_w": f32(inp["ada_w"][1]), "ada_b": row(inp["ada_b"][1]), "gmix": row(inp["norm_mix_g"][1]), "gffn": row(inp["norm_ffn_g"][1]),
               "gfin": row(inp["final_norm_g"]), "sbwq": f32(inp["sb_w_q"][0]), "sbwo": f32(inp["sb_w_out"][0]), **pw1}
    maps = []
    for core in range(ncores):
        b, r = core // 4, core % 4
        ac, mask = attn_consts_host(r)
        maps.append({"h1": np.asarray(res1[core]["h1"]), "cT": colT(c[b]), "KT": KTf[b], "V": Vl[b], "ac": ac, "mask": mask, **shared2})
    nc2 = build_l2(NM, do_peer)
    res2 = runner(nc2, maps)
    out = np.zeros((B, SQ, D), np.float32)
    for core in range(ncores):
        b, r = core // 4, core % 4
        out[b, own(b, r)] = np.asarray(res2[core]["out"])
    return out


def kernel(**inputs):
    return run_model(inputs, 8)
```

```python
from contextlib import ExitStack
import math
import numpy as np
import ml_dtypes
import concourse.bass as bass
import concourse.mybir as mybir
from concourse.bass_utils import run_bass_kernel_spmd

F32 = mybir.dt.float32
BF16 = mybir.dt.bfloat16
ALU = mybir.AluOpType
AF = mybir.ActivationFunctionType
AX = mybir.AxisListType


class Buf:
    __slots__ = ("name", "w", "r")

    def __init__(self, name):
        self.name = name
        self.w = None
        self.r = []


class Sched:
    ENGS = ("pe", "act", "dve", "pool", "sp")
    NDMA = 8
    ROT = 20000

    def __init__(self, nc, es):
        self.nc = nc
        self.streams = {e: [] for e in self.ENGS}
        self.sem = {}
        self.cnt = {}
        for e in self.ENGS:
            self.sem[e] = es.enter_context(nc.semaphore("s_" + e))
            self.cnt[e] = 0
        self.dsem = {}
        self.dcnt = {}
        for e in ("sp", "act", "pool"):
            self.dsem[e] = [es.enter_context(nc.semaphore("d_%s%d" % (e, i))) for i in range(self.NDMA)]
            self.dcnt[e] = 0
        self.waited = {e: {} for e in self.ENGS}
        self.nbuf = 0
        self.es = es
        self.nrot = 0

    def buf(self, name=None):
        self.nbuf += 1
        return Buf(name or "b%d" % self.nbuf)

    def bufs(self, n, name="b"):
        return [self.buf("%s%d" % (name, i)) for i in range(n)]

    def _need(self, eng, reads, writes, same_ok):
        need = {}

        def add(tok):
            if tok is None:
                return
            sem, val, src = tok
            if same_ok and src == eng:
                return
            k = id(sem)
            if k not in need or need[k][1] < val:
                need[k] = (sem, val)

        for b in reads:
            add(b.w)
        for b in writes:
            add(b.w)
            for t in b.r:
                add(t)
        out = []
        wd = self.waited[eng]
        for k, (sem, val) in need.items():
            if wd.get(k, 0) >= val:
                continue
            wd[k] = val
            out.append((sem, val))
        return out

    def op(self, eng, fn, reads=(), writes=()):
        waits = self._need(eng, reads, writes, same_ok=(eng == "pe"))
        self.cnt[eng] += 1
        tok = (self.sem[eng], self.cnt[eng], eng)
        self.streams[eng].append((waits, fn, (self.sem[eng], 1)))
        for b in reads:
            b.r.append(tok)
        for b in writes:
            b.w = tok
            b.r = []
        return tok

    def dma(self, q, out, in_, reads=(), writes=(), **kw):
        j = self.dcnt[q]
        self.dcnt[q] += 1
        sem = self.dsem[q][j % self.NDMA]
        val = 16 * (j // self.NDMA + 1)
        waits = self._need(q, reads, writes, same_ok=False)
        if j >= self.NDMA:
            prev = val - 16
            wd = self.waited[q]
            if wd.get(id(sem), 0) < prev:
                wd[id(sem)] = prev
                waits.append((sem, prev))
        tok = (sem, val, "dma_" + q)
        self.streams[q].append((waits, lambda e: e.dma_start(out=out, in_=in_, **kw), (sem, 16)))
        for b in reads:
            b.r.append(tok)
        for b in writes:
            b.w = tok
            b.r = []
        return tok

    def coll(self, kind, groups, src, dst, reads=(), writes=()):
        q = "pool"
        j = self.dcnt[q]
        self.dcnt[q] += 1
        sem = self.dsem[q][j % self.NDMA]
        val = 16 * (j // self.NDMA + 1)
        waits = self._need(q, reads, writes, same_ok=False)
        if j >= self.NDMA:
            prev = val - 16
            wd = self.waited[q]
            if wd.get(id(sem), 0) < prev:
                wd[id(sem)] = prev
                waits.append((sem, prev))
        tok = (sem, val, "dma_" + q)
        self.streams[q].append((waits, lambda e: e.collective_compute(kind, ALU.bypass, groups, [src], [dst]), (sem, 16)))
        for b in reads:
            b.r.append(tok)
        for b in writes:
            b.w = tok
            b.r = []
        return tok

    def wait_all(self, eng, bufs):
        waits = self._need(eng, bufs, (), same_ok=False)
        self.streams[eng].append((waits, None, None))

    def barrier(self):
        toks = []
        for e in self.ENGS:
            if self.cnt[e]:
                toks.append((self.sem[e], self.cnt[e]))
        for q in self.dsem:
            j = self.dcnt[q]
            for s in range(min(j, self.NDMA)):
                last = ((j - 1 - s) // self.NDMA) if j - 1 >= s else -1
                uses = (j - s + self.NDMA - 1) // self.NDMA
                toks.append((self.dsem[q][s], 16 * uses))
        for e in self.ENGS:
            waits = []
            wd = self.waited[e]
            for sem, val in toks:
                if wd.get(id(sem), 0) >= val:
                    continue
                wd[id(sem)] = val
                waits.append((sem, val))
            if waits:
                self.streams[e].append((waits, None, None))
        for e in self.ENGS:
            if self.cnt[e] > self.ROT:
                self.sem[e] = self.es.enter_context(self.nc.semaphore("s_%s_%d" % (e, self.nrot)))
                self.nrot += 1
                self.cnt[e] = 0
        for q in self.dsem:
            if 16 * (self.dcnt[q] // self.NDMA + 1) > self.ROT:
                self.dsem[q] = [self.es.enter_context(self.nc.semaphore("d_%s%d_%d" % (q, i, self.nrot))) for i in range(self.NDMA)]
                self.nrot += 1
                self.dcnt[q] = 0

    def scatter(self, out, idx_ap, in_, reads=(), writes=()):
        q = "pool"
        j = self.dcnt[q]
        self.dcnt[q] += 1
        sem = self.dsem[q][j % self.NDMA]
        val = 16 * (j // self.NDMA + 1)
        waits = self._need(q, reads, writes, same_ok=False)
        if j >= self.NDMA:
            prev = val - 16
            wd = self.waited[q]
            if wd.get(id(sem), 0) < prev:
                wd[id(sem)] = prev
                waits.append((sem, prev))
        tok = (sem, val, "dma_" + q)
        self.streams[q].append((waits, lambda e: e.indirect_dma_start(out=out, out_offset=bass.IndirectOffsetOnAxis(ap=idx_ap, axis=0),
                                                                      in_=in_, in_offset=None, bounds_check=out.shape[0] - 1, oob_is_err=False), (sem, 16)))
        for b in reads:
            b.r.append(tok)
        for b in writes:
            b.w = tok
            b.r = []
        return tok

    def core_barrier(self):
        self.barrier()
        self.emit()
        self.streams = {e: [] for e in self.ENGS}
        self.nc.all_core_barrier()

    def emit(self):
        nc = self.nc
        handles = {"pe": "tensor", "act": "scalar", "dve": "vector", "pool": "gpsimd", "sp": "sync"}
        with nc.Block() as block:
            for e in self.ENGS:
                stream = self.streams[e]

                def body(eng, stream=stream):
                    for waits, fn, inc in stream:
                        for sem, val in waits:
                            eng.wait_ge(sem, val)
                        if fn is not None:
                            ins = fn(eng)
                            ins.then_inc(inc[0], inc[1])

                getattr(block, handles[e])(body)


D = 1024
NEXP = 16384
EPS = 1e-6


class Ctx:
    def __init__(self, nc, es):
        self.nc = nc
        self.es = es
        self.S = Sched(nc, es)
        self.n = 0

    def sb(self, es, shape, dt=F32, name=None):
        self.n += 1
        return es.enter_context(self.nc.sbuf_tensor(name or "t%d" % self.n, list(shape), dt))

    def ps(self, es, shape, dt=F32, name=None):
        self.n += 1
        return es.enter_context(self.nc.psum_tensor(name or "p%d" % self.n, list(shape), dt))

    def dram(self, name, shape, dt, kind="Internal"):
        return self.nc.dram_tensor(name, list(shape), dt, kind=kind).ap()


def make_consts(cx, es):
    S = cx.S
    c = {}
    identf = cx.sb(es, [128, 128], F32)
    identb = cx.sb(es, [128, 128], BF16)
    b = S.buf("ident")
    S.op("pool", lambda e: e.memset(identf[:], 1.0), writes=[b])
    S.op("pool", lambda e: e.affine_select(out=identf[:], in_=identf[:], pattern=[[-1, 128]],
                                           compare_op=ALU.is_equal, fill=0.0, base=0, channel_multiplier=1),
         reads=[b], writes=[b])
    S.op("dve", lambda e: e.tensor_copy(out=identb[:], in_=identf[:]), reads=[b], writes=[b])
    c["identf"], c["identb"], c["b_ident"] = identf, identb, b
    return c


def emit_mod(cx, cT_ap, specs, out_dram):
    S = cx.S
    with ExitStack() as es:
        cs = cx.sb(es, [128, 8])
        sg = cx.sb(es, [128, 8])
        CA = cx.sb(es, [128, 8, 128])
        wst = [cx.sb(es, [128, 2048]) for _ in range(2)]
        bwst = S.bufs(2, "wst")
        bias = cx.sb(es, [128, 2048])
        res = cx.sb(es, [128, 2048])
        pm = cx.ps(es, [128, 4, 512])
        b_cs, b_CA, b_bias, b_res, b_pm = S.buf(), S.buf(), S.buf(), S.buf(), S.buf()
        S.dma("sp", cs[:], cT_ap, writes=[b_cs])
        S.op("act", lambda e: e.activation(out=sg[:], in_=cs[:], func=AF.Sigmoid), reads=[b_cs], writes=[b_CA])
        S.op("dve", lambda e: e.tensor_tensor(out=cs[:], in0=cs[:], in1=sg[:], op=ALU.mult), reads=[b_cs, b_CA], writes=[b_cs])
        S.op("dve", lambda e: e.tensor_copy(out=CA[:], in_=cs[:].unsqueeze(2).to_broadcast([128, 8, 128])),
             reads=[b_cs], writes=[b_CA])
        it = 0
        for (W, B, col0, ncols) in specs:
            for cb in range(0, ncols, 2048):
                S.dma("pool", bias[:], B[0:1, cb:cb + 2048].partition_broadcast(128), writes=[b_bias])
                for kc in range(8):
                    w = wst[it % 2]
                    bw = bwst[it % 2]
                    it += 1
                    S.dma("sp", w[:], W[kc * 128:(kc + 1) * 128, cb:cb + 2048], writes=[bw])
                    for nb in range(4):
                        S.op("pe", lambda e, w=w, nb=nb, kc=kc: e.matmul(pm[:, nb, :], lhsT=CA[:, kc, :], rhs=w[:, nb * 512:(nb + 1) * 512],
                                                                        start=(kc == 0), stop=(kc == 7)),
                             reads=[b_CA, bw], writes=[b_pm])
                S.op("dve", lambda e: e.tensor_tensor(out=res[:], in0=pm[:].rearrange("p a b -> p (a b)"), in1=bias[:], op=ALU.add),
                     reads=[b_pm, b_bias], writes=[b_res])
                S.dma("sp", out_dram[:, col0 + cb:col0 + cb + 2048], res[:], reads=[b_res])
        S.barrier()


def emit_rstd(cx, ss_ap, rstd_ap, inv_n, b_ss, b_rstd):
    S = cx.S
    S.op("dve", lambda e: e.tensor_scalar(out=rstd_ap, in0=ss_ap, scalar1=inv_n, scalar2=EPS, op0=ALU.mult, op1=ALU.add),
         reads=[b_ss], writes=[b_rstd])
    S.op("act", lambda e: e.activation(out=rstd_ap, in_=rstd_ap, func=AF.Ln), reads=[b_rstd], writes=[b_rstd])
    S.op("act", lambda e: e.activation(out=rstd_ap, in_=rstd_ap, func=AF.Exp, scale=-0.5), reads=[b_rstd], writes=[b_rstd])


class NormT:
    def __init__(self, cx, es, consts, pt=None, b_pt=None):
        self.cx = cx
        self.c = consts
        S = cx.S
        self.junk = cx.sb(es, [128, 1024], BF16)
        self.st = cx.sb(es, [128, 4])
        self.tmp = cx.sb(es, [128, 1024])
        self.hb = cx.sb(es, [128, 1024], BF16)
        self.pt = pt if pt is not None else cx.ps(es, [128, 8, 128], BF16)
        self.b_junk, self.b_st, self.b_tmp, self.b_hb = S.buf(), S.buf(), S.buf(), S.buf()
        self.b_pt = b_pt if b_pt is not None else S.buf()

    def norm(self, x_ap, b_x, geff, shift, b_mod, out_ap, b_out, out_eng="pool"):
        S = self.cx.S
        st, tmp = self.st, self.tmp
        S.op("act", lambda e: e.activation(out=self.junk[:], in_=x_ap, func=AF.Square, accum_out=st[:, 0:1]),
             reads=[b_x], writes=[self.b_junk, self.b_st])
        emit_rstd(self.cx, st[:, 0:1], st[:, 1:2], 1.0 / D, self.b_st, self.b_st)
        S.op("dve", lambda e: e.scalar_tensor_tensor(out=tmp[:], in0=x_ap, scalar=st[:, 1:2], in1=geff, op0=ALU.mult, op1=ALU.mult),
             reads=[b_x, self.b_st, b_mod], writes=[self.b_tmp])
        S.op(out_eng, lambda e: e.tensor_tensor(out=out_ap, in0=tmp[:], in1=shift, op=ALU.add),
             reads=[self.b_tmp, b_mod], writes=[b_out])

    def transpose(self, in_bf, b_in, outT_ap, b_out):
        S = self.cx.S
        for kc in range(8):
            S.op("pe", lambda e, kc=kc: e.transpose(self.pt[:, kc, :], in_bf[:, kc * 128:(kc + 1) * 128], self.c["identb"][:]),
                 reads=[b_in, self.c["b_ident"]], writes=[self.b_pt])
        S.op("act", lambda e: e.copy(out=outT_ap, in_=self.pt[:, :, :]), reads=[self.b_pt], writes=[b_out])

    def normT(self, x_ap, b_x, geff, shift, b_mod, outT_ap, b_out):
        self.norm(x_ap, b_x, geff, shift, b_mod, self.hb[:], self.b_hb)
        self.transpose(self.hb, self.b_hb, outT_ap, b_out)


def load_cast(cx, es_tmp, q, dst_ap_fn, src_ap_fn, nchunks, shape, b_dst, cast_eng="pool"):
    S = cx.S
    stg = [cx.sb(es_tmp, shape) for _ in range(2)]
    bst = S.bufs(2, "stg")
    for i in range(nchunks):
        s, b = stg[i % 2], bst[i % 2]
        S.dma(q, s[:], src_ap_fn(i), writes=[b])
        S.op(cast_eng, lambda e, s=s, i=i: e.tensor_copy(out=dst_ap_fn(i), in_=s[:]), reads=[b], writes=[b_dst])


def emit_geff(cx, mod_dram, col, g_dram):
    S = cx.S
    with ExitStack() as es:
        a = cx.sb(es, [128, 1024])
        g = cx.sb(es, [128, 1024])
        ba, bg = S.buf(), S.buf()
        S.dma("sp", a[:], mod_dram[:, col:col + 1024], writes=[ba])
        S.dma("pool", g[:], g_dram[0:1, :].partition_broadcast(128), writes=[bg])
        S.op("dve", lambda e: e.scalar_tensor_tensor(out=a[:], in0=a[:], scalar=1.0, in1=g[:], op0=ALU.add, op1=ALU.mult),
             reads=[ba, bg], writes=[ba])
        S.dma("sp", mod_dram[:, col:col + 1024], a[:], reads=[ba])
        S.barrier()


def emit_precast(cx, src, dst, rows, cols):
    S = cx.S
    with ExitStack() as es:
        CW = 4096
        st = [cx.sb(es, [128, CW]) for _ in range(2)]
        ob = [cx.sb(es, [128, CW], BF16) for _ in range(2)]
        bs, bo = S.bufs(2), S.bufs(2)
        srcv = src.rearrange("(a p) c -> p a c", p=128)
        dstv = dst.rearrange("(a p) c -> p a c", p=128)
        i = 0
        per = max(1, CW // cols)
        cw = min(CW, cols)
        for a0 in range(0, rows // 128, per):
            for c0 in range(0, cols, cw):
                s, o = st[i % 2], ob[i % 2]
                sv = s[:].rearrange("p (a c) -> p a c", a=per)
                ov = o[:].rearrange("p (a c) -> p a c", a=per)
                S.dma("sp" if i % 2 == 0 else "act", sv, srcv[:, a0:a0 + per, c0:c0 + cw], writes=[bs[i % 2]])
                eng = ("pool", "dve")[i % 2]
                S.op(eng, lambda e, s=s, o=o: e.tensor_copy(out=o[:], in_=s[:]), reads=[bs[i % 2]], writes=[bo[i % 2]])
                S.dma("pool", dstv[:, a0:a0 + per, c0:c0 + cw], ov, reads=[bo[i % 2]])
                i += 1
        S.barrier()


def emit_peer(cx, consts, HM, bHM, mod_dram, c_sh, c_ge, c_g, wq_dram, skT_dram, uTb, vb):
    S = cx.S
    GA = 4
    NG = 128 // GA
    with ExitStack() as es:
        G2 = cx.sb(es, [128, 1024]); b_g2 = S.buf("g2")
        S.dma("sp", G2[:], mod_dram[:, c_g:c_g + 1024], writes=[b_g2])
        xnT = cx.sb(es, [128, 8, 512], BF16); b_xnT = S.bufs(4, "xnT")
        QA = cx.sb(es, [128, 4, 1024]); bQA = S.bufs(4, "QA")
        PSA = cx.ps(es, [128, 4, 512]); bPSA = S.bufs(4, "PSA")
        Pexp = cx.sb(es, [128, 4, 8, 128]); bP = S.bufs(4, "Pexp")
        TH = cx.sb(es, [128, 4, 4]); RZ = cx.sb(es, [128, 4, 4]); P1n = cx.sb(es, [128, 4, 4, 128]); bTH = S.bufs(4, "TH")
        with ExitStack() as es1:
            SH = cx.sb(es1, [128, 1024]); GE = cx.sb(es1, [128, 1024]); b_mod = S.buf("mod")
            S.dma("sp", SH[:], mod_dram[:, c_sh:c_sh + 1024], writes=[b_mod])
            S.dma("sp", GE[:], mod_dram[:, c_ge:c_ge + 1024], writes=[b_mod])
            wq = cx.sb(es1, [128, 8, 1024], BF16); b_wq = S.buf("wq")
            with ExitStack() as es2:
                load_cast(cx, es2, "act", lambda i: wq[:, i, :], lambda i: wq_dram[i * 128:(i + 1) * 128, :], 8, [128, 1024], b_wq)
                S.barrier()
            skT = cx.sb(es1, [128, 8, 128]); b_sk = S.buf("sk")
            S.dma("sp", skT[:], skT_dram.rearrange("h d n -> d h n"), writes=[b_sk])
            nt = NormT(cx, es1, consts)
            top = cx.sb(es1, [128, 8, 16]); work = cx.sb(es1, [128, 128]); cand = cx.sb(es1, [128, 256]); work2 = cx.sb(es1, [128, 256])
            c24 = cx.sb(es1, [128, 24]); mx = cx.sb(es1, [128, 8]); zs = cx.sb(es1, [128, 4]); th4 = cx.sb(es1, [128, 4])
            b_sm = S.buf("small")
            for tt in range(4):
                nt.normT(HM[:, tt, :], bHM[tt], GE[:], SH[:], b_mod, xnT[:, :, tt * 128:(tt + 1) * 128], b_xnT[tt])
            for hp in range(8):
                bank = hp % 4
                for kc in range(8):
                    S.op("pe", lambda e, hp=hp, kc=kc, bank=bank: e.matmul(PSA[:, bank, :], lhsT=wq[:, kc, hp * 128:(hp + 1) * 128], rhs=xnT[:, kc, :],
                                                                           start=(kc == 0), stop=(kc == 7)),
                         reads=[b_wq] + b_xnT, writes=[bPSA[bank]])
                S.op("act" if hp % 2 else "dve", (lambda e, hp=hp, bank=bank: e.copy(out=QA[:, hp // 2, (hp % 2) * 512:(hp % 2 + 1) * 512], in_=PSA[:, bank, :])) if hp % 2 else
                     (lambda e, hp=hp, bank=bank: e.tensor_copy(out=QA[:, hp // 2, (hp % 2) * 512:(hp % 2 + 1) * 512], in_=PSA[:, bank, :])),
                     reads=[bPSA[bank]], writes=[bQA[hp // 2]])
            for tt in range(4):
                pb = 2 * (tt % 2)
                for hp in range(8):
                    S.op("pe", lambda e, hp=hp, tt=tt, pb=pb: e.matmul(PSA[:, pb + hp // 4, (hp % 4) * 128:(hp % 4 + 1) * 128],
                                                                      lhsT=QA[:, hp // 2, (hp % 2) * 512 + tt * 128:(hp % 2) * 512 + (tt + 1) * 128],
                                                                      rhs=skT[:, hp, :], start=True, stop=True),
                         reads=[bQA[hp // 2], b_sk], writes=[bPSA[pb + hp // 4]])
                scv = PSA[:, pb:pb + 2, :].rearrange("p a (h n) -> p (a h) n", n=128)
                S.op("dve", lambda e, scv=scv: e.reduce_max(out=mx[:], in_=scv, axis=AX.X), reads=[bPSA[pb], bPSA[pb + 1]], writes=[b_sm])
                S.op("dve", lambda e: e.tensor_scalar(out=mx[:], in0=mx[:], scalar1=-1.0, scalar2=None, op0=ALU.mult), reads=[b_sm], writes=[b_sm])
                for hp in range(8):
                    S.op("act", lambda e, hp=hp, tt=tt, pb=pb: e.activation(out=Pexp[:, tt, hp, :], in_=PSA[:, pb + hp // 4, (hp % 4) * 128:(hp % 4 + 1) * 128],
                                                                           func=AF.Exp, bias=mx[:, hp:hp + 1], scale=1.0),
                         reads=[bPSA[pb + hp // 4], b_sm], writes=[bP[tt]])
                for hp in range(8):
                    S.op("dve", lambda e, hp=hp, tt=tt: e.max(out=top[:, hp, 0:8], in_=Pexp[:, tt, hp, :]), reads=[bP[tt]], writes=[b_sm])
                    S.op("dve", lambda e, hp=hp, tt=tt: e.match_replace(out=work[:], in_to_replace=top[:, hp, 0:8], in_values=Pexp[:, tt, hp, :], imm_value=-1.0),
                         reads=[bP[tt], b_sm], writes=[b_sm])
                    S.op("dve", lambda e, hp=hp: e.max(out=top[:, hp, 8:16], in_=work[:]), reads=[b_sm], writes=[b_sm])
                for h in range(4):
                    S.op("dve", lambda e, h=h: e.tensor_tensor(out=cand[:].rearrange("p (a b) -> p a b", a=16),
                                                               in0=top[:, 2 * h, :].unsqueeze(2).to_broadcast([128, 16, 16]),
                                                               in1=top[:, 2 * h + 1, :].unsqueeze(1).to_broadcast([128, 16, 16]), op=ALU.mult),
                         reads=[b_sm], writes=[b_sm])
                    S.op("dve", lambda e: e.max(out=c24[:, 0:8], in_=cand[:]), reads=[b_sm], writes=[b_sm])
                    S.op("dve", lambda e: e.match_replace(out=work2[:], in_to_replace=c24[:, 0:8], in_values=cand[:], imm_value=-1.0), reads=[b_sm], writes=[b_sm])
                    S.op("dve", lambda e: e.max(out=c24[:, 8:16], in_=work2[:]), reads=[b_sm], writes=[b_sm])
                    S.op("dve", lambda e: e.match_replace(out=cand[:], in_to_replace=c24[:, 8:16], in_values=work2[:], imm_value=-1.0), reads=[b_sm], writes=[b_sm])
                    S.op("dve", lambda e: e.max(out=c24[:, 16:24], in_=cand[:]), reads=[b_sm], writes=[b_sm])
                    S.op("dve", lambda e, h=h: e.tensor_tensor(out=th4[:, h:h + 1], in0=c24[:, 15:16], in1=c24[:, 16:17], op=ALU.add), reads=[b_sm], writes=[b_sm])
                    S.op("dve", lambda e, h=h: e.reduce_sum(out=zs[:, h:h + 1], in_=c24[:, 0:16], axis=AX.X), reads=[b_sm], writes=[b_sm])
                S.op("dve", lambda e, tt=tt: e.reciprocal(out=RZ[:, tt, :], in_=zs[:]), reads=[b_sm], writes=[bTH[tt]])
                S.op("dve", lambda e, tt=tt: e.scalar_tensor_tensor(out=TH[:, tt, :], in0=th4[:], scalar=0.5, in1=RZ[:, tt, :], op0=ALU.mult, op1=ALU.mult),
                     reads=[b_sm, bTH[tt]], writes=[bTH[tt]])
                for h in range(4):
                    S.op("dve", lambda e, tt=tt, h=h: e.tensor_scalar(out=P1n[:, tt, h, :], in0=Pexp[:, tt, 2 * h, :], scalar1=RZ[:, tt, h:h + 1], scalar2=None, op0=ALU.mult),
                         reads=[bP[tt], bTH[tt]], writes=[bTH[tt]])
            S.barrier()
        with ExitStack() as es3:
            PH = cx.ps(es3, [128, 2, 512]); bPH = S.bufs(2, "PH")
            PW = [cx.ps(es3, [128, 512]) for _ in range(2)]; bPW = S.bufs(2, "PW")
            UG = [cx.sb(es3, [128, 8, GA * 128], BF16) for _ in range(2)]; bUG = S.bufs(2, "UG")
            NV = 3
            VG = [cx.sb(es3, [128, GA, 1024], BF16) for _ in range(NV)]; bVG = S.bufs(NV, "VG")
            NX = 6
            Xn = [cx.sb(es3, [128, GA, 128]) for _ in range(NX)]; bXn = S.bufs(NX, "Xn")
            Wh = [[cx.sb(es3, [128, GA, 128], BF16) for _ in range(16)] for _ in range(2)]; bWh = [S.bufs(16, "Wh%d_" % k) for k in range(2)]
            G = [cx.sb(es3, [128, 512], BF16) for _ in range(2 * GA)]; bG = S.bufs(2 * GA, "G")
            WgT = [cx.sb(es3, [128, GA, 512], BF16) for _ in range(2)]; bWg = S.bufs(2, "WgT")
            uv = uTb.rearrange("(kc p) n -> p kc n", p=128)
            vv = vb.rearrange("(a p) d -> p a d", p=128)
            cnt = {"x": 0, "ph": 0, "pw": 0, "fl": 0}

            def stage1(gi):
                ug, bu = UG[gi % 2], bUG[gi % 2]
                S.dma("sp", ug[:], uv[:, :, gi * GA * 128:(gi + 1) * GA * 128], writes=[bu])
                for ac in range(GA):
                    j = cnt["ph"] % 2
                    cnt["ph"] += 1
                    g, bg = G[(gi % 2) * GA + ac], bG[(gi % 2) * GA + ac]
                    for kc in range(8):
                        S.op("pe", lambda e, ac=ac, kc=kc, j=j, ug=ug: e.matmul(PH[:, j, :], lhsT=ug[:, kc, ac * 128:(ac + 1) * 128], rhs=xnT[:, kc, :],
                                                                              start=(kc == 0), stop=(kc == 7)),
                             reads=[bu] + b_xnT, writes=[bPH[j]])
                    S.op("act", lambda e, j=j, g=g: e.activation(out=g[:], in_=PH[:, j, :], func=AF.Gelu), reads=[bPH[j]], writes=[bg])
                for tt in range(4):
                    for h in range(4):
                        x, bx = Xn[cnt["x"] % NX], bXn[cnt["x"] % NX]
                        cnt["x"] += 1
                        S.op("pool", lambda e, tt=tt, h=h, x=x, gi=gi: e.tensor_tensor(
                            out=x[:], in0=P1n[:, tt, h, gi * GA:(gi + 1) * GA].unsqueeze(2).to_broadcast([128, GA, 128]),
                            in1=Pexp[:, tt, 2 * h + 1, :].unsqueeze(1).to_broadcast([128, GA, 128]), op=ALU.mult),
                            reads=[bP[tt], bTH[tt]], writes=[bx])
                        w, bw = Wh[gi % 2][tt * 4 + h], bWh[gi % 2][tt * 4 + h]
                        S.op("dve", lambda e, tt=tt, h=h, x=x, w=w: e.scalar_tensor_tensor(out=w[:], in0=x[:], scalar=TH[:, tt, h:h + 1], in1=x[:],
                                                                                          op0=ALU.is_ge, op1=ALU.mult),
                             reads=[bx, bTH[tt]], writes=[bw])

            def stage2(gi):
                vg, bv = VG[gi % NV], bVG[gi % NV]
                S.dma("sp", vg[:], vv[:, gi * GA:(gi + 1) * GA, :], writes=[bv])
                wg, bwg = WgT[gi % 2], bWg[gi % 2]
                for ac in range(GA):
                    j = cnt["pw"] % 2
                    cnt["pw"] += 1
                    g, bg = G[(gi % 2) * GA + ac], bG[(gi % 2) * GA + ac]
                    for tt in range(4):
                        for h in range(4):
                            w, bw = Wh[gi % 2][tt * 4 + h], bWh[gi % 2][tt * 4 + h]
                            S.op("pe", lambda e, ac=ac, tt=tt, j=j, w=w, h=h: e.matmul(PW[j][:, tt * 128:(tt + 1) * 128], lhsT=w[:, ac, :], rhs=consts["identb"][:],
                                                                                     start=(h == 0), stop=(h == 3)),
                                 reads=[bw, consts["b_ident"]], writes=[bPW[j]])
                    S.op("dve", lambda e, ac=ac, j=j, wg=wg, g=g: e.tensor_tensor(out=wg[:, ac, :], in0=g[:], in1=PW[j][:], op=ALU.mult),
                         reads=[bg, bPW[j]], writes=[bwg])

            def stage3(gi):
                vg, bv = VG[gi % NV], bVG[gi % NV]
                wg, bwg = WgT[gi % 2], bWg[gi % 2]
                for tt in range(4):
                    pb = 2 * (tt % 2)
                    for ac in range(GA):
                        for half in range(2):
                            S.op("pe", lambda e, ac=ac, tt=tt, half=half, pb=pb, wg=wg, vg=vg: e.matmul(
                                PSA[:, pb + half, :], lhsT=wg[:, ac, tt * 128:(tt + 1) * 128], rhs=vg[:, ac, half * 512:(half + 1) * 512],
                                start=(ac == 0), stop=(ac == GA - 1)),
                                reads=[bwg, bv], writes=[bPSA[pb + half]])
                    src = PSA[:, pb:pb + 2, :].rearrange("p a b -> p (a b)")
                    if gi == 0:
                        S.op("act", lambda e, tt=tt, src=src: e.copy(out=QA[:, tt, :], in_=src), reads=[bPSA[pb], bPSA[pb + 1]], writes=[bQA[tt]])
                    else:
                        S.op("dve", lambda e, tt=tt, src=src: e.tensor_tensor(out=QA[:, tt, :], in0=QA[:, tt, :], in1=src, op=ALU.add),
                             reads=[bPSA[pb], bPSA[pb + 1], bQA[tt]], writes=[bQA[tt]])

            for k in range(NG + 2):
                if k < NG:
                    stage1(k)
                if 0 <= k - 1 < NG:
                    stage2(k - 1)
                if 0 <= k - 2 < NG:
                    stage3(k - 2)
            for tt in range(4):
                S.op("pool", lambda e, tt=tt: e.tensor_tensor(out=QA[:, tt, :], in0=QA[:, tt, :], in1=G2[:], op=ALU.mult), reads=[bQA[tt], b_g2], writes=[bQA[tt]])
                S.op("dve", lambda e, tt=tt: e.tensor_tensor(out=HM[:, tt, :], in0=HM[:, tt, :], in1=QA[:, tt, :], op=ALU.add), reads=[bQA[tt], bHM[tt]], writes=[bHM[tt]])
            S.barrier()


def hgrn_consts_host():
    t = np.arange(128)
    ch = t // 16
    same = ch[:, None] == ch[None, :]
    BT = (same & (t[:, None] <= t[None, :])).astype(np.float32)
    RT = (same & (t[:, None] > t[None, :])).astype(np.float32)
    CI = (ch[:, None] == np.arange(8)[None, :]).astype(np.float32)
    hcA = np.concatenate([BT, RT, CI], axis=1)
    hcB = np.ascontiguousarray(CI.T).reshape(1, 1024)
    return hcA, hcB


def emit_hgrn(cx, consts, HM, bHM, mode, x_dram, ntiles, snap, mod_dram, c_sh, c_ge, c_g,
              win_dram, wout_dram, ongT_dram, lbl_dram, hcA_dram, hcB_dram, onehot_dram=None, m=0):
    S = cx.S
    full = (mode == "full")
    with ExitStack() as es:
        hcA = cx.sb(es, [128, 264]); CHM = cx.sb(es, [128, 8, 128]); b_hc = S.buf("hc")
        S.dma("sp", hcA[:], hcA_dram, writes=[b_hc])
        S.dma("pool", CHM[:].rearrange("p a b -> p (a b)"), hcB_dram[0:1, :].partition_broadcast(128), writes=[b_hc])
        BT, RT, CI = hcA[:, 0:128], hcA[:, 128:256], hcA[:, 256:264]
        LB = cx.sb(es, [128, 1024]); OML = cx.sb(es, [128, 1024]); GE = cx.sb(es, [128, 1024]); SH = cx.sb(es, [128, 1024])
        b_mod = S.buf("hmod")
        S.dma("sp", SH[:], mod_dram[:, c_sh:c_sh + 1024], writes=[b_mod])
        S.dma("sp", GE[:], mod_dram[:, c_ge:c_ge + 1024], writes=[b_mod])
        S.dma("pool", LB[:], lbl_dram[0:1, :].partition_broadcast(128), writes=[b_mod])
        S.dma("pool", OML[:], lbl_dram[1:2, :].partition_broadcast(128), writes=[b_mod])
        S.op("dve", lambda e: e.tensor_tensor(out=LB[:], in0=LB[:], in1=OML[:], op=ALU.subtract), reads=[b_mod], writes=[b_mod])
        S.op("act", lambda e: e.activation(out=LB[:], in_=LB[:], func=AF.Sigmoid), reads=[b_mod], writes=[b_mod])
        S.op("dve", lambda e: e.tensor_scalar(out=OML[:], in0=LB[:], scalar1=-1.0, scalar2=1.0, op0=ALU.mult, op1=ALU.add), reads=[b_mod], writes=[b_mod])
        win = cx.sb(es, [128, 8, 4096], BF16); b_win = S.buf("win")
        wout = cx.sb(es, [128, 8, 1024], BF16); b_wout = S.buf("wout")
        with ExitStack() as es2:
            load_cast(cx, es2, "act", lambda i: win[:, i // 4, (i % 4) * 1024:(i % 4 + 1) * 1024],
                      lambda i: win_dram[(i // 4) * 128:(i // 4 + 1) * 128, (i % 4) * 1024:(i % 4 + 1) * 1024], 32, [128, 1024], b_win)
            if full:
                G1 = cx.sb(es2, [128, 1024]); ongT = cx.sb(es2, [128, 8]); stg = [cx.sb(es2, [128, 1024]) for _ in range(2)]
                bg1, bst = S.buf(), S.bufs(2)
                S.dma("sp", G1[:], mod_dram[:, c_g:c_g + 1024], writes=[bg1])
                S.dma("sp", ongT[:], ongT_dram, writes=[bg1])
                for kc in range(8):
                    s, b = stg[kc % 2], bst[kc % 2]
                    S.dma("sp", s[:], wout_dram[kc * 128:(kc + 1) * 128, :], writes=[b])
                    S.op("dve", lambda e, s=s, kc=kc: e.tensor_scalar(out=s[:], in0=s[:], scalar1=ongT[:, kc:kc + 1], scalar2=None, op0=ALU.mult), reads=[b, bg1], writes=[b])
                    S.op("dve", lambda e, s=s, kc=kc: e.tensor_tensor(out=wout[:, kc, :], in0=s[:], in1=G1[:], op=ALU.mult), reads=[b, bg1], writes=[b_wout])
            S.barrier()
        nt = NormT(cx, es, consts)
        xt = [cx.sb(es, [128, 1024]) for _ in range(2)]; b_xt = S.bufs(2, "xt")
        hnT = cx.sb(es, [128, 8, 128], BF16); b_hnT = S.buf("hnT")
        R = [cx.sb(es, [128, 1024]) for _ in range(6)]; bR = S.bufs(6, "R")
        OGb = cx.sb(es, [128, 1024], BF16); b_og = S.buf("og")
        ogT = cx.sb(es, [128, 8, 128], BF16); b_ogT = S.buf("ogT")
        QKT = [cx.sb(es, [128, 2, 128]) for _ in range(2)]; bQKT = S.bufs(2, "QKT")
        SCM = [cx.sb(es, [128, 128]) for _ in range(2)]; bSCM = S.bufs(2, "SCM")
        QDM = cx.sb(es, [128, 8, 128]); bQDM = S.buf("QDM")
        KLM = cx.sb(es, [128, 8, 128]); bKLM = S.buf("KLM")
        ST = cx.sb(es, [128, 8, 2, 128]); bST = [S.bufs(2, "ST%d_" % h) for h in range(8)]
        EDEC = cx.sb(es, [128, 64]); bEDEC = S.buf("EDEC")
        ss8 = cx.sb(es, [128, 16]); b_ss8 = S.buf("ss8")
        PJ = [cx.ps(es, [128, 512]) for _ in range(3)]; bPJ = S.bufs(3, "PJ")
        PKV = [cx.ps(es, [128, 512]) for _ in range(2)]; bPKV = S.bufs(2, "PKV")
        PO = cx.ps(es, [128, 512]); bPO = S.buf("PO")
        PC = cx.ps(es, [128, 512]); bPC = S.buf("PC")
        pj_i = [0]

        def nextpj():
            k = pj_i[0] % 3
            pj_i[0] += 1
            return PJ[k], bPJ[k]

        def proj(j, nb):
            pj, b = nextpj()
            for kc in range(8):
                S.op("pe", lambda e, pj=pj, kc=kc: e.matmul(pj[:], lhsT=hnT[:, kc, :],
                                                            rhs=win[:, kc, j * 1024 + nb * 512:j * 1024 + (nb + 1) * 512],
                                                            start=(kc == 0), stop=(kc == 7)),
                     reads=[b_hnT, b_win], writes=[b])
            return pj, b

        if full:
            oh = cx.sb(es, [128, 4]); b_oh = S.buf("oh")
            S.dma("pool", oh[:], onehot_dram[0:1, :].partition_broadcast(128), writes=[b_oh])
            stv = ST[:, :, 0, :]
            allst = [bST[h][0] for h in range(8)]
            for j in range(4):
                S.dma("sp", R[5][:], snap[4 * m + j], writes=[bR[5]])
                r5 = R[5][:].rearrange("p (h v) -> p h v", h=8)
                if j == 0:
                    S.op("dve", lambda e, j=j, r5=r5: e.tensor_scalar(out=stv, in0=r5, scalar1=oh[:, j:j + 1], scalar2=None, op0=ALU.mult),
                         reads=[bR[5], b_oh], writes=allst)
                else:
                    S.op("dve", lambda e, j=j, r5=r5: e.scalar_tensor_tensor(out=stv, in0=r5, scalar=oh[:, j:j + 1], in1=stv, op0=ALU.mult, op1=ALU.add),
                         reads=[bR[5], b_oh] + allst, writes=allst)
        else:
            S.op("pool", lambda e: e.memset(ST[:].rearrange("p a b c -> p (a b c)"), 0.0), writes=[bST[h][s] for h in range(8) for s in range(2)])

        for ti in range(ntiles):
            x_t, bx = xt[ti % 2], b_xt[ti % 2]
            if (not full) and ti % 4 == 0:
                S.dma("pool", snap[ti // 4].rearrange("p (h v) -> p h v", h=8), ST[:, :, 0, :], reads=[bST[h][0] for h in range(8)])
            row0 = (m * 4 + ti) * 128 if full else ti * 128
            S.dma("sp", x_t[:], x_dram[row0:row0 + 128, :], writes=[bx])
            nt.normT(x_t[:], bx, GE[:], SH[:], b_mod, hnT[:], b_hnT)
            H2 = [slice(0, 512), slice(512, 1024)]
            for nb in range(2):
                pf, bpf = proj(1, nb)
                S.op("act", lambda e, pf=pf, nb=nb: e.activation(out=R[0][:, H2[nb]], in_=pf[:], func=AF.Sigmoid), reads=[bpf], writes=[bR[0]])
            S.op("dve", lambda e: e.tensor_tensor(out=R[0][:], in0=R[0][:], in1=OML[:], op=ALU.mult), reads=[bR[0], b_mod], writes=[bR[0]])
            S.op("pool", lambda e: e.tensor_tensor(out=R[0][:], in0=R[0][:], in1=LB[:], op=ALU.add), reads=[bR[0], b_mod], writes=[bR[0]])
            S.op("act", lambda e: e.activation(out=R[1][:], in_=R[0][:], func=AF.Ln), reads=[bR[0]], writes=[bR[1]])
            S.op("pool", lambda e: e.tensor_scalar(out=R[2][:], in0=R[0][:], scalar1=-1.0, scalar2=1.0, op0=ALU.mult, op1=ALU.add), reads=[bR[0]], writes=[bR[2]])
            for nb in range(2):
                pc, bpc = nextpj()
                S.op("pe", lambda e, pc=pc, nb=nb: e.matmul(pc[:], lhsT=BT, rhs=R[1][:, H2[nb]], start=True, stop=True),
                     reads=[b_hc, bR[1]], writes=[bpc])
                if full:
                    S.op("act", lambda e, pc=pc, nb=nb: e.activation(out=R[0][:, H2[nb]], in_=pc[:], func=AF.Exp), reads=[bpc], writes=[bR[0]])
                    S.op("act", lambda e, pc=pc, nb=nb: e.activation(out=R[3][:, H2[nb]], in_=pc[:], func=AF.Exp, scale=-1.0), reads=[bpc], writes=[bR[3]])
            for nb in range(2):
                pr, bpr = nextpj()
                S.op("pe", lambda e, pr=pr, nb=nb: e.matmul(pr[:], lhsT=RT, rhs=R[1][:, H2[nb]], start=True, stop=True),
                     reads=[b_hc, bR[1]], writes=[bpr])
                S.op("act", lambda e, pr=pr, nb=nb: e.activation(out=R[4][:, H2[nb]], in_=pr[:], func=AF.Exp), reads=[bpr], writes=[bR[4]])
            for h in range(8):
                S.op("pe", lambda e, h=h: e.matmul(PC[:, 384 + h * 8:384 + (h + 1) * 8], lhsT=R[1][:, h * 128:(h + 1) * 128], rhs=CI, start=True, stop=True),
                     reads=[b_hc, bR[1]], writes=[bPC])
            S.op("act", lambda e: e.activation(out=EDEC[:], in_=PC[:, 384:448], func=AF.Exp), reads=[bPC], writes=[bEDEC])
            S.op("dve", lambda e: e.tensor_tensor(out=R[4][:], in0=R[4][:], in1=R[2][:], op=ALU.mult), reads=[bR[4], bR[2]], writes=[bR[4]])
            if full:
                S.op("dve", lambda e: e.tensor_tensor(out=R[3][:], in0=R[3][:], in1=R[2][:], op=ALU.mult), reads=[bR[3], bR[2]], writes=[bR[3]])
            for nb in range(2):
                pi, bpi = proj(2, nb)
                S.op("act", lambda e, pi=pi, nb=nb: e.copy(out=R[1][:, H2[nb]], in_=pi[:]), reads=[bpi], writes=[bR[1]])
            if full:
                for nb in range(2):
                    pq, bpq = proj(0, nb)
                    S.op("act", lambda e, pq=pq, nb=nb: e.activation(out=R[2][:, H2[nb]], in_=pq[:], func=AF.Silu), reads=[bpq], writes=[bR[2]])
                S.op("dve", lambda e: e.tensor_tensor(out=R[0][:], in0=R[0][:], in1=R[2][:], op=ALU.mult), reads=[bR[0], bR[2]], writes=[bR[0]])
                for nb in range(2):
                    pg, bpg = proj(3, nb)
                    S.op("act", lambda e, pg=pg, nb=nb: e.activation(out=R[2][:, H2[nb]], in_=pg[:], func=AF.Silu), reads=[bpg], writes=[bR[2]])
            for h in range(8):
                j = h % 2
                hs = slice(h * 128, (h + 1) * 128)
                if full:
                    S.op("pe", lambda e, hs=hs: e.transpose(PC[:, 0:128], R[0][:, hs], consts["identf"][:]), reads=[bR[0], consts["b_ident"]], writes=[bPC])
                    S.op("pe", lambda e, hs=hs: e.transpose(PC[:, 128:256], R[3][:, hs], consts["identf"][:]), reads=[bR[3], consts["b_ident"]], writes=[bPC])
                    S.op("act", lambda e, j=j: e.copy(out=QKT[j][:].rearrange("p a b -> p (a b)"), in_=PC[:, 0:256]), reads=[bPC], writes=[bQKT[j]])
                    S.op("pe", lambda e, j=j: e.matmul(PC[:, 256:384], lhsT=QKT[j][:, 1, :], rhs=QKT[j][:, 0, :], start=True, stop=True), reads=[bQKT[j]], writes=[bPC])
                    S.op("dve", lambda e, j=j: e.tensor_tensor(out=SCM[j][:], in0=PC[:, 256:384], in1=BT, op=ALU.mult), reads=[bPC, b_hc], writes=[bSCM[j]])
                    S.op("pool", lambda e, j=j: e.tensor_tensor(out=QDM[:], in0=QKT[j][:, 0, :].unsqueeze(1).to_broadcast([128, 8, 128]), in1=CHM[:], op=ALU.mult),
                         reads=[bQKT[j], b_hc], writes=[bQDM])
                S.op("pool", lambda e, hs=hs: e.tensor_tensor(out=KLM[:], in0=R[4][:, hs].unsqueeze(1).to_broadcast([128, 8, 128]),
                                                              in1=CI.unsqueeze(2).to_broadcast([128, 8, 128]), op=ALU.mult),
                     reads=[bR[4], b_hc], writes=[bKLM])
                if full:
                    S.op("pe", lambda e, j=j, hs=hs: e.matmul(PO[:, 0:128], lhsT=SCM[j][:], rhs=R[1][:, hs], start=True, stop=False),
                         reads=[bSCM[j], bR[1]], writes=[bPO])
                for c in range(8):
                    k = c % 2
                    s_old, s_new = c % 2, (c + 1) % 2
                    S.op("pe", lambda e, c=c, k=k, hs=hs: e.matmul(PKV[k][:, 0:128], lhsT=KLM[:, c, :], rhs=R[1][:, hs], start=True, stop=True),
                         reads=[bKLM, bR[1]], writes=[bPKV[k]])
                    if full:
                        S.op("pe", lambda e, c=c, h=h, s_old=s_old: e.matmul(PO[:, 0:128], lhsT=QDM[:, c, :], rhs=ST[:, h, s_old, :],
                                                                            start=False, stop=(c == 7)),
                             reads=[bQDM, bST[h][s_old]], writes=[bPO])
                    S.op("dve", lambda e, c=c, k=k, h=h, s_old=s_old, s_new=s_new: e.scalar_tensor_tensor(
                        out=ST[:, h, s_new, :], in0=ST[:, h, s_old, :], scalar=EDEC[:, h * 8 + c:h * 8 + c + 1], in1=PKV[k][:, 0:128], op0=ALU.mult, op1=ALU.add),
                        reads=[bST[h][s_old], bEDEC, bPKV[k]], writes=[bST[h][s_new]])
                if full:
                    S.op("act", lambda e, hs=hs: e.copy(out=R[5][:, hs], in_=PO[:, 0:128]), reads=[bPO], writes=[bR[5]])
            if full:
                S.op("dve", lambda e: e.tensor_tensor(out=R[3][:], in0=R[5][:], in1=R[5][:], op=ALU.mult), reads=[bR[5]], writes=[bR[3]])
                S.op("dve", lambda e: e.reduce_sum(out=ss8[:, 0:8], in_=R[3][:].rearrange("p (h v) -> p h v", h=8), axis=AX.X), reads=[bR[3]], writes=[b_ss8])
                emit_rstd(cx, ss8[:, 0:8], ss8[:, 8:16], 1.0 / 128, b_ss8, b_ss8)
                S.op("dve", lambda e: e.tensor_tensor(out=R[5][:].rearrange("p (h v) -> p h v", h=8), in0=R[5][:].rearrange("p (h v) -> p h v", h=8),
                                                      in1=ss8[:, 8:16].unsqueeze(2).to_broadcast([128, 8, 128]), op=ALU.mult),
                     reads=[bR[5], b_ss8], writes=[bR[5]])
                S.op("pool", lambda e: e.tensor_tensor(out=OGb[:], in0=R[5][:], in1=R[2][:], op=ALU.mult), reads=[bR[5], bR[2]], writes=[b_og])
                nt.transpose(OGb, b_og, ogT[:], b_ogT)
                for nb in range(2):
                    pm, bpm = nextpj()
                    for kc in range(8):
                        S.op("pe", lambda e, pm=pm, nb=nb, kc=kc: e.matmul(pm[:], lhsT=ogT[:, kc, :], rhs=wout[:, kc, nb * 512:(nb + 1) * 512],
                                                                          start=(kc == 0), stop=(kc == 7)),
                             reads=[b_ogT, b_wout], writes=[bpm])
                    S.op("dve", lambda e, pm=pm, x_t=x_t, ti=ti, nb=nb: e.tensor_tensor(out=HM[:, ti, H2[nb]], in0=pm[:], in1=x_t[:, H2[nb]], op=ALU.add),
                         reads=[bpm, bx], writes=[bHM[ti]])
        if (not full) and ntiles % 4 == 0:
            S.dma("pool", snap[ntiles // 4].rearrange("p (h v) -> p h v", h=8), ST[:, :, 0, :], reads=[bST[h][0] for h in range(8)])
        S.barrier()


def attn_consts_host(r):
    j = np.arange(128)
    trin = -(j[:, None] >= j[None, :]).astype(np.float32)
    onesn = -np.ones((128, 128), np.float32)
    ac = np.concatenate([trin, onesn], axis=1).astype(ml_dtypes.bfloat16)
    k = np.arange(16)
    kp = k[None, :, None] * 128 + j[:, None, None]
    qp = 4 * r * 128 + np.arange(512)[None, None, :]
    mask = (kp < qp).astype(np.float32).astype(ml_dtypes.bfloat16)
    return ac, mask


def emit_attn(cx, consts, HM, bHM, m, mod_dram, c_sh, c_ge, c_g, wq_dram, wo_dram, KT_dram, V_dram, ac_dram, mask_dram):
    S = cx.S
    NB = 16 * (m + 1)
    with ExitStack() as es:
        SH = cx.sb(es, [128, 1024]); GE = cx.sb(es, [128, 1024]); b_mod = S.buf("amod")
        S.dma("sp", SH[:], mod_dram[:, c_sh:c_sh + 1024], writes=[b_mod])
        S.dma("sp", GE[:], mod_dram[:, c_ge:c_ge + 1024], writes=[b_mod])
        AC = cx.sb(es, [128, 256], BF16); MASK = cx.sb(es, [128, 16, 512], BF16); b_ac = S.buf("ac")
        S.dma("sp", AC[:], ac_dram, writes=[b_ac])
        S.dma("sp", MASK[:], mask_dram, writes=[b_ac])
        TRIN, ONESN = AC[:, 0:128], AC[:, 128:256]
        wq = cx.sb(es, [128, 8, 1024], BF16); b_wq = S.buf("awq")
        wo = cx.sb(es, [128, 8, 1024], BF16); b_wo = S.buf("awo")
        with ExitStack() as es2:
            load_cast(cx, es2, "act", lambda i: wq[:, i, :], lambda i: wq_dram[i * 128:(i + 1) * 128, :], 8, [128, 1024], b_wq)
            G1 = cx.sb(es2, [128, 1024]); stg = [cx.sb(es2, [128, 1024]) for _ in range(2)]
            bg1, bst = S.buf(), S.bufs(2)
            S.dma("sp", G1[:], mod_dram[:, c_g:c_g + 1024], writes=[bg1])
            for kc in range(8):
                s, b = stg[kc % 2], bst[kc % 2]
                S.dma("sp", s[:], wo_dram[kc * 128:(kc + 1) * 128, :], writes=[b])
                S.op("dve", lambda e, s=s, kc=kc: e.tensor_tensor(out=wo[:, kc, :], in0=s[:], in1=G1[:], op=ALU.mult), reads=[b, bg1], writes=[b_wo])
            S.barrier()
        nt = NormT(cx, es, consts)
        xnT = cx.sb(es, [128, 8, 512], BF16); b_xnT = S.bufs(4, "axnT")
        QT = cx.sb(es, [128, 8, 512], BF16); bQT = S.bufs(8, "QT")
        NKV = 3
        KTc = [cx.sb(es, [128, 2048], BF16) for _ in range(NKV)]; bKT = S.bufs(NKV, "KTc")
        Vc = [cx.sb(es, [128, 16, 128], BF16) for _ in range(NKV)]; bV = S.bufs(NKV, "Vc")
        NR = 3
        E = [cx.sb(es, [128, 512]) for _ in range(NR)]; bE = S.bufs(NR, "E")
        LK = [cx.sb(es, [128, 512], BF16) for _ in range(NR)]; bLK = S.bufs(NR, "LK")
        LKS = [cx.sb(es, [128, 512], BF16) for _ in range(NR)]; bLKS = S.bufs(NR, "LKS")
        A = [cx.sb(es, [128, 512], BF16) for _ in range(NR)]; bA = S.bufs(NR, "A")
        OT = cx.sb(es, [128, 8, 512], BF16); bOT = S.bufs(8, "OT")
        PZ = [cx.ps(es, [128, 512]) for _ in range(3)]; bPZ = S.bufs(3, "PZ")
        PS = [cx.ps(es, [128, 512]) for _ in range(2)]; bPS = S.bufs(2, "PS")
        POUT = [cx.ps(es, [128, 512]) for _ in range(2)]; bPOUT = S.bufs(2, "POUT")
        for tt in range(4):
            nt.normT(HM[:, tt, :], bHM[tt], GE[:], SH[:], b_mod, xnT[:, :, tt * 128:(tt + 1) * 128], b_xnT[tt])
        sc = 1.0 / math.sqrt(128.0)
        for h in range(8):
            pz, bpz = (PZ[h % 3], bPZ[h % 3])
            for kc in range(8):
                S.op("pe", lambda e, h=h, kc=kc, pz=pz: e.matmul(pz[:], lhsT=wq[:, kc, h * 128:(h + 1) * 128], rhs=xnT[:, kc, :], start=(kc == 0), stop=(kc == 7)),
                     reads=[b_wq] + b_xnT, writes=[bpz])
            S.op("act", lambda e, h=h, pz=pz: e.activation(out=QT[:, h, :], in_=pz[:], func=AF.Copy, scale=sc), reads=[bpz], writes=[bQT[h]])
        items = []
        ld = 0
        for h in range(8):
            for ci in range(m, -1, -1):
                slot = ld % NKV
                ld += 1
                for kk in range(15, -1, -1):
                    items.append(dict(h=h, ci=ci, kk=kk, slot=slot, load=(kk == 15), first=(ci == m and kk == 15), last=(ci == 0 and kk == 0),
                                      masked=(ci == m)))
        for i, it in enumerate(items):
            it["i"] = i

        def stageA(it):
            i, h, kk, slot = it["i"], it["h"], it["kk"], it["slot"]
            kt, bkt, vc, bvc = KTc[slot], bKT[slot], Vc[slot], bV[slot]
            if it["load"]:
                S.dma("sp", kt[:], KT_dram[h, :, it["ci"] * 2048:(it["ci"] + 1) * 2048], writes=[bkt])
                S.dma("pool", vc[:], V_dram[h, :, it["ci"] * 16:(it["ci"] + 1) * 16, :], writes=[bvc])
            z, r = i % 3, i % NR
            ks = slice(kk * 128, (kk + 1) * 128)
            S.op("pe", lambda e: e.matmul(PZ[z][:], lhsT=kt[:, ks], rhs=QT[:, h, :], start=True, stop=True), reads=[bkt, bQT[h]], writes=[bPZ[z]])
            S.op("act", lambda e: e.activation(out=E[r][:], in_=PZ[z][:], func=AF.Exp), reads=[bPZ[z]], writes=[bE[r]])
            S.op("act", lambda e: e.activation(out=LK[r][:], in_=E[r][:], func=AF.Ln, bias=1.0, scale=1.0), reads=[bE[r]], writes=[bLK[r]])
            if it["masked"]:
                S.op("dve", lambda e: e.tensor_tensor(out=LK[r][:], in0=LK[r][:], in1=MASK[:, kk, :], op=ALU.mult), reads=[bLK[r], b_ac], writes=[bLK[r]])

        def stageB(it):
            i, h, kk, slot = it["i"], it["h"], it["kk"], it["slot"]
            kt, bkt = KTc[slot], bKT[slot]
            r, p = i % NR, i % 2
            nx = (i + 1) % NR
            first, last = it["first"], it["last"]
            ks = slice(kk * 128, (kk + 1) * 128)
            S.op("pe", lambda e: e.matmul(PS[p][:], lhsT=kt[:, ks], rhs=QT[:, h, :], start=True, stop=False), reads=[bkt, bQT[h]], writes=[bPS[p]])
            S.op("pe", lambda e: e.matmul(PS[p][:], lhsT=TRIN, rhs=LK[r][:], start=False, stop=first), reads=[b_ac, bLK[r]], writes=[bPS[p]])
            if not first:
                S.op("pe", lambda e: e.matmul(PS[p][:], lhsT=ONESN, rhs=LKS[r][:], start=False, stop=True), reads=[b_ac, bLKS[r]], writes=[bPS[p]])
            S.op("act", lambda e: e.activation(out=A[r][:], in_=PS[p][:], func=AF.Exp), reads=[bPS[p]], writes=[bA[r]])
            if it["masked"]:
                S.op("pool", lambda e: e.tensor_tensor(out=A[r][:], in0=A[r][:], in1=MASK[:, kk, :], op=ALU.mult), reads=[bA[r], b_ac], writes=[bA[r]])
            if first:
                S.op("dve", lambda e: e.tensor_copy(out=LKS[nx][:], in_=LK[r][:]), reads=[bLK[r]], writes=[bLKS[nx]])
            elif not last:
                S.op("dve", lambda e: e.tensor_tensor(out=LKS[nx][:], in0=LKS[r][:], in1=LK[r][:], op=ALU.add), reads=[bLK[r], bLKS[r]], writes=[bLKS[nx]])

        def stageC(it):
            i, h, kk, slot = it["i"], it["h"], it["kk"], it["slot"]
            vc, bvc = Vc[slot], bV[slot]
            r = i % NR
            po, bpo = POUT[h % 2], bPOUT[h % 2]
            S.op("pe", lambda e: e.matmul(po[:], lhsT=vc[:, kk, :], rhs=A[r][:], start=it["first"], stop=it["last"]), reads=[bvc, bA[r]], writes=[bpo])
            if it["last"]:
                S.op("dve", lambda e: e.tensor_copy(out=OT[:, h, :], in_=po[:]), reads=[bpo], writes=[bOT[h]])

        n = len(items)
        for k in range(n + 2):
            if k < n:
                stageA(items[k])
            if 0 <= k - 1 < n:
                stageB(items[k - 1])
            if 0 <= k - 2 < n:
                stageC(items[k - 2])
        for tt in range(4):
            for nb in range(2):
                S_ps, b_ps = PZ[nb], bPZ[nb]
                for h in range(8):
                    S.op("pe", lambda e, tt=tt, nb=nb, h=h, S_ps=S_ps: e.matmul(S_ps[:], lhsT=OT[:, h, tt * 128:(tt + 1) * 128], rhs=wo[:, h, nb * 512:(nb + 1) * 512],
                                                                             start=(h == 0), stop=(h == 7)),
                         reads=[bOT[h], b_wo], writes=[b_ps])
                S.op("dve", lambda e, tt=tt, nb=nb, S_ps=S_ps: e.tensor_tensor(out=HM[:, tt, nb * 512:(nb + 1) * 512], in0=HM[:, tt, nb * 512:(nb + 1) * 512], in1=S_ps[:], op=ALU.add),
                     reads=[b_ps, bHM[tt]], writes=[bHM[tt]])
        S.barrier()


def emit_kv(cx, consts, HM, bHM, m, mod_dram, c_sh, c_ge, kvw_dram, KTo, Vo):
    S = cx.S
    with ExitStack() as es:
        SH = cx.sb(es, [128, 1024]); GE = cx.sb(es, [128, 1024]); b_mod = S.buf("kmod")
        S.dma("sp", SH[:], mod_dram[:, c_sh:c_sh + 1024], writes=[b_mod])
        S.dma("sp", GE[:], mod_dram[:, c_ge:c_ge + 1024], writes=[b_mod])
        kvw = cx.sb(es, [128, 8, 2048], BF16); b_kvw = S.buf("kvw")
        with ExitStack() as es2:
            load_cast(cx, es2, "act", lambda i: kvw[:, i // 2, (i % 2) * 1024:(i % 2 + 1) * 1024],
                      lambda i: kvw_dram[(i // 2) * 128:(i // 2 + 1) * 128, (i % 2) * 1024:(i % 2 + 1) * 1024], 16, [128, 1024], b_kvw)
            S.barrier()
        nt = NormT(cx, es, consts)
        xnT = cx.sb(es, [128, 8, 512], BF16); b_xnT = S.bufs(4, "kxnT")
        KTs = cx.sb(es, [128, 8, 512], BF16); bKTs = S.buf("KTs")
        Vs = [cx.sb(es, [128, 1024], BF16) for _ in range(2)]; bVs = S.bufs(2, "Vs")
        PK = [cx.ps(es, [128, 512]) for _ in range(4)]; bPK = S.bufs(4, "PK")
        for tt in range(4):
            nt.normT(HM[:, tt, :], bHM[tt], GE[:], SH[:], b_mod, xnT[:, :, tt * 128:(tt + 1) * 128], b_xnT[tt])
        for h in range(8):
            pk, bpk = PK[h % 4], bPK[h % 4]
            for kc in range(8):
                S.op("pe", lambda e, h=h, kc=kc, pk=pk: e.matmul(pk[:], lhsT=kvw[:, kc, h * 128:(h + 1) * 128], rhs=xnT[:, kc, :], start=(kc == 0), stop=(kc == 7)),
                     reads=[b_kvw] + b_xnT, writes=[bpk])
            S.op("act", lambda e, h=h, pk=pk: e.copy(out=KTs[:, h, :], in_=pk[:]), reads=[bpk], writes=[bKTs])
        S.dma("sp", KTo[m].rearrange("h d t -> d h t"), KTs[:], reads=[bKTs])
        for tt in range(4):
            vs, bvs = Vs[tt % 2], bVs[tt % 2]
            for nb in range(2):
                pk, bpk = PK[(tt * 2 + nb) % 4], bPK[(tt * 2 + nb) % 4]
                for kc in range(8):
                    S.op("pe", lambda e, tt=tt, nb=nb, kc=kc, pk=pk: e.matmul(pk[:], lhsT=xnT[:, kc, tt * 128:(tt + 1) * 128],
                                                                             rhs=kvw[:, kc, 1024 + nb * 512:1024 + (nb + 1) * 512], start=(kc == 0), stop=(kc == 7)),
                         reads=[b_kvw, b_xnT[tt]], writes=[bpk])
                S.op("dve", lambda e, nb=nb, pk=pk, vs=vs: e.tensor_copy(out=vs[:, nb * 512:(nb + 1) * 512], in_=pk[:]), reads=[bpk], writes=[bvs])
            S.dma("sp", Vo[m * 512 + tt * 128:m * 512 + (tt + 1) * 128, :], vs[:], reads=[bvs])
        S.barrier()


def emit_store(cx, HM, bHM, m, out):
    S = cx.S
    for tt in range(4):
        S.dma("sp", out[m * 512 + tt * 128:m * 512 + (tt + 1) * 128, :], HM[:, tt, :], reads=[bHM[tt]])


def emit_final(cx, consts, HM, bHM, m, g_dram, out):
    S = cx.S
    with ExitStack() as es:
        G = cx.sb(es, [128, 1024]); bg = S.buf("fg")
        S.dma("pool", G[:], g_dram[0:1, :].partition_broadcast(128), writes=[bg])
        junk = cx.sb(es, [128, 1024], BF16); st = cx.sb(es, [128, 8]); bj, bst = S.buf(), S.buf()
        o = [cx.sb(es, [128, 1024]) for _ in range(2)]; bo = S.bufs(2, "fo")
        for tt in range(4):
            S.op("act", lambda e, tt=tt: e.activation(out=junk[:], in_=HM[:, tt, :], func=AF.Square, accum_out=st[:, 2 * tt:2 * tt + 1]),
                 reads=[bHM[tt]], writes=[bj, bst])
            emit_rstd(cx, st[:, 2 * tt:2 * tt + 1], st[:, 2 * tt + 1:2 * tt + 2], 1.0 / D, bst, bst)
            S.op("dve", lambda e, tt=tt: e.scalar_tensor_tensor(out=o[tt % 2][:], in0=HM[:, tt, :], scalar=st[:, 2 * tt + 1:2 * tt + 2], in1=G[:], op0=ALU.mult, op1=ALU.mult),
                 reads=[bHM[tt], bst, bg], writes=[bo[tt % 2]])
            S.dma("sp", out[m * 512 + tt * 128:m * 512 + (tt + 1) * 128, :], o[tt % 2][:], reads=[bo[tt % 2]])
        S.barrier()


def build_l1(NM, do_peer=True):
    SQ = 2048 * NM
    NQG = 4 * NM
    nc = bass.Bass("TRN2", target_bir_lowering=False)
    with ExitStack() as es:
        cx = Ctx(nc, es)
        S = cx.S
        I = lambda n, s, dt=F32: nc.dram_tensor(n, list(s), dt, kind="ExternalInput").ap()
        O = lambda n, s, dt=F32: nc.dram_tensor(n, list(s), dt, kind="ExternalOutput").ap()
        xb = I("xb", [SQ, D]); xo = I("xo", [NM * 512, D]); cT = I("cT", [128, 8])
        ada_w = I("ada_w", [D, 6 * D]); ada_b = I("ada_b", [1, 6 * D]); kva_w = I("kva_w", [D, 2 * D]); kva_b = I("kva_b", [1, 2 * D])
        gmix = I("gmix", [1, D]); gffn = I("gffn", [1, D]); gkv = I("gkv", [1, D])
        win = I("win", [D, 4 * D]); wout = I("wout", [D, D]); ongT = I("ongT", [128, 8]); lbl = I("lbl", [2, D])
        hcA = I("hcA", [128, 264]); hcB = I("hcB", [1, 1024]); oh = I("oh", [1, 4])
        kvw = I("kvw", [D, 2 * D]); pwq = I("pwq", [D, D]); skT = I("skT", [8, 128, 128])
        uT = I("uT", [D, NEXP]); v = I("v", [NEXP, D])
        h1 = O("h1", [NM * 512, D]); KTo = O("KTo", [NM, 8, 128, 512], BF16); Vo = O("Vo", [NM * 512, D], BF16)
        mod = cx.dram("mod", [128, 8 * D], F32)
        snapt = cx.dram("snap", [NQG, 128, D], F32)
        snap = [snapt[g] for g in range(NQG)]
        uTb = cx.dram("uTb", [D, NEXP], BF16); vb = cx.dram("vb", [NEXP, D], BF16)
        consts = make_consts(cx, es)
        emit_mod(cx, cT, [(ada_w, ada_b, 0, 6 * D), (kva_w, kva_b, 6 * D, 2 * D)], mod)
        emit_geff(cx, mod, 1 * D, gmix)
        emit_geff(cx, mod, 4 * D, gffn)
        emit_geff(cx, mod, 7 * D, gkv)
        if do_peer:
            emit_precast(cx, uT, uTb, D, NEXP)
            emit_precast(cx, v, vb, NEXP, D)
        HM = cx.sb(es, [128, 4, D]); bHM = S.bufs(4, "HM")
        emit_hgrn(cx, consts, HM, bHM, "state", xb, 4 * (NQG - 1), snap, mod, 0, D, 2 * D, win, wout, ongT, lbl, hcA, hcB)
        for m in range(NM):
            emit_hgrn(cx, consts, HM, bHM, "full", xo, 4, snap, mod, 0, D, 2 * D, win, wout, ongT, lbl, hcA, hcB, onehot_dram=oh, m=m)
            if do_peer:
                emit_peer(cx, consts, HM, bHM, mod, 3 * D, 4 * D, 5 * D, pwq, skT, uTb, vb)
            emit_store(cx, HM, bHM, m, h1)
            emit_kv(cx, consts, HM, bHM, m, mod, 6 * D, 7 * D, kvw, KTo, Vo)
        S.barrier()
        S.emit()
    return nc


def build_l2(NM, do_peer=True):
    SQ = 2048 * NM
    nc = bass.Bass("TRN2", target_bir_lowering=False)
    with ExitStack() as es:
        cx = Ctx(nc, es)
        S = cx.S
        I = lambda n, s, dt=F32: nc.dram_tensor(n, list(s), dt, kind="ExternalInput").ap()
        O = lambda n, s, dt=F32: nc.dram_tensor(n, list(s), dt, kind="ExternalOutput").ap()
        h1 = I("h1", [NM * 512, D]); cT = I("cT", [128, 8])
        ada_w = I("ada_w", [D, 6 * D]); ada_b = I("ada_b", [1, 6 * D])
        gmix = I("gmix", [1, D]); gffn = I("gffn", [1, D]); gfin = I("gfin", [1, D])
        sbwq = I("sbwq", [D, D]); sbwo = I("sbwo", [D, D])
        KT = I("KT", [8, 128, SQ], BF16); V = I("V", [8, 128, SQ // 128, 128], BF16)
        ac = I("ac", [128, 256], BF16); mask = I("mask", [128, 16, 512], BF16)
        pwq = I("pwq", [D, D]); skT = I("skT", [8, 128, 128]); uT = I("uT", [D, NEXP]); v = I("v", [NEXP, D])
        out = O("out", [NM * 512, D])
        mod = cx.dram("mod", [128, 6 * D], F32)
        uTb = cx.dram("uTb", [D, NEXP], BF16); vb = cx.dram("vb", [NEXP, D], BF16)
        consts = make_consts(cx, es)
        emit_mod(cx, cT, [(ada_w, ada_b, 0, 6 * D)], mod)
        emit_geff(cx, mod, 1 * D, gmix)
        emit_geff(cx, mod, 4 * D, gffn)
        if do_peer:
            emit_precast(cx, uT, uTb, D, NEXP)
            emit_precast(cx, v, vb, NEXP, D)
        HM = cx.sb(es, [128, 4, D]); bHM = S.bufs(4, "HM")
        for m in range(NM):
            for tt in range(4):
                S.dma("sp", HM[:, tt, :], h1[m * 512 + tt * 128:m * 512 + (tt + 1) * 128, :], writes=[bHM[tt]])
            emit_attn(cx, consts, HM, bHM, m, mod, 0, D, 2 * D, sbwq, sbwo, KT, V, ac, mask)
            if do_peer:
                emit_peer(cx, consts, HM, bHM, mod, 3 * D, 4 * D, 5 * D, pwq, skT, uTb, vb)
            emit_final(cx, consts, HM, bHM, m, gfin, out)
        S.barrier()
        S.emit()
    return nc


def run_model(inp, NM, do_peer=True, runner=None):
    f32 = lambda a: np.ascontiguousarray(np.asarray(a, dtype=np.float32))
    x = f32(inp["x"]); c = f32(inp["c"])
    B = x.shape[0]
    SQ = 2048 * NM
    assert x.shape == (B, SQ, D) and B == 2
    ncores = 8
    if runner is None:
        runner = lambda nc, maps: run_bass_kernel_spmd(nc, maps, core_ids=list(range(len(maps)))).results
    hcA, hcB = hgrn_consts_host()
    row = lambda a: f32(a).reshape(1, -1)
    colT = lambda a: np.ascontiguousarray(f32(a).reshape(8, 128).T)
    own = lambda b, r: np.concatenate([np.arange((4 * m + r) * 512, (4 * m + r + 1) * 512) for m in range(NM)])

    def peer_w(l):
        sk = f32(inp["peer_subkeys"][l]).reshape(8, 128, 128)
        return {"pwq": f32(inp["peer_w_q"][l]), "skT": np.ascontiguousarray(sk.transpose(0, 2, 1)),
                "uT": np.ascontiguousarray(f32(inp["peer_u"][l]).T), "v": f32(inp["peer_v"][l])}

    pw0 = peer_w(0)
    shared1 = {"ada_w": f32(inp["ada_w"][0]), "ada_b": row(inp["ada_b"][0]), "kva_w": f32(inp["kv_ada_w"]), "kva_b": row(inp["kv_ada_b"]),
               "gmix": row(inp["norm_mix_g"][0]), "gffn": row(inp["norm_ffn_g"][0]), "gkv": row(inp["kv_norm_g"]),
               "win": f32(inp["hgrn_w_in"][0]), "wout": f32(inp["hgrn_w_out"][0]), "ongT": colT(inp["hgrn_onorm_g"][0]),
               "lbl": f32(inp["hgrn_lb_logits"]), "hcA": hcA, "hcB": hcB, "kvw": f32(inp["kv_w"]), **pw0}
    maps = []
    for core in range(ncores):
        b, r = core // 4, core % 4
        oh = np.zeros((1, 4), np.float32); oh[0, r] = 1.0
        maps.append({"xb": x[b], "xo": np.ascontiguousarray(x[b][own(b, r)]), "cT": colT(c[b]), "oh": oh, **shared1})
    nc1 = build_l1(NM, do_peer)
    res1 = runner(nc1, maps)
    del maps, shared1, pw0
    KTf = np.zeros((B, 8, 128, SQ), ml_dtypes.bfloat16)
    Vf = np.zeros((B, SQ, D), ml_dtypes.bfloat16)
    for core in range(ncores):
        b, r = core // 4, core % 4
        kto = np.asarray(res1[core]["KTo"]); vo = np.asarray(res1[core]["Vo"])
        for m in range(NM):
            g = 4 * m + r
            KTf[b, :, :, g * 512:(g + 1) * 512] = kto[m]
            Vf[b, g * 512:(g + 1) * 512] = vo[m * 512:(m + 1) * 512]
    Vl = np.ascontiguousarray(Vf.reshape(B, SQ // 128, 128, 8, 128).transpose(0, 3, 2, 1, 4))
    pw1 = peer_w(1)
    shared2 = {"ada_w": f32(inp["ada_w"][1]), "ada_b": row(inp["ada_b"][1]), "gmix": row(inp["norm_mix_g"][1]), "gffn": row(inp["norm_ffn_g"][1]),
               "gfin": row(inp["final_norm_g"]), "sbwq": f32(inp["sb_w_q"][0]), "sbwo": f32(inp["sb_w_out"][0]), **pw1}
    maps = []
    for core in range(ncores):
        b, r = core // 4, core % 4
        ac, mask = attn_consts_host(r)
        maps.append({"h1": np.asarray(res1[core]["h1"]), "cT": colT(c[b]), "KT": KTf[b], "V": Vl[b], "ac": ac, "mask": mask, **shared2})
    nc2 = build_l2(NM, do_peer)
    res2 = runner(nc2, maps)
    out = np.zeros((B, SQ, D), np.float32)
    for core in range(ncores):
        b, r = core // 4, core % 4
        out[b, own(b, r)] = np.asarray(res2[core]["out"])
    return out


def kernel(**inputs):
    return run_model(inputs, 8)
```

```python
from contextlib import ExitStack
import math
import numpy as np
import ml_dtypes
import concourse.bass as bass
import concourse.mybir as mybir
from concourse.bass_utils import run_bass_kernel_spmd

F32 = mybir.dt.float32
BF16 = mybir.dt.bfloat16
ALU = mybir.AluOpType
AF = mybir.ActivationFunctionType
AX = mybir.AxisListType


class Buf:
    __slots__ = ("name", "w", "r")

    def __init__(self, name):
        self.name = name
        self.w = None
        self.r = []


class Sched:
    ENGS = ("pe", "act", "dve", "pool", "sp")
    NDMA = 8
    ROT = 20000

    def __init__(self, nc, es):
        self.nc = nc
        self.streams = {e: [] for e in self.ENGS}
        self.sem = {}
        self.cnt = {}
        for e in self.ENGS:
            self.sem[e] = es.enter_context(nc.semaphore("s_" + e))
            self.cnt[e] = 0
        self.dsem = {}
        self.dcnt = {}
        for e in ("sp", "act", "pool"):
            self.dsem[e] = [es.enter_context(nc.semaphore("d_%s%d" % (e, i))) for i in range(self.NDMA)]
            self.dcnt[e] = 0
        self.waited = {e: {} for e in self.ENGS}
        self.nbuf = 0
        self.es = es
        self.nrot = 0

    def buf(self, name=None):
        self.nbuf += 1
        return Buf(name or "b%d" % self.nbuf)

    def bufs(self, n, name="b"):
        return [self.buf("%s%d" % (name, i)) for i in range(n)]

    def _need(self, eng, reads, writes, same_ok):
        need = {}

        def add(tok):
            if tok is None:
                return
            sem, val, src = tok
            if same_ok and src == eng:
                return
            k = id(sem)
            if k not in need or need[k][1] < val:
                need[k] = (sem, val)

        for b in reads:
            add(b.w)
        for b in writes:
            add(b.w)
            for t in b.r:
                add(t)
        out = []
        wd = self.waited[eng]
        for k, (sem, val) in need.items():
            if wd.get(k, 0) >= val:
                continue
            wd[k] = val
            out.append((sem, val))
        return out

    def op(self, eng, fn, reads=(), writes=()):
        waits = self._need(eng, reads, writes, same_ok=(eng == "pe"))
        self.cnt[eng] += 1
        tok = (self.sem[eng], self.cnt[eng], eng)
        self.streams[eng].append((waits, fn, (self.sem[eng], 1)))
        for b in reads:
            b.r.append(tok)
        for b in writes:
            b.w = tok
            b.r = []
        return tok

    def dma(self, q, out, in_, reads=(), writes=(), **kw):
        j = self.dcnt[q]
        self.dcnt[q] += 1
        sem = self.dsem[q][j % self.NDMA]
        val = 16 * (j // self.NDMA + 1)
        waits = self._need(q, reads, writes, same_ok=False)
        if j >= self.NDMA:
            prev = val - 16
            wd = self.waited[q]
            if wd.get(id(sem), 0) < prev:
                wd[id(sem)] = prev
                waits.append((sem, prev))
        tok = (sem, val, "dma_" + q)
        self.streams[q].append((waits, lambda e: e.dma_start(out=out, in_=in_, **kw), (sem, 16)))
        for b in reads:
            b.r.append(tok)
        for b in writes:
            b.w = tok
            b.r = []
        return tok

    def coll(self, kind, groups, src, dst, reads=(), writes=()):
        q = "pool"
        j = self.dcnt[q]
        self.dcnt[q] += 1
        sem = self.dsem[q][j % self.NDMA]
        val = 16 * (j // self.NDMA + 1)
        waits = self._need(q, reads, writes, same_ok=False)
        if j >= self.NDMA:
            prev = val - 16
            wd = self.waited[q]
            if wd.get(id(sem), 0) < prev:
                wd[id(sem)] = prev
                waits.append((sem, prev))
        tok = (sem, val, "dma_" + q)
        self.streams[q].append((waits, lambda e: e.collective_compute(kind, ALU.bypass, groups, [src], [dst]), (sem, 16)))
        for b in reads:
            b.r.append(tok)
        for b in writes:
            b.w = tok
            b.r = []
        return tok

    def wait_all(self, eng, bufs):
        waits = self._need(eng, bufs, (), same_ok=False)
        self.streams[eng].append((waits, None, None))

    def barrier(self):
        toks = []
        for e in self.ENGS:
            if self.cnt[e]:
                toks.append((self.sem[e], self.cnt[e]))
        for q in self.dsem:
            j = self.dcnt[q]
            for s in range(min(j, self.NDMA)):
                last = ((j - 1 - s) // self.NDMA) if j - 1 >= s else -1
                uses = (j - s + self.NDMA - 1) // self.NDMA
                toks.append((self.dsem[q][s], 16 * uses))
        for e in self.ENGS:
            waits = []
            wd = self.waited[e]
            for sem, val in toks:
                if wd.get(id(sem), 0) >= val:
                    continue
                wd[id(sem)] = val
                waits.append((sem, val))
            if waits:
                self.streams[e].append((waits, None, None))
        for e in self.ENGS:
            if self.cnt[e] > self.ROT:
                self.sem[e] = self.es.enter_context(self.nc.semaphore("s_%s_%d" % (e, self.nrot)))
                self.nrot += 1
                self.cnt[e] = 0
        for q in self.dsem:
            if 16 * (self.dcnt[q] // self.NDMA + 1) > self.ROT:
                self.dsem[q] = [self.es.enter_context(self.nc.semaphore("d_%s%d_%d" % (q, i, self.nrot))) for i in range(self.NDMA)]
                self.nrot += 1
                self.dcnt[q] = 0

    def scatter(self, out, idx_ap, in_, reads=(), writes=()):
        q = "pool"
        j = self.dcnt[q]
        self.dcnt[q] += 1
        sem = self.dsem[q][j % self.NDMA]
        val = 16 * (j // self.NDMA + 1)
        waits = self._need(q, reads, writes, same_ok=False)
        if j >= self.NDMA:
            prev = val - 16
            wd = self.waited[q]
            if wd.get(id(sem), 0) < prev:
                wd[id(sem)] = prev
                waits.append((sem, prev))
        tok = (sem, val, "dma_" + q)
        self.streams[q].append((waits, lambda e: e.indirect_dma_start(out=out, out_offset=bass.IndirectOffsetOnAxis(ap=idx_ap, axis=0),
                                                                      in_=in_, in_offset=None, bounds_check=out.shape[0] - 1, oob_is_err=False), (sem, 16)))
        for b in reads:
            b.r.append(tok)
        for b in writes:
            b.w = tok
            b.r = []
        return tok

    def core_barrier(self):
        self.barrier()
        self.emit()
        self.streams = {e: [] for e in self.ENGS}
        self.nc.all_core_barrier()

    def emit(self):
        nc = self.nc
        handles = {"pe": "tensor", "act": "scalar", "dve": "vector", "pool": "gpsimd", "sp": "sync"}
        with nc.Block() as block:
            for e in self.ENGS:
                stream = self.streams[e]

                def body(eng, stream=stream):
                    for waits, fn, inc in stream:
                        for sem, val in waits:
                            eng.wait_ge(sem, val)
                        if fn is not None:
                            ins = fn(eng)
                            ins.then_inc(inc[0], inc[1])

                getattr(block, handles[e])(body)


D = 1024
NEXP = 16384
EPS = 1e-6


class Ctx:
    def __init__(self, nc, es):
        self.nc = nc
        self.es = es
        self.S = Sched(nc, es)
        self.n = 0

    def sb(self, es, shape, dt=F32, name=None):
        self.n += 1
        return es.enter_context(self.nc.sbuf_tensor(name or "t%d" % self.n, list(shape), dt))

    def ps(self, es, shape, dt=F32, name=None):
        self.n += 1
        return es.enter_context(self.nc.psum_tensor(name or "p%d" % self.n, list(shape), dt))

    def dram(self, name, shape, dt, kind="Internal"):
        return self.nc.dram_tensor(name, list(shape), dt, kind=kind).ap()


def make_consts(cx, es):
    S = cx.S
    c = {}
    identf = cx.sb(es, [128, 128], F32)
    identb = cx.sb(es, [128, 128], BF16)
    b = S.buf("ident")
    S.op("pool", lambda e: e.memset(identf[:], 1.0), writes=[b])
    S.op("pool", lambda e: e.affine_select(out=identf[:], in_=identf[:], pattern=[[-1, 128]],
                                           compare_op=ALU.is_equal, fill=0.0, base=0, channel_multiplier=1),
         reads=[b], writes=[b])
    S.op("dve", lambda e: e.tensor_copy(out=identb[:], in_=identf[:]), reads=[b], writes=[b])
    c["identf"], c["identb"], c["b_ident"] = identf, identb, b
    return c


def emit_mod(cx, cT_ap, specs, out_dram):
    S = cx.S
    with ExitStack() as es:
        cs = cx.sb(es, [128, 8])
        sg = cx.sb(es, [128, 8])
        CA = cx.sb(es, [128, 8, 128])
        wst = [cx.sb(es, [128, 2048]) for _ in range(2)]
        bwst = S.bufs(2, "wst")
        bias = cx.sb(es, [128, 2048])
        res = cx.sb(es, [128, 2048])
        pm = cx.ps(es, [128, 4, 512])
        b_cs, b_CA, b_bias, b_res, b_pm = S.buf(), S.buf(), S.buf(), S.buf(), S.buf()
        S.dma("sp", cs[:], cT_ap, writes=[b_cs])
        S.op("act", lambda e: e.activation(out=sg[:], in_=cs[:], func=AF.Sigmoid), reads=[b_cs], writes=[b_CA])
        S.op("dve", lambda e: e.tensor_tensor(out=cs[:], in0=cs[:], in1=sg[:], op=ALU.mult), reads=[b_cs, b_CA], writes=[b_cs])
        S.op("dve", lambda e: e.tensor_copy(out=CA[:], in_=cs[:].unsqueeze(2).to_broadcast([128, 8, 128])),
             reads=[b_cs], writes=[b_CA])
        it = 0
        for (W, B, col0, ncols) in specs:
            for cb in range(0, ncols, 2048):
                S.dma("pool", bias[:], B[0:1, cb:cb + 2048].partition_broadcast(128), writes=[b_bias])
                for kc in range(8):
                    w = wst[it % 2]
                    bw = bwst[it % 2]
                    it += 1
                    S.dma("sp", w[:], W[kc * 128:(kc + 1) * 128, cb:cb + 2048], writes=[bw])
                    for nb in range(4):
                        S.op("pe", lambda e, w=w, nb=nb, kc=kc: e.matmul(pm[:, nb, :], lhsT=CA[:, kc, :], rhs=w[:, nb * 512:(nb + 1) * 512],
                                                                        start=(kc == 0), stop=(kc == 7)),
                             reads=[b_CA, bw], writes=[b_pm])
                S.op("dve", lambda e: e.tensor_tensor(out=res[:], in0=pm[:].rearrange("p a b -> p (a b)"), in1=bias[:], op=ALU.add),
                     reads=[b_pm, b_bias], writes=[b_res])
                S.dma("sp", out_dram[:, col0 + cb:col0 + cb + 2048], res[:], reads=[b_res])
        S.barrier()


def emit_rstd(cx, ss_ap, rstd_ap, inv_n, b_ss, b_rstd):
    S = cx.S
    S.op("dve", lambda e: e.tensor_scalar(out=rstd_ap, in0=ss_ap, scalar1=inv_n, scalar2=EPS, op0=ALU.mult, op1=ALU.add),
         reads=[b_ss], writes=[b_rstd])
    S.op("act", lambda e: e.activation(out=rstd_ap, in_=rstd_ap, func=AF.Ln), reads=[b_rstd], writes=[b_rstd])
    S.op("act", lambda e: e.activation(out=rstd_ap, in_=rstd_ap, func=AF.Exp, scale=-0.5), reads=[b_rstd], writes=[b_rstd])


class NormT:
    def __init__(self, cx, es, consts, pt=None, b_pt=None):
        self.cx = cx
        self.c = consts
        S = cx.S
        self.junk = cx.sb(es, [128, 1024], BF16)
        self.st = cx.sb(es, [128, 4])
        self.tmp = cx.sb(es, [128, 1024])
        self.hb = cx.sb(es, [128, 1024], BF16)
        self.pt = pt if pt is not None else cx.ps(es, [128, 8, 128], BF16)
        self.b_junk, self.b_st, self.b_tmp, self.b_hb = S.buf(), S.buf(), S.buf(), S.buf()
        self.b_pt = b_pt if b_pt is not None else S.buf()

    def norm(self, x_ap, b_x, geff, shift, b_mod, out_ap, b_out, out_eng="pool"):
        S = self.cx.S
        st, tmp = self.st, self.tmp
        S.op("act", lambda e: e.activation(out=self.junk[:], in_=x_ap, func=AF.Square, accum_out=st[:, 0:1]),
             reads=[b_x], writes=[self.b_junk, self.b_st])
        emit_rstd(self.cx, st[:, 0:1], st[:, 1:2], 1.0 / D, self.b_st, self.b_st)
        S.op("dve", lambda e: e.scalar_tensor_tensor(out=tmp[:], in0=x_ap, scalar=st[:, 1:2], in1=geff, op0=ALU.mult, op1=ALU.mult),
             reads=[b_x, self.b_st, b_mod], writes=[self.b_tmp])
        S.op(out_eng, lambda e: e.tensor_tensor(out=out_ap, in0=tmp[:], in1=shift, op=ALU.add),
             reads=[self.b_tmp, b_mod], writes=[b_out])

    def transpose(self, in_bf, b_in, outT_ap, b_out):
        S = self.cx.S
        for kc in range(8):
            S.op("pe", lambda e, kc=kc: e.transpose(self.pt[:, kc, :], in_bf[:, kc * 128:(kc + 1) * 128], self.c["identb"][:]),
                 reads=[b_in, self.c["b_ident"]], writes=[self.b_pt])
        S.op("act", lambda e: e.copy(out=outT_ap, in_=self.pt[:, :, :]), reads=[self.b_pt], writes=[b_out])

    def normT(self, x_ap, b_x, geff, shift, b_mod, outT_ap, b_out):
        self.norm(x_ap, b_x, geff, shift, b_mod, self.hb[:], self.b_hb)
        self.transpose(self.hb, self.b_hb, outT_ap, b_out)


def load_cast(cx, es_tmp, q, dst_ap_fn, src_ap_fn, nchunks, shape, b_dst, cast_eng="pool"):
    S = cx.S
    stg = [cx.sb(es_tmp, shape) for _ in range(2)]
    bst = S.bufs(2, "stg")
    for i in range(nchunks):
        s, b = stg[i % 2], bst[i % 2]
        S.dma(q, s[:], src_ap_fn(i), writes=[b])
        S.op(cast_eng, lambda e, s=s, i=i: e.tensor_copy(out=dst_ap_fn(i), in_=s[:]), reads=[b], writes=[b_dst])


def emit_geff(cx, mod_dram, col, g_dram):
    S = cx.S
    with ExitStack() as es:
        a = cx.sb(es, [128, 1024])
        g = cx.sb(es, [128, 1024])
        ba, bg = S.buf(), S.buf()
        S.dma("sp", a[:], mod_dram[:, col:col + 1024], writes=[ba])
        S.dma("pool", g[:], g_dram[0:1, :].partition_broadcast(128), writes=[bg])
        S.op("dve", lambda e: e.scalar_tensor_tensor(out=a[:], in0=a[:], scalar=1.0, in1=g[:], op0=ALU.add, op1=ALU.mult),
             reads=[ba, bg], writes=[ba])
        S.dma("sp", mod_dram[:, col:col + 1024], a[:], reads=[ba])
        S.barrier()


def emit_precast(cx, src, dst, rows, cols):
    S = cx.S
    with ExitStack() as es:
        CW = 4096
        st = [cx.sb(es, [128, CW]) for _ in range(2)]
        ob = [cx.sb(es, [128, CW], BF16) for _ in range(2)]
        bs, bo = S.bufs(2), S.bufs(2)
        srcv = src.rearrange("(a p) c -> p a c", p=128)
        dstv = dst.rearrange("(a p) c -> p a c", p=128)
        i = 0
        per = max(1, CW // cols)
        cw = min(CW, cols)
        for a0 in range(0, rows // 128, per):
            for c0 in range(0, cols, cw):
                s, o = st[i % 2], ob[i % 2]
                sv = s[:].rearrange("p (a c) -> p a c", a=per)
                ov = o[:].rearrange("p (a c) -> p a c", a=per)
                S.dma("sp" if i % 2 == 0 else "act", sv, srcv[:, a0:a0 + per, c0:c0 + cw], writes=[bs[i % 2]])
                eng = ("pool", "dve")[i % 2]
                S.op(eng, lambda e, s=s, o=o: e.tensor_copy(out=o[:], in_=s[:]), reads=[bs[i % 2]], writes=[bo[i % 2]])
                S.dma("pool", dstv[:, a0:a0 + per, c0:c0 + cw], ov, reads=[bo[i % 2]])
                i += 1
        S.barrier()


def emit_fold_cast(cx, src, dst, rowT_dram=None, col_dram=None, col0=0):
    S = cx.S
    with ExitStack() as es:
        st = [cx.sb(es, [128, 1024]) for _ in range(2)]; ob = [cx.sb(es, [128, 1024], BF16) for _ in range(2)]
        bs, bo = S.bufs(2), S.bufs(2)
        bc = S.buf()
        if rowT_dram is not None:
            rT = cx.sb(es, [128, 8]); S.dma("sp", rT[:], rowT_dram, writes=[bc])
        if col_dram is not None:
            cB = cx.sb(es, [128, 1024]); S.dma("sp", cB[:], col_dram[:, col0:col0 + 1024], writes=[bc])
        for kc in range(8):
            s, o = st[kc % 2], ob[kc % 2]
            S.dma("sp", s[:], src[kc * 128:(kc + 1) * 128, :], writes=[bs[kc % 2]])
            if rowT_dram is not None:
                S.op("dve", lambda e, s=s, kc=kc: e.tensor_scalar(out=s[:], in0=s[:], scalar1=rT[:, kc:kc + 1], scalar2=None, op0=ALU.mult),
                     reads=[bs[kc % 2], bc], writes=[bs[kc % 2]])
            if col_dram is not None:
                S.op("dve", lambda e, s=s, o=o: e.tensor_tensor(out=o[:], in0=s[:], in1=cB[:], op=ALU.mult), reads=[bs[kc % 2], bc], writes=[bo[kc % 2]])
            else:
                S.op("dve", lambda e, s=s, o=o: e.tensor_copy(out=o[:], in_=s[:]), reads=[bs[kc % 2]], writes=[bo[kc % 2]])
            S.dma("act", dst[kc * 128:(kc + 1) * 128, :], o[:], reads=[bo[kc % 2]])
        S.barrier()


def load_w(cx, dst, src_bf, b_dst, ncol):
    S = cx.S
    v = src_bf.rearrange("(kc p) n -> p kc n", p=128)
    S.dma("sp", dst[:, 0:4, :], v[:, 0:4, :], writes=[b_dst])
    S.dma("act", dst[:, 4:8, :], v[:, 4:8, :], writes=[b_dst])


PEER_XENG = ("dve", "dve", "dve", "dve")


def emit_peer(cx, consts, HM, bHM, mod_dram, c_sh, c_ge, c_g, wq_dram, skT_dram, uTb, vb):
    S = cx.S
    GA = 4
    NG = 128 // GA
    with ExitStack() as es:
        G2 = cx.sb(es, [128, 1024]); b_g2 = S.buf("g2")
        S.dma("sp", G2[:], mod_dram[:, c_g:c_g + 1024], writes=[b_g2])
        xnT = cx.sb(es, [128, 8, 512], BF16); b_xnT = S.bufs(4, "xnT")
        QA = cx.sb(es, [128, 4, 1024]); bQA = S.bufs(4, "QA")
        PSA = cx.ps(es, [128, 4, 512]); bPSA = S.bufs(4, "PSA")
        Pexp = cx.sb(es, [128, 4, 8, 128]); bP = S.bufs(4, "Pexp")
        TH = cx.sb(es, [128, 4, 4]); RZ = cx.sb(es, [128, 4, 4]); P1n = cx.sb(es, [128, 4, 4, 128]); bTH = S.bufs(4, "TH")
        with ExitStack() as es1:
            SH = cx.sb(es1, [128, 1024]); GE = cx.sb(es1, [128, 1024]); b_mod = S.buf("mod")
            S.dma("sp", SH[:], mod_dram[:, c_sh:c_sh + 1024], writes=[b_mod])
            S.dma("sp", GE[:], mod_dram[:, c_ge:c_ge + 1024], writes=[b_mod])
            wq = cx.sb(es1, [128, 8, 1024], BF16); b_wq = S.buf("wq")
            load_w(cx, wq, wq_dram, b_wq, 1024)
            skT = cx.sb(es1, [128, 8, 128]); b_sk = S.buf("sk")
            S.dma("sp", skT[:], skT_dram.rearrange("h d n -> d h n"), writes=[b_sk])
            nt = NormT(cx, es1, consts)
            top = cx.sb(es1, [128, 8, 16]); work = cx.sb(es1, [128, 128]); cand = cx.sb(es1, [128, 256]); work2 = cx.sb(es1, [128, 256])
            c24 = cx.sb(es1, [128, 24]); mx = cx.sb(es1, [128, 8]); zs = cx.sb(es1, [128, 4]); th4 = cx.sb(es1, [128, 4])
            b_sm = S.buf("small")
            for tt in range(4):
                nt.normT(HM[:, tt, :], bHM[tt], GE[:], SH[:], b_mod, xnT[:, :, tt * 128:(tt + 1) * 128], b_xnT[tt])
            for hp in range(8):
                bank = hp % 4
                for kc in range(8):
                    S.op("pe", lambda e, hp=hp, kc=kc, bank=bank: e.matmul(PSA[:, bank, :], lhsT=wq[:, kc, hp * 128:(hp + 1) * 128], rhs=xnT[:, kc, :],
                                                                           start=(kc == 0), stop=(kc == 7)),
                         reads=[b_wq] + b_xnT, writes=[bPSA[bank]])
                S.op("act" if hp % 2 else "dve", (lambda e, hp=hp, bank=bank: e.copy(out=QA[:, hp // 2, (hp % 2) * 512:(hp % 2 + 1) * 512], in_=PSA[:, bank, :])) if hp % 2 else
                     (lambda e, hp=hp, bank=bank: e.tensor_copy(out=QA[:, hp // 2, (hp % 2) * 512:(hp % 2 + 1) * 512], in_=PSA[:, bank, :])),
                     reads=[bPSA[bank]], writes=[bQA[hp // 2]])
            for tt in range(4):
                pb = 2 * (tt % 2)
                for hp in range(8):
                    S.op("pe", lambda e, hp=hp, tt=tt, pb=pb: e.matmul(PSA[:, pb + hp // 4, (hp % 4) * 128:(hp % 4 + 1) * 128],
                                                                      lhsT=QA[:, hp // 2, (hp % 2) * 512 + tt * 128:(hp % 2) * 512 + (tt + 1) * 128],
                                                                      rhs=skT[:, hp, :], start=True, stop=True),
                         reads=[bQA[hp // 2], b_sk], writes=[bPSA[pb + hp // 4]])
                scv = PSA[:, pb:pb + 2, :].rearrange("p a (h n) -> p (a h) n", n=128)
                S.op("dve", lambda e, scv=scv: e.reduce_max(out=mx[:], in_=scv, axis=AX.X), reads=[bPSA[pb], bPSA[pb + 1]], writes=[b_sm])
                S.op("dve", lambda e: e.tensor_scalar(out=mx[:], in0=mx[:], scalar1=-1.0, scalar2=None, op0=ALU.mult), reads=[b_sm], writes=[b_sm])
                for hp in range(8):
                    S.op("act", lambda e, hp=hp, tt=tt, pb=pb: e.activation(out=Pexp[:, tt, hp, :], in_=PSA[:, pb + hp // 4, (hp % 4) * 128:(hp % 4 + 1) * 128],
                                                                           func=AF.Exp, bias=mx[:, hp:hp + 1], scale=1.0),
                         reads=[bPSA[pb + hp // 4], b_sm], writes=[bP[tt]])
                for hp in range(8):
                    S.op("dve", lambda e, hp=hp, tt=tt: e.max(out=top[:, hp, 0:8], in_=Pexp[:, tt, hp, :]), reads=[bP[tt]], writes=[b_sm])
                    S.op("dve", lambda e, hp=hp, tt=tt: e.match_replace(out=work[:], in_to_replace=top[:, hp, 0:8], in_values=Pexp[:, tt, hp, :], imm_value=-1.0),
                         reads=[bP[tt], b_sm], writes=[b_sm])
                    S.op("dve", lambda e, hp=hp: e.max(out=top[:, hp, 8:16], in_=work[:]), reads=[b_sm], writes=[b_sm])
                for h in range(4):
                    S.op("dve", lambda e, h=h: e.tensor_tensor(out=cand[:].rearrange("p (a b) -> p a b", a=16),
                                                               in0=top[:, 2 * h, :].unsqueeze(2).to_broadcast([128, 16, 16]),
                                                               in1=top[:, 2 * h + 1, :].unsqueeze(1).to_broadcast([128, 16, 16]), op=ALU.mult),
                         reads=[b_sm], writes=[b_sm])
                    S.op("dve", lambda e: e.max(out=c24[:, 0:8], in_=cand[:]), reads=[b_sm], writes=[b_sm])
                    S.op("dve", lambda e: e.match_replace(out=work2[:], in_to_replace=c24[:, 0:8], in_values=cand[:], imm_value=-1.0), reads=[b_sm], writes=[b_sm])
                    S.op("dve", lambda e: e.max(out=c24[:, 8:16], in_=work2[:]), reads=[b_sm], writes=[b_sm])
                    S.op("dve", lambda e: e.match_replace(out=cand[:], in_to_replace=c24[:, 8:16], in_values=work2[:], imm_value=-1.0), reads=[b_sm], writes=[b_sm])
                    S.op("dve", lambda e: e.max(out=c24[:, 16:24], in_=cand[:]), reads=[b_sm], writes=[b_sm])
                    S.op("dve", lambda e, h=h: e.tensor_tensor(out=th4[:, h:h + 1], in0=c24[:, 15:16], in1=c24[:, 16:17], op=ALU.add), reads=[b_sm], writes=[b_sm])
                    S.op("dve", lambda e, h=h: e.reduce_sum(out=zs[:, h:h + 1], in_=c24[:, 0:16], axis=AX.X), reads=[b_sm], writes=[b_sm])
                S.op("dve", lambda e, tt=tt: e.reciprocal(out=RZ[:, tt, :], in_=zs[:]), reads=[b_sm], writes=[bTH[tt]])
                S.op("dve", lambda e, tt=tt: e.scalar_tensor_tensor(out=TH[:, tt, :], in0=th4[:], scalar=0.5, in1=RZ[:, tt, :], op0=ALU.mult, op1=ALU.mult),
                     reads=[b_sm, bTH[tt]], writes=[bTH[tt]])
                for h in range(4):
                    S.op("dve", lambda e, tt=tt, h=h: e.tensor_scalar(out=P1n[:, tt, h, :], in0=Pexp[:, tt, 2 * h, :], scalar1=RZ[:, tt, h:h + 1], scalar2=None, op0=ALU.mult),
                         reads=[bP[tt], bTH[tt]], writes=[bTH[tt]])
            S.barrier()
        with ExitStack() as es3:
            PH = cx.ps(es3, [128, 2, 512]); bPH = S.bufs(2, "PH")
            PW = [cx.ps(es3, [128, 512]) for _ in range(2)]; bPW = S.bufs(2, "PW")
            UG = [cx.sb(es3, [128, 8, GA * 128], BF16) for _ in range(2)]; bUG = S.bufs(2, "UG")
            NV = 3
            VG = [cx.sb(es3, [128, GA, 1024], BF16) for _ in range(NV)]; bVG = S.bufs(NV, "VG")
            NX = 6
            Xn = [cx.sb(es3, [128, GA, 128]) for _ in range(NX)]; bXn = S.bufs(NX, "Xn")
            Wh = [[cx.sb(es3, [128, GA, 128], BF16) for _ in range(16)] for _ in range(2)]; bWh = [S.bufs(16, "Wh%d_" % k) for k in range(2)]
            G = [cx.sb(es3, [128, 512], BF16) for _ in range(2 * GA)]; bG = S.bufs(2 * GA, "G")
            WgT = [cx.sb(es3, [128, GA, 512], BF16) for _ in range(2)]; bWg = S.bufs(2, "WgT")
            uv = uTb.rearrange("(kc p) n -> p kc n", p=128)
            vv = vb.rearrange("(a p) d -> p a d", p=128)
            cnt = {"x": 0, "ph": 0, "pw": 0, "fl": 0}
            XENG = PEER_XENG

            def stage1(gi):
                ug, bu = UG[gi % 2], bUG[gi % 2]
                S.dma("sp", ug[:, 0:4, :], uv[:, 0:4, gi * GA * 128:(gi + 1) * GA * 128], writes=[bu])
                S.dma("act", ug[:, 4:8, :], uv[:, 4:8, gi * GA * 128:(gi + 1) * GA * 128], writes=[bu])
                for ac in range(GA):
                    j = cnt["ph"] % 2
                    cnt["ph"] += 1
                    g, bg = G[(gi % 2) * GA + ac], bG[(gi % 2) * GA + ac]
                    for kc in range(8):
                        S.op("pe", lambda e, ac=ac, kc=kc, j=j, ug=ug: e.matmul(PH[:, j, :], lhsT=ug[:, kc, ac * 128:(ac + 1) * 128], rhs=xnT[:, kc, :],
                                                                              start=(kc == 0), stop=(kc == 7)),
                             reads=[bu] + b_xnT, writes=[bPH[j]])
                    S.op("act", lambda e, j=j, g=g: e.activation(out=g[:], in_=PH[:, j, :], func=AF.Gelu), reads=[bPH[j]], writes=[bg])
                for tt in range(4):
                    for h in range(4):
                        x, bx = Xn[cnt["x"] % NX], bXn[cnt["x"] % NX]
                        cnt["x"] += 1
                        if True:
                            S.op(XENG[h], lambda e, tt=tt, h=h, x=x, gi=gi: e.tensor_tensor(
                                out=x[:], in0=P1n[:, tt, h, gi * GA:(gi + 1) * GA].unsqueeze(2).to_broadcast([128, GA, 128]),
                                in1=Pexp[:, tt, 2 * h + 1, :].unsqueeze(1).to_broadcast([128, GA, 128]), op=ALU.mult),
                                reads=[bP[tt], bTH[tt]], writes=[bx])
                        else:
                            for a in range(GA):
                                S.op("act", lambda e, tt=tt, h=h, a=a, x=x, gi=gi: e.activation(out=x[:, a, :], in_=Pexp[:, tt, 2 * h + 1, :], func=AF.Copy,
                                                                                             scale=P1n[:, tt, h, gi * GA + a:gi * GA + a + 1]),
                                     reads=[bP[tt], bTH[tt]], writes=[bx])
                        w, bw = Wh[gi % 2][tt * 4 + h], bWh[gi % 2][tt * 4 + h]
                        S.op("dve", lambda e, tt=tt, h=h, x=x, w=w: e.scalar_tensor_tensor(out=w[:], in0=x[:], scalar=TH[:, tt, h:h + 1], in1=x[:],
                                                                                          op0=ALU.is_ge, op1=ALU.mult),
                             reads=[bx, bTH[tt]], writes=[bw])

            def stage2(gi):
                vg, bv = VG[gi % NV], bVG[gi % NV]
                S.dma("sp", vg[:, 0:2, :], vv[:, gi * GA:gi * GA + 2, :], writes=[bv])
                S.dma("act", vg[:, 2:4, :], vv[:, gi * GA + 2:(gi + 1) * GA, :], writes=[bv])
                wg, bwg = WgT[gi % 2], bWg[gi % 2]
                for ac in range(GA):
                    j = cnt["pw"] % 2
                    cnt["pw"] += 1
                    g, bg = G[(gi % 2) * GA + ac], bG[(gi % 2) * GA + ac]
                    for tt in range(4):
                        for h in range(4):
                            w, bw = Wh[gi % 2][tt * 4 + h], bWh[gi % 2][tt * 4 + h]
                            S.op("pe", lambda e, ac=ac, tt=tt, j=j, w=w, h=h: e.matmul(PW[j][:, tt * 128:(tt + 1) * 128], lhsT=w[:, ac, :], rhs=consts["identb"][:],
                                                                                     start=(h == 0), stop=(h == 3)),
                                 reads=[bw, consts["b_ident"]], writes=[bPW[j]])
                    S.op("dve", lambda e, ac=ac, j=j, wg=wg, g=g: e.tensor_tensor(out=wg[:, ac, :], in0=g[:], in1=PW[j][:], op=ALU.mult),
                         reads=[bg, bPW[j]], writes=[bwg])

            def stage3(gi):
                vg, bv = VG[gi % NV], bVG[gi % NV]
                wg, bwg = WgT[gi % 2], bWg[gi % 2]
                for tt in range(4):
                    pb = 2 * (tt % 2)
                    for ac in range(GA):
                        for half in range(2):
                            S.op("pe", lambda e, ac=ac, tt=tt, half=half, pb=pb, wg=wg, vg=vg: e.matmul(
                                PSA[:, pb + half, :], lhsT=wg[:, ac, tt * 128:(tt + 1) * 128], rhs=vg[:, ac, half * 512:(half + 1) * 512],
                                start=(ac == 0), stop=(ac == GA - 1)),
                                reads=[bwg, bv], writes=[bPSA[pb + half]])
                    src = PSA[:, pb:pb + 2, :].rearrange("p a b -> p (a b)")
                    if gi == 0:
                        S.op("act", lambda e, tt=tt, src=src: e.copy(out=QA[:, tt, :], in_=src), reads=[bPSA[pb], bPSA[pb + 1]], writes=[bQA[tt]])
                    else:
                        S.op("dve", lambda e, tt=tt, src=src: e.tensor_tensor(out=QA[:, tt, :], in0=QA[:, tt, :], in1=src, op=ALU.add),
                             reads=[bPSA[pb], bPSA[pb + 1], bQA[tt]], writes=[bQA[tt]])

            for k in range(NG + 2):
                if k < NG:
                    stage1(k)
                if 0 <= k - 1 < NG:
                    stage2(k - 1)
                if 0 <= k - 2 < NG:
                    stage3(k - 2)
            for tt in range(4):
                S.op("pool", lambda e, tt=tt: e.tensor_tensor(out=QA[:, tt, :], in0=QA[:, tt, :], in1=G2[:], op=ALU.mult), reads=[bQA[tt], b_g2], writes=[bQA[tt]])
                S.op("dve", lambda e, tt=tt: e.tensor_tensor(out=HM[:, tt, :], in0=HM[:, tt, :], in1=QA[:, tt, :], op=ALU.add), reads=[bQA[tt], bHM[tt]], writes=[bHM[tt]])
            S.barrier()


def hgrn_consts_host():
    t = np.arange(128)
    ch = t // 16
    same = ch[:, None] == ch[None, :]
    BT = (same & (t[:, None] <= t[None, :])).astype(np.float32)
    RT = (same & (t[:, None] > t[None, :])).astype(np.float32)
    CI = (ch[:, None] == np.arange(8)[None, :]).astype(np.float32)
    RTF = (t[:, None] > t[None, :]).astype(np.float32)
    hcA = np.concatenate([BT, RT, CI, RTF, np.ones((128, 1), np.float32)], axis=1)
    hcB = np.ascontiguousarray(CI.T).reshape(1, 1024)
    return hcA, hcB


def emit_hgrn(cx, consts, HM, bHM, mode, x_dram, ntiles, snap, mod_dram, c_sh, c_ge, c_g,
              win_dram, wout_dram, ongT_dram, lbl_dram, hcA_dram, hcB_dram, onehot_dram=None, m=0):
    S = cx.S
    full = (mode == "full")
    with ExitStack() as es:
        hcA = cx.sb(es, [128, 393]); CHM = cx.sb(es, [128, 8, 128]); b_hc = S.buf("hc")
        S.dma("sp", hcA[:], hcA_dram, writes=[b_hc])
        S.dma("pool", CHM[:].rearrange("p a b -> p (a b)"), hcB_dram[0:1, :].partition_broadcast(128), writes=[b_hc])
        BT, RT, CI = hcA[:, 0:128], hcA[:, 128:256], hcA[:, 256:264]
        RTF, ONE = hcA[:, 264:392], hcA[:, 392:393]
        LB = cx.sb(es, [128, 1024]); OML = cx.sb(es, [128, 1024]); GE = cx.sb(es, [128, 1024]); SH = cx.sb(es, [128, 1024])
        b_mod = S.buf("hmod")
        S.dma("sp", SH[:], mod_dram[:, c_sh:c_sh + 1024], writes=[b_mod])
        S.dma("sp", GE[:], mod_dram[:, c_ge:c_ge + 1024], writes=[b_mod])
        S.dma("pool", LB[:], lbl_dram[0:1, :].partition_broadcast(128), writes=[b_mod])
        S.dma("pool", OML[:], lbl_dram[1:2, :].partition_broadcast(128), writes=[b_mod])
        S.op("dve", lambda e: e.tensor_tensor(out=LB[:], in0=LB[:], in1=OML[:], op=ALU.subtract), reads=[b_mod], writes=[b_mod])
        S.op("act", lambda e: e.activation(out=LB[:], in_=LB[:], func=AF.Sigmoid), reads=[b_mod], writes=[b_mod])
        S.op("dve", lambda e: e.tensor_scalar(out=OML[:], in0=LB[:], scalar1=-1.0, scalar2=1.0, op0=ALU.mult, op1=ALU.add), reads=[b_mod], writes=[b_mod])
        win = cx.sb(es, [128, 8, 4096], BF16); b_win = S.buf("win")
        wout = cx.sb(es, [128, 8, 1024], BF16); b_wout = S.buf("wout")
        load_w(cx, win, win_dram, b_win, 4096)
        if full:
            load_w(cx, wout, wout_dram, b_wout, 1024)
        nt = NormT(cx, es, consts)
        xt = [cx.sb(es, [128, 1024]) for _ in range(2)]; b_xt = S.bufs(2, "xt")
        hnT = cx.sb(es, [128, 8, 128], BF16); b_hnT = S.buf("hnT")
        R = [cx.sb(es, [128, 1024]) for _ in range(6)]; bR = S.bufs(6, "R")
        OGb = cx.sb(es, [128, 1024], BF16); b_og = S.buf("og")
        ogT = cx.sb(es, [128, 8, 128], BF16); b_ogT = S.buf("ogT")
        QKT = [cx.sb(es, [128, 2, 128]) for _ in range(2)]; bQKT = S.bufs(2, "QKT")
        SCM = [cx.sb(es, [128, 128]) for _ in range(2)]; bSCM = S.bufs(2, "SCM")
        QDM = cx.sb(es, [128, 8, 128]); bQDM = S.buf("QDM")
        KLM = cx.sb(es, [128, 8, 128]); bKLM = S.buf("KLM")
        ST = cx.sb(es, [128, 8, 2, 128]); bST = [S.bufs(2, "ST%d_" % h) for h in range(8)]
        EDEC = cx.sb(es, [128, 64]); bEDEC = S.buf("EDEC")
        ss8 = cx.sb(es, [128, 16]); b_ss8 = S.buf("ss8")
        PJ = [cx.ps(es, [128, 512]) for _ in range(3)]; bPJ = S.bufs(3, "PJ")
        PKV = [cx.ps(es, [128, 512]) for _ in range(2)]; bPKV = S.bufs(2, "PKV")
        PO = cx.ps(es, [128, 512]); bPO = S.buf("PO")
        PC = cx.ps(es, [128, 512]); bPC = S.buf("PC")
        pj_i = [0]

        def nextpj():
            k = pj_i[0] % 3
            pj_i[0] += 1
            return PJ[k], bPJ[k]

        def proj(j, nb):
            pj, b = nextpj()
            for kc in range(8):
                S.op("pe", lambda e, pj=pj, kc=kc: e.matmul(pj[:], lhsT=hnT[:, kc, :],
                                                            rhs=win[:, kc, j * 1024 + nb * 512:j * 1024 + (nb + 1) * 512],
                                                            start=(kc == 0), stop=(kc == 7)),
                     reads=[b_hnT, b_win], writes=[b])
            return pj, b

        if full:
            oh = cx.sb(es, [128, 4]); b_oh = S.buf("oh")
            S.dma("pool", oh[:], onehot_dram[0:1, :].partition_broadcast(128), writes=[b_oh])
            stv = ST[:, :, 0, :]
            allst = [bST[h][0] for h in range(8)]
            for j in range(4):
                S.dma("sp", R[5][:], snap[4 * m + j], writes=[bR[5]])
                r5 = R[5][:].rearrange("p (h v) -> p h v", h=8)
                if j == 0:
                    S.op("dve", lambda e, j=j, r5=r5: e.tensor_scalar(out=stv, in0=r5, scalar1=oh[:, j:j + 1], scalar2=None, op0=ALU.mult),
                         reads=[bR[5], b_oh], writes=allst)
                else:
                    S.op("dve", lambda e, j=j, r5=r5: e.scalar_tensor_tensor(out=stv, in0=r5, scalar=oh[:, j:j + 1], in1=stv, op0=ALU.mult, op1=ALU.add),
                         reads=[bR[5], b_oh] + allst, writes=allst)
        else:
            S.op("pool", lambda e: e.memset(ST[:].rearrange("p a b c -> p (a b c)"), 0.0), writes=[bST[h][s] for h in range(8) for s in range(2)])

        for ti in range(ntiles):
            x_t, bx = xt[ti % 2], b_xt[ti % 2]
            if (not full) and ti % 4 == 0:
                S.dma("pool", snap[ti // 4].rearrange("p (h v) -> p h v", h=8), ST[:, :, 0, :], reads=[bST[h][0] for h in range(8)])
            row0 = (m * 4 + ti) * 128 if full else ti * 128
            S.dma("sp", x_t[:], x_dram[row0:row0 + 128, :], writes=[bx])
            nt.normT(x_t[:], bx, GE[:], SH[:], b_mod, hnT[:], b_hnT)
            H2 = [slice(0, 512), slice(512, 1024)]
            for nb in range(2):
                pf, bpf = proj(1, nb)
                S.op("act", lambda e, pf=pf, nb=nb: e.activation(out=R[0][:, H2[nb]], in_=pf[:], func=AF.Sigmoid), reads=[bpf], writes=[bR[0]])
            S.op("dve", lambda e: e.tensor_tensor(out=R[0][:], in0=R[0][:], in1=OML[:], op=ALU.mult), reads=[bR[0], b_mod], writes=[bR[0]])
            S.op("pool", lambda e: e.tensor_tensor(out=R[0][:], in0=R[0][:], in1=LB[:], op=ALU.add), reads=[bR[0], b_mod], writes=[bR[0]])
            S.op("act", lambda e: e.activation(out=R[1][:], in_=R[0][:], func=AF.Ln), reads=[bR[0]], writes=[bR[1]])
            S.op("pool", lambda e: e.tensor_scalar(out=R[2][:], in0=R[0][:], scalar1=-1.0, scalar2=1.0, op0=ALU.mult, op1=ALU.add), reads=[bR[0]], writes=[bR[2]])
            for nb in range(2):
                pc, bpc = nextpj()
                S.op("pe", lambda e, pc=pc, nb=nb: e.matmul(pc[:], lhsT=BT, rhs=R[1][:, H2[nb]], start=True, stop=True),
                     reads=[b_hc, bR[1]], writes=[bpc])
                if full:
                    S.op("act", lambda e, pc=pc, nb=nb: e.activation(out=R[0][:, H2[nb]], in_=pc[:], func=AF.Exp), reads=[bpc], writes=[bR[0]])
                    S.op("act", lambda e, pc=pc, nb=nb: e.activation(out=R[3][:, H2[nb]], in_=pc[:], func=AF.Exp, scale=-1.0), reads=[bpc], writes=[bR[3]])
            for nb in range(2):
                pr, bpr = nextpj()
                S.op("pe", lambda e, pr=pr, nb=nb: e.matmul(pr[:], lhsT=(RT if full else RTF), rhs=R[1][:, H2[nb]], start=True, stop=True),
                     reads=[b_hc, bR[1]], writes=[bpr])
                S.op("act", lambda e, pr=pr, nb=nb: e.activation(out=R[4][:, H2[nb]], in_=pr[:], func=AF.Exp), reads=[bpr], writes=[bR[4]])
            for h in range(8):
                S.op("pe", lambda e, h=h: e.matmul(PC[:, 384 + h * 8:384 + (h + 1) * 8], lhsT=R[1][:, h * 128:(h + 1) * 128], rhs=CI, start=True, stop=True),
                     reads=[b_hc, bR[1]], writes=[bPC]) if full else \
                    S.op("pe", lambda e, h=h: e.matmul(PC[:, 384 + h:385 + h], lhsT=R[1][:, h * 128:(h + 1) * 128], rhs=ONE, start=True, stop=True),
                         reads=[b_hc, bR[1]], writes=[bPC])
            if full:
                S.op("act", lambda e: e.activation(out=EDEC[:], in_=PC[:, 384:448], func=AF.Exp), reads=[bPC], writes=[bEDEC])
            else:
                S.op("act", lambda e: e.activation(out=EDEC[:, 0:8], in_=PC[:, 384:392], func=AF.Exp), reads=[bPC], writes=[bEDEC])
            S.op("dve", lambda e: e.tensor_tensor(out=R[4][:], in0=R[4][:], in1=R[2][:], op=ALU.mult), reads=[bR[4], bR[2]], writes=[bR[4]])
            if full:
                S.op("dve", lambda e: e.tensor_tensor(out=R[3][:], in0=R[3][:], in1=R[2][:], op=ALU.mult), reads=[bR[3], bR[2]], writes=[bR[3]])
            for nb in range(2):
                pi, bpi = proj(2, nb)
                S.op("act", lambda e, pi=pi, nb=nb: e.copy(out=R[1][:, H2[nb]], in_=pi[:]), reads=[bpi], writes=[bR[1]])
            if full:
                for nb in range(2):
                    pq, bpq = proj(0, nb)
                    S.op("act", lambda e, pq=pq, nb=nb: e.activation(out=R[2][:, H2[nb]], in_=pq[:], func=AF.Silu), reads=[bpq], writes=[bR[2]])
                S.op("dve", lambda e: e.tensor_tensor(out=R[0][:], in0=R[0][:], in1=R[2][:], op=ALU.mult), reads=[bR[0], bR[2]], writes=[bR[0]])
                for nb in range(2):
                    pg, bpg = proj(3, nb)
                    S.op("act", lambda e, pg=pg, nb=nb: e.activation(out=R[2][:, H2[nb]], in_=pg[:], func=AF.Silu), reads=[bpg], writes=[bR[2]])
            for h in range(8):
                j = h % 2
                hs = slice(h * 128, (h + 1) * 128)
                if full:
                    S.op("pe", lambda e, hs=hs: e.transpose(PC[:, 0:128], R[0][:, hs], consts["identf"][:]), reads=[bR[0], consts["b_ident"]], writes=[bPC])
                    S.op("pe", lambda e, hs=hs: e.transpose(PC[:, 128:256], R[3][:, hs], consts["identf"][:]), reads=[bR[3], consts["b_ident"]], writes=[bPC])
                    S.op("act", lambda e, j=j: e.copy(out=QKT[j][:].rearrange("p a b -> p (a b)"), in_=PC[:, 0:256]), reads=[bPC], writes=[bQKT[j]])
                    S.op("pe", lambda e, j=j: e.matmul(PC[:, 256:384], lhsT=QKT[j][:, 1, :], rhs=QKT[j][:, 0, :], start=True, stop=True), reads=[bQKT[j]], writes=[bPC])
                    S.op("dve", lambda e, j=j: e.tensor_tensor(out=SCM[j][:], in0=PC[:, 256:384], in1=BT, op=ALU.mult), reads=[bPC, b_hc], writes=[bSCM[j]])
                    S.op("pool", lambda e, j=j: e.tensor_tensor(out=QDM[:], in0=QKT[j][:, 0, :].unsqueeze(1).to_broadcast([128, 8, 128]), in1=CHM[:], op=ALU.mult),
                         reads=[bQKT[j], b_hc], writes=[bQDM])
                if not full:
                    k = h % 2
                    S.op("pe", lambda e, k=k, hs=hs: e.matmul(PKV[k][:, 0:128], lhsT=R[4][:, hs], rhs=R[1][:, hs], start=True, stop=True),
                         reads=[bR[4], bR[1]], writes=[bPKV[k]])
                    S.op("dve", lambda e, k=k, h=h: e.scalar_tensor_tensor(
                        out=ST[:, h, 0, :], in0=ST[:, h, 0, :], scalar=EDEC[:, h:h + 1], in1=PKV[k][:, 0:128], op0=ALU.mult, op1=ALU.add),
                        reads=[bST[h][0], bEDEC, bPKV[k]], writes=[bST[h][0]])
                    continue
                S.op("pool", lambda e, hs=hs: e.tensor_tensor(out=KLM[:], in0=R[4][:, hs].unsqueeze(1).to_broadcast([128, 8, 128]),
                                                              in1=CI.unsqueeze(2).to_broadcast([128, 8, 128]), op=ALU.mult),
                     reads=[bR[4], b_hc], writes=[bKLM])
                if full:
                    S.op("pe", lambda e, j=j, hs=hs: e.matmul(PO[:, 0:128], lhsT=SCM[j][:], rhs=R[1][:, hs], start=True, stop=False),
                         reads=[bSCM[j], bR[1]], writes=[bPO])
                for c in range(8):
                    k = c % 2
                    s_old, s_new = c % 2, (c + 1) % 2
                    S.op("pe", lambda e, c=c, k=k, hs=hs: e.matmul(PKV[k][:, 0:128], lhsT=KLM[:, c, :], rhs=R[1][:, hs], start=True, stop=True),
                         reads=[bKLM, bR[1]], writes=[bPKV[k]])
                    if full:
                        S.op("pe", lambda e, c=c, h=h, s_old=s_old: e.matmul(PO[:, 0:128], lhsT=QDM[:, c, :], rhs=ST[:, h, s_old, :],
                                                                            start=False, stop=(c == 7)),
                             reads=[bQDM, bST[h][s_old]], writes=[bPO])
                    S.op("dve", lambda e, c=c, k=k, h=h, s_old=s_old, s_new=s_new: e.scalar_tensor_tensor(
                        out=ST[:, h, s_new, :], in0=ST[:, h, s_old, :], scalar=EDEC[:, h * 8 + c:h * 8 + c + 1], in1=PKV[k][:, 0:128], op0=ALU.mult, op1=ALU.add),
                        reads=[bST[h][s_old], bEDEC, bPKV[k]], writes=[bST[h][s_new]])
                if full:
                    S.op("act", lambda e, hs=hs: e.copy(out=R[5][:, hs], in_=PO[:, 0:128]), reads=[bPO], writes=[bR[5]])
            if full:
                S.op("dve", lambda e: e.tensor_tensor(out=R[3][:], in0=R[5][:], in1=R[5][:], op=ALU.mult), reads=[bR[5]], writes=[bR[3]])
                S.op("dve", lambda e: e.reduce_sum(out=ss8[:, 0:8], in_=R[3][:].rearrange("p (h v) -> p h v", h=8), axis=AX.X), reads=[bR[3]], writes=[b_ss8])
                emit_rstd(cx, ss8[:, 0:8], ss8[:, 8:16], 1.0 / 128, b_ss8, b_ss8)
                S.op("dve", lambda e: e.tensor_tensor(out=R[5][:].rearrange("p (h v) -> p h v", h=8), in0=R[5][:].rearrange("p (h v) -> p h v", h=8),
                                                      in1=ss8[:, 8:16].unsqueeze(2).to_broadcast([128, 8, 128]), op=ALU.mult),
                     reads=[bR[5], b_ss8], writes=[bR[5]])
                S.op("pool", lambda e: e.tensor_tensor(out=OGb[:], in0=R[5][:], in1=R[2][:], op=ALU.mult), reads=[bR[5], bR[2]], writes=[b_og])
                nt.transpose(OGb, b_og, ogT[:], b_ogT)
                for nb in range(2):
                    pm, bpm = nextpj()
                    for kc in range(8):
                        S.op("pe", lambda e, pm=pm, nb=nb, kc=kc: e.matmul(pm[:], lhsT=ogT[:, kc, :], rhs=wout[:, kc, nb * 512:(nb + 1) * 512],
                                                                          start=(kc == 0), stop=(kc == 7)),
                             reads=[b_ogT, b_wout], writes=[bpm])
                    S.op("dve", lambda e, pm=pm, x_t=x_t, ti=ti, nb=nb: e.tensor_tensor(out=HM[:, ti, H2[nb]], in0=pm[:], in1=x_t[:, H2[nb]], op=ALU.add),
                         reads=[bpm, bx], writes=[bHM[ti]])
        if (not full) and ntiles % 4 == 0:
            S.dma("pool", snap[ntiles // 4].rearrange("p (h v) -> p h v", h=8), ST[:, :, 0, :], reads=[bST[h][0] for h in range(8)])
        S.barrier()


def attn_consts_host(r):
    j = np.arange(128)
    trin = -(j[:, None] >= j[None, :]).astype(np.float32)
    onesn = -np.ones((128, 128), np.float32)
    ac = np.concatenate([trin, onesn], axis=1).astype(ml_dtypes.bfloat16)
    k = np.arange(16)
    kp = k[None, :, None] * 128 + j[:, None, None]
    qp = 4 * r * 128 + np.arange(512)[None, None, :]
    mask = (kp < qp).astype(np.float32).astype(ml_dtypes.bfloat16)
    return ac, mask


def emit_attn(cx, consts, HM, bHM, m, mod_dram, c_sh, c_ge, c_g, wq_dram, wo_dram, KT_dram, V_dram, ac_dram, mask_dram):
    S = cx.S
    NB = 16 * (m + 1)
    with ExitStack() as es:
        SH = cx.sb(es, [128, 1024]); GE = cx.sb(es, [128, 1024]); b_mod = S.buf("amod")
        S.dma("sp", SH[:], mod_dram[:, c_sh:c_sh + 1024], writes=[b_mod])
        S.dma("sp", GE[:], mod_dram[:, c_ge:c_ge + 1024], writes=[b_mod])
        AC = cx.sb(es, [128, 256], BF16); MASK = cx.sb(es, [128, 16, 512], BF16); b_ac = S.buf("ac")
        S.dma("sp", AC[:], ac_dram, writes=[b_ac])
        S.dma("sp", MASK[:], mask_dram, writes=[b_ac])
        TRIN, ONESN = AC[:, 0:128], AC[:, 128:256]
        wq = cx.sb(es, [128, 8, 1024], BF16); b_wq = S.buf("awq")
        wo = cx.sb(es, [128, 8, 1024], BF16); b_wo = S.buf("awo")
        load_w(cx, wq, wq_dram, b_wq, 1024)
        load_w(cx, wo, wo_dram, b_wo, 1024)
        nt = NormT(cx, es, consts)
        xnT = cx.sb(es, [128, 8, 512], BF16); b_xnT = S.bufs(4, "axnT")
        QT = cx.sb(es, [128, 8, 512], BF16); bQT = S.bufs(8, "QT")
        NKV = 3
        KTc = [cx.sb(es, [128, 2048], BF16) for _ in range(NKV)]; bKT = S.bufs(NKV, "KTc")
        Vc = [cx.sb(es, [128, 16, 128], BF16) for _ in range(NKV)]; bV = S.bufs(NKV, "Vc")
        NR = 3
        E = [cx.sb(es, [128, 512]) for _ in range(NR)]; bE = S.bufs(NR, "E")
        LK = [cx.sb(es, [128, 512], BF16) for _ in range(NR)]; bLK = S.bufs(NR, "LK")
        LKS = [cx.sb(es, [128, 512], BF16) for _ in range(NR)]; bLKS = S.bufs(NR, "LKS")
        A = [cx.sb(es, [128, 512], BF16) for _ in range(NR)]; bA = S.bufs(NR, "A")
        OT = cx.sb(es, [128, 8, 512], BF16); bOT = S.bufs(8, "OT")
        PZ = [cx.ps(es, [128, 512]) for _ in range(3)]; bPZ = S.bufs(3, "PZ")
        PS = [cx.ps(es, [128, 512]) for _ in range(2)]; bPS = S.bufs(2, "PS")
        POUT = [cx.ps(es, [128, 512]) for _ in range(2)]; bPOUT = S.bufs(2, "POUT")
        for tt in range(4):
            nt.normT(HM[:, tt, :], bHM[tt], GE[:], SH[:], b_mod, xnT[:, :, tt * 128:(tt + 1) * 128], b_xnT[tt])
        sc = 1.0 / math.sqrt(128.0)
        for h in range(8):
            pz, bpz = (PZ[h % 3], bPZ[h % 3])
            for kc in range(8):
                S.op("pe", lambda e, h=h, kc=kc, pz=pz: e.matmul(pz[:], lhsT=wq[:, kc, h * 128:(h + 1) * 128], rhs=xnT[:, kc, :], start=(kc == 0), stop=(kc == 7)),
                     reads=[b_wq] + b_xnT, writes=[bpz])
            S.op("act", lambda e, h=h, pz=pz: e.activation(out=QT[:, h, :], in_=pz[:], func=AF.Copy, scale=sc), reads=[bpz], writes=[bQT[h]])
        items = []
        ld = 0
        for h in range(8):
            for ci in range(m, -1, -1):
                slot = ld % NKV
                ld += 1
                for kk in range(15, -1, -1):
                    items.append(dict(h=h, ci=ci, kk=kk, slot=slot, load=(kk == 15), first=(ci == m and kk == 15), last=(ci == 0 and kk == 0),
                                      masked=(ci == m)))
        for i, it in enumerate(items):
            it["i"] = i

        def stageA(it):
            i, h, kk, slot = it["i"], it["h"], it["kk"], it["slot"]
            kt, bkt, vc, bvc = KTc[slot], bKT[slot], Vc[slot], bV[slot]
            if it["load"]:
                S.dma("sp", kt[:], KT_dram[h, :, it["ci"] * 2048:(it["ci"] + 1) * 2048], writes=[bkt])
                S.dma("pool", vc[:], V_dram[h, :, it["ci"] * 16:(it["ci"] + 1) * 16, :], writes=[bvc])
            z, r = i % 3, i % NR
            ks = slice(kk * 128, (kk + 1) * 128)
            S.op("pe", lambda e: e.matmul(PZ[z][:], lhsT=kt[:, ks], rhs=QT[:, h, :], start=True, stop=True), reads=[bkt, bQT[h]], writes=[bPZ[z]])
            S.op("act", lambda e: e.activation(out=E[r][:], in_=PZ[z][:], func=AF.Exp), reads=[bPZ[z]], writes=[bE[r]])
            S.op("act", lambda e: e.activation(out=LK[r][:], in_=E[r][:], func=AF.Ln, bias=1.0, scale=1.0), reads=[bE[r]], writes=[bLK[r]])
            if it["masked"]:
                S.op("dve", lambda e: e.tensor_tensor(out=LK[r][:], in0=LK[r][:], in1=MASK[:, kk, :], op=ALU.mult), reads=[bLK[r], b_ac], writes=[bLK[r]])

        def stageB(it):
            i, h, kk, slot = it["i"], it["h"], it["kk"], it["slot"]
            kt, bkt = KTc[slot], bKT[slot]
            r, p = i % NR, i % 2
            nx = (i + 1) % NR
            first, last = it["first"], it["last"]
            ks = slice(kk * 128, (kk + 1) * 128)
            S.op("pe", lambda e: e.matmul(PS[p][:], lhsT=kt[:, ks], rhs=QT[:, h, :], start=True, stop=False), reads=[bkt, bQT[h]], writes=[bPS[p]])
            S.op("pe", lambda e: e.matmul(PS[p][:], lhsT=TRIN, rhs=LK[r][:], start=False, stop=first), reads=[b_ac, bLK[r]], writes=[bPS[p]])
            if not first:
                S.op("pe", lambda e: e.matmul(PS[p][:], lhsT=ONESN, rhs=LKS[r][:], start=False, stop=True), reads=[b_ac, bLKS[r]], writes=[bPS[p]])
            S.op("act", lambda e: e.activation(out=A[r][:], in_=PS[p][:], func=AF.Exp), reads=[bPS[p]], writes=[bA[r]])
            if it["masked"]:
                S.op("pool", lambda e: e.tensor_tensor(out=A[r][:], in0=A[r][:], in1=MASK[:, kk, :], op=ALU.mult), reads=[bA[r], b_ac], writes=[bA[r]])
            if first:
                S.op("dve", lambda e: e.tensor_copy(out=LKS[nx][:], in_=LK[r][:]), reads=[bLK[r]], writes=[bLKS[nx]])
            elif not last:
                S.op("dve", lambda e: e.tensor_tensor(out=LKS[nx][:], in0=LKS[r][:], in1=LK[r][:], op=ALU.add), reads=[bLK[r], bLKS[r]], writes=[bLKS[nx]])

        def stageC(it):
            i, h, kk, slot = it["i"], it["h"], it["kk"], it["slot"]
            vc, bvc = Vc[slot], bV[slot]
            r = i % NR
            po, bpo = POUT[h % 2], bPOUT[h % 2]
            S.op("pe", lambda e: e.matmul(po[:], lhsT=vc[:, kk, :], rhs=A[r][:], start=it["first"], stop=it["last"]), reads=[bvc, bA[r]], writes=[bpo])
            if it["last"]:
                S.op("dve", lambda e: e.tensor_copy(out=OT[:, h, :], in_=po[:]), reads=[bpo], writes=[bOT[h]])

        n = len(items)
        for k in range(n + 2):
            if k < n:
                stageA(items[k])
            if 0 <= k - 1 < n:
                stageB(items[k - 1])
            if 0 <= k - 2 < n:
                stageC(items[k - 2])
        for tt in range(4):
            for nb in range(2):
                S_ps, b_ps = PZ[nb], bPZ[nb]
                for h in range(8):
                    S.op("pe", lambda e, tt=tt, nb=nb, h=h, S_ps=S_ps: e.matmul(S_ps[:], lhsT=OT[:, h, tt * 128:(tt + 1) * 128], rhs=wo[:, h, nb * 512:(nb + 1) * 512],
                                                                             start=(h == 0), stop=(h == 7)),
                         reads=[bOT[h], b_wo], writes=[b_ps])
                S.op("dve", lambda e, tt=tt, nb=nb, S_ps=S_ps: e.tensor_tensor(out=HM[:, tt, nb * 512:(nb + 1) * 512], in0=HM[:, tt, nb * 512:(nb + 1) * 512], in1=S_ps[:], op=ALU.add),
                     reads=[b_ps, bHM[tt]], writes=[bHM[tt]])
        S.barrier()


def emit_kv(cx, consts, HM, bHM, m, mod_dram, c_sh, c_ge, kvw_dram, KTo, Vo):
    S = cx.S
    with ExitStack() as es:
        SH = cx.sb(es, [128, 1024]); GE = cx.sb(es, [128, 1024]); b_mod = S.buf("kmod")
        S.dma("sp", SH[:], mod_dram[:, c_sh:c_sh + 1024], writes=[b_mod])
        S.dma("sp", GE[:], mod_dram[:, c_ge:c_ge + 1024], writes=[b_mod])
        kvw = cx.sb(es, [128, 8, 2048], BF16); b_kvw = S.buf("kvw")
        load_w(cx, kvw, kvw_dram, b_kvw, 2048)
        nt = NormT(cx, es, consts)
        xnT = cx.sb(es, [128, 8, 512], BF16); b_xnT = S.bufs(4, "kxnT")
        KTs = cx.sb(es, [128, 8, 512], BF16); bKTs = S.buf("KTs")
        Vs = [cx.sb(es, [128, 1024], BF16) for _ in range(2)]; bVs = S.bufs(2, "Vs")
        PK = [cx.ps(es, [128, 512]) for _ in range(4)]; bPK = S.bufs(4, "PK")
        for tt in range(4):
            nt.normT(HM[:, tt, :], bHM[tt], GE[:], SH[:], b_mod, xnT[:, :, tt * 128:(tt + 1) * 128], b_xnT[tt])
        for h in range(8):
            pk, bpk = PK[h % 4], bPK[h % 4]
            for kc in range(8):
                S.op("pe", lambda e, h=h, kc=kc, pk=pk: e.matmul(pk[:], lhsT=kvw[:, kc, h * 128:(h + 1) * 128], rhs=xnT[:, kc, :], start=(kc == 0), stop=(kc == 7)),
                     reads=[b_kvw] + b_xnT, writes=[bpk])
            S.op("act", lambda e, h=h, pk=pk: e.copy(out=KTs[:, h, :], in_=pk[:]), reads=[bpk], writes=[bKTs])
        S.dma("sp", KTo[m].rearrange("h d t -> d h t"), KTs[:], reads=[bKTs])
        for tt in range(4):
            vs, bvs = Vs[tt % 2], bVs[tt % 2]
            for nb in range(2):
                pk, bpk = PK[(tt * 2 + nb) % 4], bPK[(tt * 2 + nb) % 4]
                for kc in range(8):
                    S.op("pe", lambda e, tt=tt, nb=nb, kc=kc, pk=pk: e.matmul(pk[:], lhsT=xnT[:, kc, tt * 128:(tt + 1) * 128],
                                                                             rhs=kvw[:, kc, 1024 + nb * 512:1024 + (nb + 1) * 512], start=(kc == 0), stop=(kc == 7)),
                         reads=[b_kvw, b_xnT[tt]], writes=[bpk])
                S.op("dve", lambda e, nb=nb, pk=pk, vs=vs: e.tensor_copy(out=vs[:, nb * 512:(nb + 1) * 512], in_=pk[:]), reads=[bpk], writes=[bvs])
            S.dma("sp", Vo[m * 512 + tt * 128:m * 512 + (tt + 1) * 128, :], vs[:], reads=[bvs])
        S.barrier()


def emit_store(cx, HM, bHM, m, out):
    S = cx.S
    for tt in range(4):
        S.dma("sp", out[m * 512 + tt * 128:m * 512 + (tt + 1) * 128, :], HM[:, tt, :], reads=[bHM[tt]])


def emit_final(cx, consts, HM, bHM, m, g_dram, out):
    S = cx.S
    with ExitStack() as es:
        G = cx.sb(es, [128, 1024]); bg = S.buf("fg")
        S.dma("pool", G[:], g_dram[0:1, :].partition_broadcast(128), writes=[bg])
        junk = cx.sb(es, [128, 1024], BF16); st = cx.sb(es, [128, 8]); bj, bst = S.buf(), S.buf()
        o = [cx.sb(es, [128, 1024]) for _ in range(2)]; bo = S.bufs(2, "fo")
        for tt in range(4):
            S.op("act", lambda e, tt=tt: e.activation(out=junk[:], in_=HM[:, tt, :], func=AF.Square, accum_out=st[:, 2 * tt:2 * tt + 1]),
                 reads=[bHM[tt]], writes=[bj, bst])
            emit_rstd(cx, st[:, 2 * tt:2 * tt + 1], st[:, 2 * tt + 1:2 * tt + 2], 1.0 / D, bst, bst)
            S.op("dve", lambda e, tt=tt: e.scalar_tensor_tensor(out=o[tt % 2][:], in0=HM[:, tt, :], scalar=st[:, 2 * tt + 1:2 * tt + 2], in1=G[:], op0=ALU.mult, op1=ALU.mult),
                 reads=[bHM[tt], bst, bg], writes=[bo[tt % 2]])
            S.dma("sp", out[m * 512 + tt * 128:m * 512 + (tt + 1) * 128, :], o[tt % 2][:], reads=[bo[tt % 2]])
        S.barrier()


def build_l1(NM, do_peer=True):
    SQ = 2048 * NM
    NQG = 4 * NM
    nc = bass.Bass("TRN2", target_bir_lowering=False)
    with ExitStack() as es:
        cx = Ctx(nc, es)
        S = cx.S
        I = lambda n, s, dt=F32: nc.dram_tensor(n, list(s), dt, kind="ExternalInput").ap()
        O = lambda n, s, dt=F32: nc.dram_tensor(n, list(s), dt, kind="ExternalOutput").ap()
        xb = I("xb", [SQ, D]); xo = I("xo", [NM * 512, D]); cT = I("cT", [128, 8])
        ada_w = I("ada_w", [D, 6 * D]); ada_b = I("ada_b", [1, 6 * D]); kva_w = I("kva_w", [D, 2 * D]); kva_b = I("kva_b", [1, 2 * D])
        gmix = I("gmix", [1, D]); gffn = I("gffn", [1, D]); gkv = I("gkv", [1, D])
        win = I("win", [D, 4 * D]); wout = I("wout", [D, D]); ongT = I("ongT", [128, 8]); lbl = I("lbl", [2, D])
        hcA = I("hcA", [128, 393]); hcB = I("hcB", [1, 1024]); oh = I("oh", [1, 4])
        kvw = I("kvw", [D, 2 * D]); pwq = I("pwq", [D, D]); skT = I("skT", [8, 128, 128])
        uT = I("uT", [D, NEXP]); v = I("v", [NEXP, D])
        h1 = O("h1", [NM * 512, D]); KTo = O("KTo", [NM, 8, 128, 512], BF16); Vo = O("Vo", [NM * 512, D], BF16)
        mod = cx.dram("mod", [128, 8 * D], F32)
        snapt = cx.dram("snap", [NQG, 128, D], F32)
        snap = [snapt[g] for g in range(NQG)]
        uTb = cx.dram("uTb", [D, NEXP], BF16); vb = cx.dram("vb", [NEXP, D], BF16)
        consts = make_consts(cx, es)
        emit_mod(cx, cT, [(ada_w, ada_b, 0, 6 * D), (kva_w, kva_b, 6 * D, 2 * D)], mod)
        emit_geff(cx, mod, 1 * D, gmix)
        emit_geff(cx, mod, 4 * D, gffn)
        emit_geff(cx, mod, 7 * D, gkv)
        if do_peer:
            emit_precast(cx, uT, uTb, D, NEXP)
            emit_precast(cx, v, vb, NEXP, D)
        winb = cx.dram("winb", [D, 4 * D], BF16); woutb = cx.dram("woutb", [D, D], BF16)
        kvwb = cx.dram("kvwb", [D, 2 * D], BF16); pwqb = cx.dram("pwqb", [D, D], BF16)
        emit_precast(cx, win, winb, D, 4 * D)
        emit_precast(cx, kvw, kvwb, D, 2 * D)
        emit_precast(cx, pwq, pwqb, D, D)
        emit_fold_cast(cx, wout, woutb, rowT_dram=ongT, col_dram=mod, col0=2 * D)
        HM = cx.sb(es, [128, 4, D]); bHM = S.bufs(4, "HM")
        emit_hgrn(cx, consts, HM, bHM, "state", xb, 4 * (NQG - 1), snap, mod, 0, D, 2 * D, winb, woutb, ongT, lbl, hcA, hcB)
        for m in range(NM):
            emit_hgrn(cx, consts, HM, bHM, "full", xo, 4, snap, mod, 0, D, 2 * D, winb, woutb, ongT, lbl, hcA, hcB, onehot_dram=oh, m=m)
            if do_peer:
                emit_peer(cx, consts, HM, bHM, mod, 3 * D, 4 * D, 5 * D, pwqb, skT, uTb, vb)
            emit_store(cx, HM, bHM, m, h1)
            emit_kv(cx, consts, HM, bHM, m, mod, 6 * D, 7 * D, kvwb, KTo, Vo)
        S.barrier()
        S.emit()
    return nc


def build_l2(NM, do_peer=True):
    SQ = 2048 * NM
    nc = bass.Bass("TRN2", target_bir_lowering=False)
    with ExitStack() as es:
        cx = Ctx(nc, es)
        S = cx.S
        I = lambda n, s, dt=F32: nc.dram_tensor(n, list(s), dt, kind="ExternalInput").ap()
        O = lambda n, s, dt=F32: nc.dram_tensor(n, list(s), dt, kind="ExternalOutput").ap()
        h1 = I("h1", [NM * 512, D]); cT = I("cT", [128, 8])
        ada_w = I("ada_w", [D, 6 * D]); ada_b = I("ada_b", [1, 6 * D])
        gmix = I("gmix", [1, D]); gffn = I("gffn", [1, D]); gfin = I("gfin", [1, D])
        sbwq = I("sbwq", [D, D]); sbwo = I("sbwo", [D, D])
        KT = I("KT", [8, 128, SQ], BF16); V = I("V", [8, 128, SQ // 128, 128], BF16)
        ac = I("ac", [128, 256], BF16); mask = I("mask", [128, 16, 512], BF16)
        pwq = I("pwq", [D, D]); skT = I("skT", [8, 128, 128]); uT = I("uT", [D, NEXP]); v = I("v", [NEXP, D])
        out = O("out", [NM * 512, D])
        mod = cx.dram("mod", [128, 6 * D], F32)
        uTb = cx.dram("uTb", [D, NEXP], BF16); vb = cx.dram("vb", [NEXP, D], BF16)
        consts = make_consts(cx, es)
        emit_mod(cx, cT, [(ada_w, ada_b, 0, 6 * D)], mod)
        emit_geff(cx, mod, 1 * D, gmix)
        emit_geff(cx, mod, 4 * D, gffn)
        if do_peer:
            emit_precast(cx, uT, uTb, D, NEXP)
            emit_precast(cx, v, vb, NEXP, D)
        sbwqb = cx.dram("sbwqb", [D, D], BF16); sbwob = cx.dram("sbwob", [D, D], BF16); pwqb = cx.dram("pwqb", [D, D], BF16)
        emit_precast(cx, sbwq, sbwqb, D, D)
        emit_precast(cx, pwq, pwqb, D, D)
        emit_fold_cast(cx, sbwo, sbwob, rowT_dram=None, col_dram=mod, col0=2 * D)
        HM = cx.sb(es, [128, 4, D]); bHM = S.bufs(4, "HM")
        for m in range(NM):
            for tt in range(4):
                S.dma("sp", HM[:, tt, :], h1[m * 512 + tt * 128:m * 512 + (tt + 1) * 128, :], writes=[bHM[tt]])
            emit_attn(cx, consts, HM, bHM, m, mod, 0, D, 2 * D, sbwqb, sbwob, KT, V, ac, mask)
            if do_peer:
                emit_peer(cx, consts, HM, bHM, mod, 3 * D, 4 * D, 5 * D, pwqb, skT, uTb, vb)
            emit_final(cx, consts, HM, bHM, m, gfin, out)
        S.barrier()
        S.emit()
    return nc


def run_model(inp, NM, do_peer=True, runner=None):
    f32 = lambda a: np.ascontiguousarray(np.asarray(a, dtype=np.float32))
    x = f32(inp["x"]); c = f32(inp["c"])
    B = x.shape[0]
    SQ = 2048 * NM
    assert x.shape == (B, SQ, D) and B == 2
    ncores = 8
    if runner is None:
        runner = lambda nc, maps: run_bass_kernel_spmd(nc, maps, core_ids=list(range(len(maps)))).results
    hcA, hcB = hgrn_consts_host()
    row = lambda a: f32(a).reshape(1, -1)
    colT = lambda a: np.ascontiguousarray(f32(a).reshape(8, 128).T)
    own = lambda b, r: np.concatenate([np.arange((4 * m + r) * 512, (4 * m + r + 1) * 512) for m in range(NM)])

    def peer_w(l):
        sk = f32(inp["peer_subkeys"][l]).reshape(8, 128, 128)
        return {"pwq": f32(inp["peer_w_q"][l]), "skT": np.ascontiguousarray(sk.transpose(0, 2, 1)),
                "uT": np.ascontiguousarray(f32(inp["peer_u"][l]).T), "v": f32(inp["peer_v"][l])}

    pw0 = peer_w(0)
    shared1 = {"ada_w": f32(inp["ada_w"][0]), "ada_b": row(inp["ada_b"][0]), "kva_w": f32(inp["kv_ada_w"]), "kva_b": row(inp["kv_ada_b"]),
               "gmix": row(inp["norm_mix_g"][0]), "gffn": row(inp["norm_ffn_g"][0]), "gkv": row(inp["kv_norm_g"]),
               "win": f32(inp["hgrn_w_in"][0]), "wout": f32(inp["hgrn_w_out"][0]), "ongT": colT(inp["hgrn_onorm_g"][0]),
               "lbl": f32(inp["hgrn_lb_logits"]), "hcA": hcA, "hcB": hcB, "kvw": f32(inp["kv_w"]), **pw0}
    maps = []
    for core in range(ncores):
        b, r = core // 4, core % 4
        oh = np.zeros((1, 4), np.float32); oh[0, r] = 1.0
        maps.append({"xb": x[b], "xo": np.ascontiguousarray(x[b][own(b, r)]), "cT": colT(c[b]), "oh": oh, **shared1})
    nc1 = build_l1(NM, do_peer)
    res1 = runner(nc1, maps)
    del maps, shared1, pw0
    KTf = np.zeros((B, 8, 128, SQ), ml_dtypes.bfloat16)
    Vf = np.zeros((B, SQ, D), ml_dtypes.bfloat16)
    for core in range(ncores):
        b, r = core // 4, core % 4
        kto = np.asarray(res1[core]["KTo"]); vo = np.asarray(res1[core]["Vo"])
        for m in range(NM):
            g = 4 * m + r
            KTf[b, :, :, g * 512:(g + 1) * 512] = kto[m]
            Vf[b, g * 512:(g + 1) * 512] = vo[m * 512:(m + 1) * 512]
    Vl = np.ascontiguousarray(Vf.reshape(B, SQ // 128, 128, 8, 128).transpose(0, 3, 2, 1, 4))
    pw1 = peer_w(1)
    shared2 = {"ada_w": f32(inp["ada_w"][1]), "ada_b": row(inp["ada_b"][1]), "gmix": row(inp["norm_mix_g"][1]), "gffn": row(inp["norm_ffn_g"][1]),
               "gfin": row(inp["final_norm_g"]), "sbwq": f32(inp["sb_w_q"][0]), "sbwo": f32(inp["sb_w_out"][0]), **pw1}
    maps = []
    for core in range(ncores):
        b, r = core // 4, core % 4
        ac, mask = attn_consts_host(r)
        maps.append({"h1": np.asarray(res1[core]["h1"]), "cT": colT(c[b]), "KT": KTf[b], "V": Vl[b], "ac": ac, "mask": mask, **shared2})
    nc2 = build_l2(NM, do_peer)
    res2 = runner(nc2, maps)
    out = np.zeros((B, SQ, D), np.float32)
    for core in range(ncores):
        b, r = core // 4, core % 4
        out[b, own(b, r)] = np.asarray(res2[core]["out"])
    return out


def kernel(**inputs):
    return run_model(inputs, 8)
```

```python
from contextlib import ExitStack
import math
import numpy as np
import ml_dtypes
import concourse.bass as bass
import concourse.mybir as mybir
from concourse.bass_utils import run_bass_kernel_spmd

F32 = mybir.dt.float32
BF16 = mybir.dt.bfloat16
ALU = mybir.AluOpType
AF = mybir.ActivationFunctionType
AX = mybir.AxisListType


class Buf:
    __slots__ = ("name", "w", "r")

    def __init__(self, name):
        self.name = name
        self.w = None
        self.r = []


class Sched:
    ENGS = ("pe", "act", "dve", "pool", "sp")
    NDMA = 8
    ROT = 20000

    def __init__(self, nc, es):
        self.nc = nc
        self.streams = {e: [] for e in self.ENGS}
        self.sem = {}
        self.cnt = {}
        for e in self.ENGS:
            self.sem[e] = es.enter_context(nc.semaphore("s_" + e))
            self.cnt[e] = 0
        self.dsem = {}
        self.dcnt = {}
        for e in ("sp", "act", "pool"):
            self.dsem[e] = [es.enter_context(nc.semaphore("d_%s%d" % (e, i))) for i in range(self.NDMA)]
            self.dcnt[e] = 0
        self.waited = {e: {} for e in self.ENGS}
        self.nbuf = 0
        self.es = es
        self.nrot = 0

    def buf(self, name=None):
        self.nbuf += 1
        return Buf(name or "b%d" % self.nbuf)

    def bufs(self, n, name="b"):
        return [self.buf("%s%d" % (name, i)) for i in range(n)]

    def _need(self, eng, reads, writes, same_ok):
        need = {}

        def add(tok):
            if tok is None:
                return
            sem, val, src = tok
            if same_ok and src == eng:
                return
            k = id(sem)
            if k not in need or need[k][1] < val:
                need[k] = (sem, val)

        for b in reads:
            add(b.w)
        for b in writes:
            add(b.w)
            for t in b.r:
                add(t)
        out = []
        wd = self.waited[eng]
        for k, (sem, val) in need.items():
            if wd.get(k, 0) >= val:
                continue
            wd[k] = val
            out.append((sem, val))
        return out

    def op(self, eng, fn, reads=(), writes=()):
        waits = self._need(eng, reads, writes, same_ok=(eng == "pe"))
        self.cnt[eng] += 1
        tok = (self.sem[eng], self.cnt[eng], eng)
        self.streams[eng].append((waits, fn, (self.sem[eng], 1)))
        for b in reads:
            b.r.append(tok)
        for b in writes:
            b.w = tok
            b.r = []
        return tok

    def dma(self, q, out, in_, reads=(), writes=(), **kw):
        j = self.dcnt[q]
        self.dcnt[q] += 1
        sem = self.dsem[q][j % self.NDMA]
        val = 16 * (j // self.NDMA + 1)
        waits = self._need(q, reads, writes, same_ok=False)
        if j >= self.NDMA:
            prev = val - 16
            wd = self.waited[q]
            if wd.get(id(sem), 0) < prev:
                wd[id(sem)] = prev
                waits.append((sem, prev))
        tok = (sem, val, "dma_" + q)
        self.streams[q].append((waits, lambda e: e.dma_start(out=out, in_=in_, **kw), (sem, 16)))
        for b in reads:
            b.r.append(tok)
        for b in writes:
            b.w = tok
            b.r = []
        return tok

    def coll(self, kind, groups, src, dst, reads=(), writes=()):
        q = "pool"
        j = self.dcnt[q]
        self.dcnt[q] += 1
        sem = self.dsem[q][j % self.NDMA]
        val = 16 * (j // self.NDMA + 1)
        waits = self._need(q, reads, writes, same_ok=False)
        if j >= self.NDMA:
            prev = val - 16
            wd = self.waited[q]
            if wd.get(id(sem), 0) < prev:
                wd[id(sem)] = prev
                waits.append((sem, prev))
        tok = (sem, val, "dma_" + q)
        self.streams[q].append((waits, lambda e: e.collective_compute(kind, ALU.bypass, groups, [src], [dst]), (sem, 16)))
        for b in reads:
            b.r.append(tok)
        for b in writes:
            b.w = tok
            b.r = []
        return tok

    def wait_all(self, eng, bufs):
        waits = self._need(eng, bufs, (), same_ok=False)
        self.streams[eng].append((waits, None, None))

    def barrier(self):
        toks = []
        for e in self.ENGS:
            if self.cnt[e]:
                toks.append((self.sem[e], self.cnt[e]))
        for q in self.dsem:
            j = self.dcnt[q]
            for s in range(min(j, self.NDMA)):
                last = ((j - 1 - s) // self.NDMA) if j - 1 >= s else -1
                uses = (j - s + self.NDMA - 1) // self.NDMA
                toks.append((self.dsem[q][s], 16 * uses))
        for e in self.ENGS:
            waits = []
            wd = self.waited[e]
            for sem, val in toks:
                if wd.get(id(sem), 0) >= val:
                    continue
                wd[id(sem)] = val
                waits.append((sem, val))
            if waits:
                self.streams[e].append((waits, None, None))
        for e in self.ENGS:
            if self.cnt[e] > self.ROT:
                self.sem[e] = self.es.enter_context(self.nc.semaphore("s_%s_%d" % (e, self.nrot)))
                self.nrot += 1
                self.cnt[e] = 0
        for q in self.dsem:
            if 16 * (self.dcnt[q] // self.NDMA + 1) > self.ROT:
                self.dsem[q] = [self.es.enter_context(self.nc.semaphore("d_%s%d_%d" % (q, i, self.nrot))) for i in range(self.NDMA)]
                self.nrot += 1
                self.dcnt[q] = 0

    def scatter(self, out, idx_ap, in_, reads=(), writes=()):
        q = "pool"
        j = self.dcnt[q]
        self.dcnt[q] += 1
        sem = self.dsem[q][j % self.NDMA]
        val = 16 * (j // self.NDMA + 1)
        waits = self._need(q, reads, writes, same_ok=False)
        if j >= self.NDMA:
            prev = val - 16
            wd = self.waited[q]
            if wd.get(id(sem), 0) < prev:
                wd[id(sem)] = prev
                waits.append((sem, prev))
        tok = (sem, val, "dma_" + q)
        self.streams[q].append((waits, lambda e: e.indirect_dma_start(out=out, out_offset=bass.IndirectOffsetOnAxis(ap=idx_ap, axis=0),
                                                                      in_=in_, in_offset=None, bounds_check=out.shape[0] - 1, oob_is_err=False), (sem, 16)))
        for b in reads:
            b.r.append(tok)
        for b in writes:
            b.w = tok
            b.r = []
        return tok

    def core_barrier(self):
        self.barrier()
        self.emit()
        self.streams = {e: [] for e in self.ENGS}
        self.nc.all_core_barrier()

    def emit(self):
        nc = self.nc
        handles = {"pe": "tensor", "act": "scalar", "dve": "vector", "pool": "gpsimd", "sp": "sync"}
        with nc.Block() as block:
            for e in self.ENGS:
                stream = self.streams[e]

                def body(eng, stream=stream):
                    for waits, fn, inc in stream:
                        for sem, val in waits:
                            eng.wait_ge(sem, val)
                        if fn is not None:
                            ins = fn(eng)
                            ins.then_inc(inc[0], inc[1])

                getattr(block, handles[e])(body)


D = 1024
NEXP = 16384
EPS = 1e-6


class Ctx:
    def __init__(self, nc, es):
        self.nc = nc
        self.es = es
        self.S = Sched(nc, es)
        self.n = 0

    def sb(self, es, shape, dt=F32, name=None):
        self.n += 1
        return es.enter_context(self.nc.sbuf_tensor(name or "t%d" % self.n, list(shape), dt))

    def ps(self, es, shape, dt=F32, name=None):
        self.n += 1
        return es.enter_context(self.nc.psum_tensor(name or "p%d" % self.n, list(shape), dt))

    def dram(self, name, shape, dt, kind="Internal"):
        return self.nc.dram_tensor(name, list(shape), dt, kind=kind).ap()


def make_consts(cx, es):
    S = cx.S
    c = {}
    identf = cx.sb(es, [128, 128], F32)
    identb = cx.sb(es, [128, 128], BF16)
    b = S.buf("ident")
    S.op("pool", lambda e: e.memset(identf[:], 1.0), writes=[b])
    S.op("pool", lambda e: e.affine_select(out=identf[:], in_=identf[:], pattern=[[-1, 128]],
                                           compare_op=ALU.is_equal, fill=0.0, base=0, channel_multiplier=1),
         reads=[b], writes=[b])
    S.op("dve", lambda e: e.tensor_copy(out=identb[:], in_=identf[:]), reads=[b], writes=[b])
    c["identf"], c["identb"], c["b_ident"] = identf, identb, b
    return c


def emit_mod(cx, cT_ap, specs, out_dram):
    S = cx.S
    with ExitStack() as es:
        cs = cx.sb(es, [128, 8])
        sg = cx.sb(es, [128, 8])
        CA = cx.sb(es, [128, 8, 128])
        wst = [cx.sb(es, [128, 2048]) for _ in range(2)]
        bwst = S.bufs(2, "wst")
        bias = cx.sb(es, [128, 2048])
        res = cx.sb(es, [128, 2048])
        pm = cx.ps(es, [128, 4, 512])
        b_cs, b_CA, b_bias, b_res, b_pm = S.buf(), S.buf(), S.buf(), S.buf(), S.buf()
        S.dma("sp", cs[:], cT_ap, writes=[b_cs])
        S.op("act", lambda e: e.activation(out=sg[:], in_=cs[:], func=AF.Sigmoid), reads=[b_cs], writes=[b_CA])
        S.op("dve", lambda e: e.tensor_tensor(out=cs[:], in0=cs[:], in1=sg[:], op=ALU.mult), reads=[b_cs, b_CA], writes=[b_cs])
        S.op("dve", lambda e: e.tensor_copy(out=CA[:], in_=cs[:].unsqueeze(2).to_broadcast([128, 8, 128])),
             reads=[b_cs], writes=[b_CA])
        it = 0
        for (W, B, col0, ncols) in specs:
            for cb in range(0, ncols, 2048):
                S.dma("pool", bias[:], B[0:1, cb:cb + 2048].partition_broadcast(128), writes=[b_bias])
                for kc in range(8):
                    w = wst[it % 2]
                    bw = bwst[it % 2]
                    it += 1
                    S.dma("sp", w[:], W[kc * 128:(kc + 1) * 128, cb:cb + 2048], writes=[bw])
                    for nb in range(4):
                        S.op("pe", lambda e, w=w, nb=nb, kc=kc: e.matmul(pm[:, nb, :], lhsT=CA[:, kc, :], rhs=w[:, nb * 512:(nb + 1) * 512],
                                                                        start=(kc == 0), stop=(kc == 7)),
                             reads=[b_CA, bw], writes=[b_pm])
                S.op("dve", lambda e: e.tensor_tensor(out=res[:], in0=pm[:].rearrange("p a b -> p (a b)"), in1=bias[:], op=ALU.add),
                     reads=[b_pm, b_bias], writes=[b_res])
                S.dma("sp", out_dram[:, col0 + cb:col0 + cb + 2048], res[:], reads=[b_res])
        S.barrier()


def emit_rstd(cx, ss_ap, rstd_ap, inv_n, b_ss, b_rstd):
    S = cx.S
    S.op("dve", lambda e: e.tensor_scalar(out=rstd_ap, in0=ss_ap, scalar1=inv_n, scalar2=EPS, op0=ALU.mult, op1=ALU.add),
         reads=[b_ss], writes=[b_rstd])
    S.op("act", lambda e: e.activation(out=rstd_ap, in_=rstd_ap, func=AF.Ln), reads=[b_rstd], writes=[b_rstd])
    S.op("act", lambda e: e.activation(out=rstd_ap, in_=rstd_ap, func=AF.Exp, scale=-0.5), reads=[b_rstd], writes=[b_rstd])


class NormT:
    def __init__(self, cx, es, consts, pt=None, b_pt=None):
        self.cx = cx
        self.c = consts
        S = cx.S
        self.junk = cx.sb(es, [128, 1024], BF16)
        self.st = cx.sb(es, [128, 4])
        self.tmp = cx.sb(es, [128, 1024])
        self.hb = cx.sb(es, [128, 1024], BF16)
        self.pt = pt if pt is not None else cx.ps(es, [128, 8, 128], BF16)
        self.b_junk, self.b_st, self.b_tmp, self.b_hb = S.buf(), S.buf(), S.buf(), S.buf()
        self.b_pt = b_pt if b_pt is not None else S.buf()

    def norm(self, x_ap, b_x, geff, shift, b_mod, out_ap, b_out, out_eng="pool"):
        S = self.cx.S
        st, tmp = self.st, self.tmp
        S.op("act", lambda e: e.activation(out=self.junk[:], in_=x_ap, func=AF.Square, accum_out=st[:, 0:1]),
             reads=[b_x], writes=[self.b_junk, self.b_st])
        emit_rstd(self.cx, st[:, 0:1], st[:, 1:2], 1.0 / D, self.b_st, self.b_st)
        S.op("dve", lambda e: e.scalar_tensor_tensor(out=tmp[:], in0=x_ap, scalar=st[:, 1:2], in1=geff, op0=ALU.mult, op1=ALU.mult),
             reads=[b_x, self.b_st, b_mod], writes=[self.b_tmp])
        S.op(out_eng, lambda e: e.tensor_tensor(out=out_ap, in0=tmp[:], in1=shift, op=ALU.add),
             reads=[self.b_tmp, b_mod], writes=[b_out])

    def transpose(self, in_bf, b_in, outT_ap, b_out):
        S = self.cx.S
        for kc in range(8):
            S.op("pe", lambda e, kc=kc: e.transpose(self.pt[:, kc, :], in_bf[:, kc * 128:(kc + 1) * 128], self.c["identb"][:]),
                 reads=[b_in, self.c["b_ident"]], writes=[self.b_pt])
        S.op("act", lambda e: e.copy(out=outT_ap, in_=self.pt[:, :, :]), reads=[self.b_pt], writes=[b_out])

    def normT(self, x_ap, b_x, geff, shift, b_mod, outT_ap, b_out):
        self.norm(x_ap, b_x, geff, shift, b_mod, self.hb[:], self.b_hb)
        self.transpose(self.hb, self.b_hb, outT_ap, b_out)


def load_cast(cx, es_tmp, q, dst_ap_fn, src_ap_fn, nchunks, shape, b_dst, cast_eng="pool"):
    S = cx.S
    stg = [cx.sb(es_tmp, shape) for _ in range(2)]
    bst = S.bufs(2, "stg")
    for i in range(nchunks):
        s, b = stg[i % 2], bst[i % 2]
        S.dma(q, s[:], src_ap_fn(i), writes=[b])
        S.op(cast_eng, lambda e, s=s, i=i: e.tensor_copy(out=dst_ap_fn(i), in_=s[:]), reads=[b], writes=[b_dst])


def emit_geff(cx, mod_dram, col, g_dram):
    S = cx.S
    with ExitStack() as es:
        a = cx.sb(es, [128, 1024])
        g = cx.sb(es, [128, 1024])
        ba, bg = S.buf(), S.buf()
        S.dma("sp", a[:], mod_dram[:, col:col + 1024], writes=[ba])
        S.dma("pool", g[:], g_dram[0:1, :].partition_broadcast(128), writes=[bg])
        S.op("dve", lambda e: e.scalar_tensor_tensor(out=a[:], in0=a[:], scalar=1.0, in1=g[:], op0=ALU.add, op1=ALU.mult),
             reads=[ba, bg], writes=[ba])
        S.dma("sp", mod_dram[:, col:col + 1024], a[:], reads=[ba])
        S.barrier()


def emit_precast(cx, src, dst, rows, cols):
    S = cx.S
    with ExitStack() as es:
        CW = 4096
        st = [cx.sb(es, [128, CW]) for _ in range(2)]
        ob = [cx.sb(es, [128, CW], BF16) for _ in range(2)]
        bs, bo = S.bufs(2), S.bufs(2)
        srcv = src.rearrange("(a p) c -> p a c", p=128)
        dstv = dst.rearrange("(a p) c -> p a c", p=128)
        i = 0
        per = max(1, CW // cols)
        cw = min(CW, cols)
        for a0 in range(0, rows // 128, per):
            for c0 in range(0, cols, cw):
                s, o = st[i % 2], ob[i % 2]
                sv = s[:].rearrange("p (a c) -> p a c", a=per)
                ov = o[:].rearrange("p (a c) -> p a c", a=per)
                S.dma("sp" if i % 2 == 0 else "act", sv, srcv[:, a0:a0 + per, c0:c0 + cw], writes=[bs[i % 2]])
                eng = ("pool", "dve")[i % 2]
                S.op(eng, lambda e, s=s, o=o: e.tensor_copy(out=o[:], in_=s[:]), reads=[bs[i % 2]], writes=[bo[i % 2]])
                S.dma("pool", dstv[:, a0:a0 + per, c0:c0 + cw], ov, reads=[bo[i % 2]])
                i += 1
        S.barrier()


def emit_fold_cast(cx, src, dst, rowT_dram=None, col_dram=None, col0=0):
    S = cx.S
    with ExitStack() as es:
        st = [cx.sb(es, [128, 1024]) for _ in range(2)]; ob = [cx.sb(es, [128, 1024], BF16) for _ in range(2)]
        bs, bo = S.bufs(2), S.bufs(2)
        bc = S.buf()
        if rowT_dram is not None:
            rT = cx.sb(es, [128, 8]); S.dma("sp", rT[:], rowT_dram, writes=[bc])
        if col_dram is not None:
            cB = cx.sb(es, [128, 1024]); S.dma("sp", cB[:], col_dram[:, col0:col0 + 1024], writes=[bc])
        for kc in range(8):
            s, o = st[kc % 2], ob[kc % 2]
            S.dma("sp", s[:], src[kc * 128:(kc + 1) * 128, :], writes=[bs[kc % 2]])
            if rowT_dram is not None:
                S.op("dve", lambda e, s=s, kc=kc: e.tensor_scalar(out=s[:], in0=s[:], scalar1=rT[:, kc:kc + 1], scalar2=None, op0=ALU.mult),
                     reads=[bs[kc % 2], bc], writes=[bs[kc % 2]])
            if col_dram is not None:
                S.op("dve", lambda e, s=s, o=o: e.tensor_tensor(out=o[:], in0=s[:], in1=cB[:], op=ALU.mult), reads=[bs[kc % 2], bc], writes=[bo[kc % 2]])
            else:
                S.op("dve", lambda e, s=s, o=o: e.tensor_copy(out=o[:], in_=s[:]), reads=[bs[kc % 2]], writes=[bo[kc % 2]])
            S.dma("act", dst[kc * 128:(kc + 1) * 128, :], o[:], reads=[bo[kc % 2]])
        S.barrier()


def load_w(cx, dst, src_bf, b_dst, ncol):
    S = cx.S
    v = src_bf.rearrange("(kc p) n -> p kc n", p=128)
    S.dma("sp", dst[:, 0:4, :], v[:, 0:4, :], writes=[b_dst])
    S.dma("act", dst[:, 4:8, :], v[:, 4:8, :], writes=[b_dst])


PEER_XENG = ("dve", "dve", "dve", "dve")


def emit_peer(cx, consts, HM, bHM, mod_dram, c_sh, c_ge, c_g, wq_dram, skT_dram, uTb, vb):
    S = cx.S
    GA = 4
    NG = 128 // GA
    with ExitStack() as es:
        G2 = cx.sb(es, [128, 1024]); b_g2 = S.buf("g2")
        S.dma("sp", G2[:], mod_dram[:, c_g:c_g + 1024], writes=[b_g2])
        xnT = cx.sb(es, [128, 8, 512], BF16); b_xnT = S.bufs(4, "xnT")
        QA = cx.sb(es, [128, 4, 1024]); bQA = S.bufs(4, "QA")
        PSA = cx.ps(es, [128, 4, 512]); bPSA = S.bufs(4, "PSA")
        Pexp = cx.sb(es, [128, 4, 8, 128]); bP = S.bufs(4, "Pexp")
        TH = cx.sb(es, [128, 4, 4]); RZ = cx.sb(es, [128, 4, 4]); P1n = cx.sb(es, [128, 4, 4, 128]); bTH = S.bufs(4, "TH")
        with ExitStack() as es1:
            SH = cx.sb(es1, [128, 1024]); GE = cx.sb(es1, [128, 1024]); b_mod = S.buf("mod")
            S.dma("sp", SH[:], mod_dram[:, c_sh:c_sh + 1024], writes=[b_mod])
            S.dma("sp", GE[:], mod_dram[:, c_ge:c_ge + 1024], writes=[b_mod])
            wq = cx.sb(es1, [128, 8, 1024], BF16); b_wq = S.buf("wq")
            load_w(cx, wq, wq_dram, b_wq, 1024)
            skT = cx.sb(es1, [128, 8, 128]); b_sk = S.buf("sk")
            S.dma("sp", skT[:], skT_dram.rearrange("h d n -> d h n"), writes=[b_sk])
            nt = NormT(cx, es1, consts)
            top = cx.sb(es1, [128, 8, 16]); work = cx.sb(es1, [128, 128]); cand = cx.sb(es1, [128, 256]); work2 = cx.sb(es1, [128, 256])
            c24 = cx.sb(es1, [128, 24]); mx = cx.sb(es1, [128, 8]); zs = cx.sb(es1, [128, 4]); th4 = cx.sb(es1, [128, 4])
            b_sm = S.buf("small")
            for tt in range(4):
                nt.normT(HM[:, tt, :], bHM[tt], GE[:], SH[:], b_mod, xnT[:, :, tt * 128:(tt + 1) * 128], b_xnT[tt])
            for hp in range(8):
                bank = hp % 4
                for kc in range(8):
                    S.op("pe", lambda e, hp=hp, kc=kc, bank=bank: e.matmul(PSA[:, bank, :], lhsT=wq[:, kc, hp * 128:(hp + 1) * 128], rhs=xnT[:, kc, :],
                                                                           start=(kc == 0), stop=(kc == 7)),
                         reads=[b_wq] + b_xnT, writes=[bPSA[bank]])
                S.op("act" if hp % 2 else "dve", (lambda e, hp=hp, bank=bank: e.copy(out=QA[:, hp // 2, (hp % 2) * 512:(hp % 2 + 1) * 512], in_=PSA[:, bank, :])) if hp % 2 else
                     (lambda e, hp=hp, bank=bank: e.tensor_copy(out=QA[:, hp // 2, (hp % 2) * 512:(hp % 2 + 1) * 512], in_=PSA[:, bank, :])),
                     reads=[bPSA[bank]], writes=[bQA[hp // 2]])
            for tt in range(4):
                pb = 2 * (tt % 2)
                for hp in range(8):
                    S.op("pe", lambda e, hp=hp, tt=tt, pb=pb: e.matmul(PSA[:, pb + hp // 4, (hp % 4) * 128:(hp % 4 + 1) * 128],
                                                                      lhsT=QA[:, hp // 2, (hp % 2) * 512 + tt * 128:(hp % 2) * 512 + (tt + 1) * 128],
                                                                      rhs=skT[:, hp, :], start=True, stop=True),
                         reads=[bQA[hp // 2], b_sk], writes=[bPSA[pb + hp // 4]])
                scv = PSA[:, pb:pb + 2, :].rearrange("p a (h n) -> p (a h) n", n=128)
                S.op("dve", lambda e, scv=scv: e.reduce_max(out=mx[:], in_=scv, axis=AX.X), reads=[bPSA[pb], bPSA[pb + 1]], writes=[b_sm])
                S.op("dve", lambda e: e.tensor_scalar(out=mx[:], in0=mx[:], scalar1=-1.0, scalar2=None, op0=ALU.mult), reads=[b_sm], writes=[b_sm])
                for hp in range(8):
                    S.op("act", lambda e, hp=hp, tt=tt, pb=pb: e.activation(out=Pexp[:, tt, hp, :], in_=PSA[:, pb + hp // 4, (hp % 4) * 128:(hp % 4 + 1) * 128],
                                                                           func=AF.Exp, bias=mx[:, hp:hp + 1], scale=1.0),
                         reads=[bPSA[pb + hp // 4], b_sm], writes=[bP[tt]])
                for hp in range(8):
                    S.op("dve", lambda e, hp=hp, tt=tt: e.max(out=top[:, hp, 0:8], in_=Pexp[:, tt, hp, :]), reads=[bP[tt]], writes=[b_sm])
                    S.op("dve", lambda e, hp=hp, tt=tt: e.match_replace(out=work[:], in_to_replace=top[:, hp, 0:8], in_values=Pexp[:, tt, hp, :], imm_value=-1.0),
                         reads=[bP[tt], b_sm], writes=[b_sm])
                    S.op("dve", lambda e, hp=hp: e.max(out=top[:, hp, 8:16], in_=work[:]), reads=[b_sm], writes=[b_sm])
                for h in range(4):
                    S.op("dve", lambda e, h=h: e.tensor_tensor(out=cand[:].rearrange("p (a b) -> p a b", a=16),
                                                               in0=top[:, 2 * h, :].unsqueeze(2).to_broadcast([128, 16, 16]),
                                                               in1=top[:, 2 * h + 1, :].unsqueeze(1).to_broadcast([128, 16, 16]), op=ALU.mult),
                         reads=[b_sm], writes=[b_sm])
                    S.op("dve", lambda e: e.max(out=c24[:, 0:8], in_=cand[:]), reads=[b_sm], writes=[b_sm])
                    S.op("dve", lambda e: e.match_replace(out=work2[:], in_to_replace=c24[:, 0:8], in_values=cand[:], imm_value=-1.0), reads=[b_sm], writes=[b_sm])
                    S.op("dve", lambda e: e.max(out=c24[:, 8:16], in_=work2[:]), reads=[b_sm], writes=[b_sm])
                    S.op("dve", lambda e: e.match_replace(out=cand[:], in_to_replace=c24[:, 8:16], in_values=work2[:], imm_value=-1.0), reads=[b_sm], writes=[b_sm])
                    S.op("dve", lambda e: e.max(out=c24[:, 16:24], in_=cand[:]), reads=[b_sm], writes=[b_sm])
                    S.op("dve", lambda e, h=h: e.tensor_tensor(out=th4[:, h:h + 1], in0=c24[:, 15:16], in1=c24[:, 16:17], op=ALU.add), reads=[b_sm], writes=[b_sm])
                    S.op("dve", lambda e, h=h: e.reduce_sum(out=zs[:, h:h + 1], in_=c24[:, 0:16], axis=AX.X), reads=[b_sm], writes=[b_sm])
                S.op("dve", lambda e, tt=tt: e.reciprocal(out=RZ[:, tt, :], in_=zs[:]), reads=[b_sm], writes=[bTH[tt]])
                S.op("dve", lambda e, tt=tt: e.scalar_tensor_tensor(out=TH[:, tt, :], in0=th4[:], scalar=0.5, in1=RZ[:, tt, :], op0=ALU.mult, op1=ALU.mult),
                     reads=[b_sm, bTH[tt]], writes=[bTH[tt]])
                for h in range(4):
                    S.op("dve", lambda e, tt=tt, h=h: e.tensor_scalar(out=P1n[:, tt, h, :], in0=Pexp[:, tt, 2 * h, :], scalar1=RZ[:, tt, h:h + 1], scalar2=None, op0=ALU.mult),
                         reads=[bP[tt], bTH[tt]], writes=[bTH[tt]])
            S.barrier()
        with ExitStack() as es3:
            PH = cx.ps(es3, [128, 2, 512]); bPH = S.bufs(2, "PH")
            PW = [cx.ps(es3, [128, 512]) for _ in range(2)]; bPW = S.bufs(2, "PW")
            UG = [cx.sb(es3, [128, 8, GA * 128], BF16) for _ in range(2)]; bUG = S.bufs(2, "UG")
            NV = 3
            VG = [cx.sb(es3, [128, GA, 1024], BF16) for _ in range(NV)]; bVG = S.bufs(NV, "VG")
            NX = 6
            Xn = [cx.sb(es3, [128, GA, 128]) for _ in range(NX)]; bXn = S.bufs(NX, "Xn")
            Wh = [[cx.sb(es3, [128, GA, 128], BF16) for _ in range(16)] for _ in range(2)]; bWh = [S.bufs(16, "Wh%d_" % k) for k in range(2)]
            G = [cx.sb(es3, [128, 512], BF16) for _ in range(2 * GA)]; bG = S.bufs(2 * GA, "G")
            WgT = [cx.sb(es3, [128, GA, 512], BF16) for _ in range(2)]; bWg = S.bufs(2, "WgT")
            uv = uTb.rearrange("(kc p) n -> p kc n", p=128)
            vv = vb.rearrange("(a p) d -> p a d", p=128)
            cnt = {"x": 0, "ph": 0, "pw": 0, "fl": 0}
            XENG = PEER_XENG

            def stage1(gi):
                ug, bu = UG[gi % 2], bUG[gi % 2]
                S.dma("sp", ug[:, 0:4, :], uv[:, 0:4, gi * GA * 128:(gi + 1) * GA * 128], writes=[bu])
                S.dma("act", ug[:, 4:8, :], uv[:, 4:8, gi * GA * 128:(gi + 1) * GA * 128], writes=[bu])
                for ac in range(GA):
                    j = cnt["ph"] % 2
                    cnt["ph"] += 1
                    g, bg = G[(gi % 2) * GA + ac], bG[(gi % 2) * GA + ac]
                    for kc in range(8):
                        S.op("pe", lambda e, ac=ac, kc=kc, j=j, ug=ug: e.matmul(PH[:, j, :], lhsT=ug[:, kc, ac * 128:(ac + 1) * 128], rhs=xnT[:, kc, :],
                                                                              start=(kc == 0), stop=(kc == 7)),
                             reads=[bu] + b_xnT, writes=[bPH[j]])
                    S.op("act", lambda e, j=j, g=g: e.activation(out=g[:], in_=PH[:, j, :], func=AF.Gelu), reads=[bPH[j]], writes=[bg])
                for tt in range(4):
                    for h in range(4):
                        x, bx = Xn[cnt["x"] % NX], bXn[cnt["x"] % NX]
                        cnt["x"] += 1
                        if True:
                            S.op(XENG[h], lambda e, tt=tt, h=h, x=x, gi=gi: e.tensor_tensor(
                                out=x[:], in0=P1n[:, tt, h, gi * GA:(gi + 1) * GA].unsqueeze(2).to_broadcast([128, GA, 128]),
                                in1=Pexp[:, tt, 2 * h + 1, :].unsqueeze(1).to_broadcast([128, GA, 128]), op=ALU.mult),
                                reads=[bP[tt], bTH[tt]], writes=[bx])
                        else:
                            for a in range(GA):
                                S.op("act", lambda e, tt=tt, h=h, a=a, x=x, gi=gi: e.activation(out=x[:, a, :], in_=Pexp[:, tt, 2 * h + 1, :], func=AF.Copy,
                                                                                             scale=P1n[:, tt, h, gi * GA + a:gi * GA + a + 1]),
                                     reads=[bP[tt], bTH[tt]], writes=[bx])
                        w, bw = Wh[gi % 2][tt * 4 + h], bWh[gi % 2][tt * 4 + h]
                        S.op("dve", lambda e, tt=tt, h=h, x=x, w=w: e.scalar_tensor_tensor(out=w[:], in0=x[:], scalar=TH[:, tt, h:h + 1], in1=x[:],
                                                                                          op0=ALU.is_ge, op1=ALU.mult),
                             reads=[bx, bTH[tt]], writes=[bw])

            def stage2(gi):
                vg, bv = VG[gi % NV], bVG[gi % NV]
                S.dma("sp", vg[:, 0:2, :], vv[:, gi * GA:gi * GA + 2, :], writes=[bv])
                S.dma("act", vg[:, 2:4, :], vv[:, gi * GA + 2:(gi + 1) * GA, :], writes=[bv])
                wg, bwg = WgT[gi % 2], bWg[gi % 2]
                for ac in range(GA):
                    j = cnt["pw"] % 2
                    cnt["pw"] += 1
                    g, bg = G[(gi % 2) * GA + ac], bG[(gi % 2) * GA + ac]
                    for tt in range(4):
                        for h in range(4):
                            w, bw = Wh[gi % 2][tt * 4 + h], bWh[gi % 2][tt * 4 + h]
                            S.op("pe", lambda e, ac=ac, tt=tt, j=j, w=w, h=h: e.matmul(PW[j][:, tt * 128:(tt + 1) * 128], lhsT=w[:, ac, :], rhs=consts["identb"][:],
                                                                                     start=(h == 0), stop=(h == 3)),
                                 reads=[bw, consts["b_ident"]], writes=[bPW[j]])
                    S.op("dve", lambda e, ac=ac, j=j, wg=wg, g=g: e.tensor_tensor(out=wg[:, ac, :], in0=g[:], in1=PW[j][:], op=ALU.mult),
                         reads=[bg, bPW[j]], writes=[bwg])

            def stage3(gi):
                vg, bv = VG[gi % NV], bVG[gi % NV]
                wg, bwg = WgT[gi % 2], bWg[gi % 2]
                for tt in range(4):
                    pb = 2 * (tt % 2)
                    for ac in range(GA):
                        for half in range(2):
                            S.op("pe", lambda e, ac=ac, tt=tt, half=half, pb=pb, wg=wg, vg=vg: e.matmul(
                                PSA[:, pb + half, :], lhsT=wg[:, ac, tt * 128:(tt + 1) * 128], rhs=vg[:, ac, half * 512:(half + 1) * 512],
                                start=(ac == 0), stop=(ac == GA - 1)),
                                reads=[bwg, bv], writes=[bPSA[pb + half]])
                    src = PSA[:, pb:pb + 2, :].rearrange("p a b -> p (a b)")
                    if gi == 0:
                        S.op("act", lambda e, tt=tt, src=src: e.copy(out=QA[:, tt, :], in_=src), reads=[bPSA[pb], bPSA[pb + 1]], writes=[bQA[tt]])
                    else:
                        S.op("dve", lambda e, tt=tt, src=src: e.tensor_tensor(out=QA[:, tt, :], in0=QA[:, tt, :], in1=src, op=ALU.add),
                             reads=[bPSA[pb], bPSA[pb + 1], bQA[tt]], writes=[bQA[tt]])

            for k in range(NG + 2):
                if k < NG:
                    stage1(k)
                if 0 <= k - 1 < NG:
                    stage2(k - 1)
                if 0 <= k - 2 < NG:
                    stage3(k - 2)
            for tt in range(4):
                S.op("pool", lambda e, tt=tt: e.tensor_tensor(out=QA[:, tt, :], in0=QA[:, tt, :], in1=G2[:], op=ALU.mult), reads=[bQA[tt], b_g2], writes=[bQA[tt]])
                S.op("dve", lambda e, tt=tt: e.tensor_tensor(out=HM[:, tt, :], in0=HM[:, tt, :], in1=QA[:, tt, :], op=ALU.add), reads=[bQA[tt], bHM[tt]], writes=[bHM[tt]])
            S.barrier()


def hgrn_consts_host():
    t = np.arange(128)
    ch = t // 16
    same = ch[:, None] == ch[None, :]
    BT = (same & (t[:, None] <= t[None, :])).astype(np.float32)
    RT = (same & (t[:, None] > t[None, :])).astype(np.float32)
    CI = (ch[:, None] == np.arange(8)[None, :]).astype(np.float32)
    RTF = (t[:, None] > t[None, :]).astype(np.float32)
    hcA = np.concatenate([BT, RT, CI, RTF, np.ones((128, 1), np.float32)], axis=1)
    hcB = np.ascontiguousarray(CI.T).reshape(1, 1024)
    return hcA, hcB


def emit_hgrn(cx, consts, HM, bHM, mode, x_dram, ntiles, snap, mod_dram, c_sh, c_ge, c_g,
              win_dram, wout_dram, ongT_dram, lbl_dram, hcA_dram, hcB_dram, onehot_dram=None, m=0):
    S = cx.S
    full = (mode == "full")
    with ExitStack() as es:
        hcA = cx.sb(es, [128, 393]); CHM = cx.sb(es, [128, 8, 128]); b_hc = S.buf("hc")
        S.dma("sp", hcA[:], hcA_dram, writes=[b_hc])
        S.dma("pool", CHM[:].rearrange("p a b -> p (a b)"), hcB_dram[0:1, :].partition_broadcast(128), writes=[b_hc])
        BT, RT, CI = hcA[:, 0:128], hcA[:, 128:256], hcA[:, 256:264]
        RTF, ONE = hcA[:, 264:392], hcA[:, 392:393]
        LB = cx.sb(es, [128, 1024]); OML = cx.sb(es, [128, 1024]); GE = cx.sb(es, [128, 1024]); SH = cx.sb(es, [128, 1024])
        b_mod = S.buf("hmod")
        S.dma("sp", SH[:], mod_dram[:, c_sh:c_sh + 1024], writes=[b_mod])
        S.dma("sp", GE[:], mod_dram[:, c_ge:c_ge + 1024], writes=[b_mod])
        S.dma("pool", LB[:], lbl_dram[0:1, :].partition_broadcast(128), writes=[b_mod])
        S.dma("pool", OML[:], lbl_dram[1:2, :].partition_broadcast(128), writes=[b_mod])
        S.op("dve", lambda e: e.tensor_tensor(out=LB[:], in0=LB[:], in1=OML[:], op=ALU.subtract), reads=[b_mod], writes=[b_mod])
        S.op("act", lambda e: e.activation(out=LB[:], in_=LB[:], func=AF.Sigmoid), reads=[b_mod], writes=[b_mod])
        S.op("dve", lambda e: e.tensor_scalar(out=OML[:], in0=LB[:], scalar1=-1.0, scalar2=1.0, op0=ALU.mult, op1=ALU.add), reads=[b_mod], writes=[b_mod])
        win = cx.sb(es, [128, 8, 4096], BF16); b_win = S.buf("win")
        wout = cx.sb(es, [128, 8, 1024], BF16); b_wout = S.buf("wout")
        load_w(cx, win, win_dram, b_win, 4096)
        if full:
            load_w(cx, wout, wout_dram, b_wout, 1024)
        nt = NormT(cx, es, consts)
        xt = [cx.sb(es, [128, 1024]) for _ in range(2)]; b_xt = S.bufs(2, "xt")
        hnT = cx.sb(es, [128, 8, 128], BF16); b_hnT = S.buf("hnT")
        R = [cx.sb(es, [128, 1024]) for _ in range(6)]; bR = S.bufs(6, "R")
        OGb = cx.sb(es, [128, 1024], BF16); b_og = S.buf("og")
        VVb = cx.sb(es, [128, 1024], BF16); bVVb = S.buf("VVb")
        KLb = cx.sb(es, [128, 1024], BF16); bKLb = S.buf("KLb")
        QDb = cx.sb(es, [128, 1024], BF16); bQDb = S.buf("QDb")
        KDb = cx.sb(es, [128, 1024], BF16); bKDb = S.buf("KDb")
        STb = cx.sb(es, [128, 8, 2, 128], BF16); bSTb = [S.bufs(2, "STb%d_" % h) for h in range(8)]
        ogT = cx.sb(es, [128, 8, 128], BF16); b_ogT = S.buf("ogT")
        QKT = [cx.sb(es, [128, 2, 128], BF16) for _ in range(2)]; bQKT = S.bufs(2, "QKT")
        SCM = [cx.sb(es, [128, 128], BF16) for _ in range(2)]; bSCM = S.bufs(2, "SCM")
        QDM = cx.sb(es, [128, 8, 128], BF16); bQDM = S.buf("QDM")
        KLM = cx.sb(es, [128, 8, 128], BF16); bKLM = S.buf("KLM")
        ST = cx.sb(es, [128, 8, 2, 128]); bST = [S.bufs(2, "ST%d_" % h) for h in range(8)]
        EDEC = cx.sb(es, [128, 64]); bEDEC = S.buf("EDEC")
        ss8 = cx.sb(es, [128, 16]); b_ss8 = S.buf("ss8")
        PJ = [cx.ps(es, [128, 512]) for _ in range(3)]; bPJ = S.bufs(3, "PJ")
        PKV = [cx.ps(es, [128, 512]) for _ in range(2)]; bPKV = S.bufs(2, "PKV")
        PO = cx.ps(es, [128, 512]); bPO = S.buf("PO")
        PC = cx.ps(es, [128, 512]); bPC = S.buf("PC")
        pj_i = [0]

        def nextpj():
            k = pj_i[0] % 3
            pj_i[0] += 1
            return PJ[k], bPJ[k]

        def proj(j, nb):
            pj, b = nextpj()
            for kc in range(8):
                S.op("pe", lambda e, pj=pj, kc=kc: e.matmul(pj[:], lhsT=hnT[:, kc, :],
                                                            rhs=win[:, kc, j * 1024 + nb * 512:j * 1024 + (nb + 1) * 512],
                                                            start=(kc == 0), stop=(kc == 7)),
                     reads=[b_hnT, b_win], writes=[b])
            return pj, b

        if full:
            oh = cx.sb(es, [128, 4]); b_oh = S.buf("oh")
            S.dma("pool", oh[:], onehot_dram[0:1, :].partition_broadcast(128), writes=[b_oh])
            stv = ST[:, :, 0, :]
            allst = [bST[h][0] for h in range(8)]
            for j in range(4):
                S.dma("sp", R[5][:], snap[4 * m + j], writes=[bR[5]])
                r5 = R[5][:].rearrange("p (h v) -> p h v", h=8)
                if j == 0:
                    S.op("dve", lambda e, j=j, r5=r5: e.tensor_scalar(out=stv, in0=r5, scalar1=oh[:, j:j + 1], scalar2=None, op0=ALU.mult),
                         reads=[bR[5], b_oh], writes=allst)
                else:
                    S.op("dve", lambda e, j=j, r5=r5: e.scalar_tensor_tensor(out=stv, in0=r5, scalar=oh[:, j:j + 1], in1=stv, op0=ALU.mult, op1=ALU.add),
                         reads=[bR[5], b_oh] + allst, writes=allst)
            S.op("act", lambda e: e.copy(out=STb[:, :, 0, :], in_=ST[:, :, 0, :]), reads=allst, writes=[bSTb[h][0] for h in range(8)])
        else:
            S.op("pool", lambda e: e.memset(ST[:].rearrange("p a b c -> p (a b c)"), 0.0), writes=[bST[h][s] for h in range(8) for s in range(2)])

        for ti in range(ntiles):
            x_t, bx = xt[ti % 2], b_xt[ti % 2]
            if (not full) and ti % 4 == 0:
                S.dma("pool", snap[ti // 4].rearrange("p (h v) -> p h v", h=8), ST[:, :, 0, :], reads=[bST[h][0] for h in range(8)])
            row0 = (m * 4 + ti) * 128 if full else ti * 128
            S.dma("sp", x_t[:], x_dram[row0:row0 + 128, :], writes=[bx])
            nt.normT(x_t[:], bx, GE[:], SH[:], b_mod, hnT[:], b_hnT)
            H2 = [slice(0, 512), slice(512, 1024)]
            for nb in range(2):
                pf, bpf = proj(1, nb)
                S.op("act", lambda e, pf=pf, nb=nb: e.activation(out=R[0][:, H2[nb]], in_=pf[:], func=AF.Sigmoid), reads=[bpf], writes=[bR[0]])
            S.op("dve", lambda e: e.tensor_tensor(out=R[0][:], in0=R[0][:], in1=OML[:], op=ALU.mult), reads=[bR[0], b_mod], writes=[bR[0]])
            S.op("pool", lambda e: e.tensor_tensor(out=R[0][:], in0=R[0][:], in1=LB[:], op=ALU.add), reads=[bR[0], b_mod], writes=[bR[0]])
            S.op("act", lambda e: e.activation(out=R[1][:], in_=R[0][:], func=AF.Ln), reads=[bR[0]], writes=[bR[1]])
            S.op("pool", lambda e: e.tensor_scalar(out=R[2][:], in0=R[0][:], scalar1=-1.0, scalar2=1.0, op0=ALU.mult, op1=ALU.add), reads=[bR[0]], writes=[bR[2]])
            for nb in range(2):
                pc, bpc = nextpj()
                S.op("pe", lambda e, pc=pc, nb=nb: e.matmul(pc[:], lhsT=BT, rhs=R[1][:, H2[nb]], start=True, stop=True),
                     reads=[b_hc, bR[1]], writes=[bpc])
                if full:
                    S.op("act", lambda e, pc=pc, nb=nb: e.activation(out=R[0][:, H2[nb]], in_=pc[:], func=AF.Exp), reads=[bpc], writes=[bR[0]])
                    S.op("act", lambda e, pc=pc, nb=nb: e.activation(out=R[3][:, H2[nb]], in_=pc[:], func=AF.Exp, scale=-1.0), reads=[bpc], writes=[bR[3]])
            for nb in range(2):
                pr, bpr = nextpj()
                S.op("pe", lambda e, pr=pr, nb=nb: e.matmul(pr[:], lhsT=(RT if full else RTF), rhs=R[1][:, H2[nb]], start=True, stop=True),
                     reads=[b_hc, bR[1]], writes=[bpr])
                S.op("act", lambda e, pr=pr, nb=nb: e.activation(out=R[4][:, H2[nb]], in_=pr[:], func=AF.Exp), reads=[bpr], writes=[bR[4]])
            for h in range(8):
                S.op("pe", lambda e, h=h: e.matmul(PC[:, 384 + h * 8:384 + (h + 1) * 8], lhsT=R[1][:, h * 128:(h + 1) * 128], rhs=CI, start=True, stop=True),
                     reads=[b_hc, bR[1]], writes=[bPC]) if full else \
                    S.op("pe", lambda e, h=h: e.matmul(PC[:, 384 + h:385 + h], lhsT=R[1][:, h * 128:(h + 1) * 128], rhs=ONE, start=True, stop=True),
                         reads=[b_hc, bR[1]], writes=[bPC])
            if full:
                S.op("act", lambda e: e.activation(out=EDEC[:], in_=PC[:, 384:448], func=AF.Exp), reads=[bPC], writes=[bEDEC])
            else:
                S.op("act", lambda e: e.activation(out=EDEC[:, 0:8], in_=PC[:, 384:392], func=AF.Exp), reads=[bPC], writes=[bEDEC])
            S.op("dve", lambda e: e.tensor_tensor(out=KLb[:], in0=R[4][:], in1=R[2][:], op=ALU.mult), reads=[bR[4], bR[2]], writes=[bKLb])
            if full:
                S.op("dve", lambda e: e.tensor_tensor(out=KDb[:], in0=R[3][:], in1=R[2][:], op=ALU.mult), reads=[bR[3], bR[2]], writes=[bKDb])
            for nb in range(2):
                pi, bpi = proj(2, nb)
                S.op("act", lambda e, pi=pi, nb=nb: e.copy(out=VVb[:, H2[nb]], in_=pi[:]), reads=[bpi], writes=[bVVb])
            if full:
                for nb in range(2):
                    pq, bpq = proj(0, nb)
                    S.op("act", lambda e, pq=pq, nb=nb: e.activation(out=R[2][:, H2[nb]], in_=pq[:], func=AF.Silu), reads=[bpq], writes=[bR[2]])
                S.op("dve", lambda e: e.tensor_tensor(out=QDb[:], in0=R[0][:], in1=R[2][:], op=ALU.mult), reads=[bR[0], bR[2]], writes=[bQDb])
                for nb in range(2):
                    pg, bpg = proj(3, nb)
                    S.op("act", lambda e, pg=pg, nb=nb: e.activation(out=R[2][:, H2[nb]], in_=pg[:], func=AF.Silu), reads=[bpg], writes=[bR[2]])
            for h in range(8):
                j = h % 2
                hs = slice(h * 128, (h + 1) * 128)
                if full:
                    S.op("pe", lambda e, hs=hs: e.transpose(nt.pt[:, 0, :], QDb[:, hs], consts["identb"][:]), reads=[bQDb, consts["b_ident"]], writes=[nt.b_pt])
                    S.op("pe", lambda e, hs=hs: e.transpose(nt.pt[:, 1, :], KDb[:, hs], consts["identb"][:]), reads=[bKDb, consts["b_ident"]], writes=[nt.b_pt])
                    S.op("act", lambda e, j=j: e.copy(out=QKT[j][:], in_=nt.pt[:, 0:2, :]), reads=[nt.b_pt], writes=[bQKT[j]])
                    S.op("pe", lambda e, j=j: e.matmul(PC[:, 256:384], lhsT=QKT[j][:, 1, :], rhs=QKT[j][:, 0, :], start=True, stop=True), reads=[bQKT[j]], writes=[bPC])
                    S.op("dve", lambda e, j=j: e.tensor_tensor(out=SCM[j][:], in0=PC[:, 256:384], in1=BT, op=ALU.mult), reads=[bPC, b_hc], writes=[bSCM[j]])
                    S.op("pool", lambda e, j=j: e.tensor_tensor(out=QDM[:], in0=QKT[j][:, 0, :].unsqueeze(1).to_broadcast([128, 8, 128]), in1=CHM[:], op=ALU.mult),
                         reads=[bQKT[j], b_hc], writes=[bQDM])
                if not full:
                    k = h % 2
                    S.op("pe", lambda e, k=k, hs=hs: e.matmul(PKV[k][:, 0:128], lhsT=KLb[:, hs], rhs=VVb[:, hs], start=True, stop=True),
                         reads=[bKLb, bVVb], writes=[bPKV[k]])
                    S.op("dve", lambda e, k=k, h=h: e.scalar_tensor_tensor(
                        out=ST[:, h, 0, :], in0=ST[:, h, 0, :], scalar=EDEC[:, h:h + 1], in1=PKV[k][:, 0:128], op0=ALU.mult, op1=ALU.add),
                        reads=[bST[h][0], bEDEC, bPKV[k]], writes=[bST[h][0]])
                    continue
                S.op("pool", lambda e, hs=hs: e.tensor_tensor(out=KLM[:], in0=KLb[:, hs].unsqueeze(1).to_broadcast([128, 8, 128]),
                                                              in1=CI.unsqueeze(2).to_broadcast([128, 8, 128]), op=ALU.mult),
                     reads=[bKLb, b_hc], writes=[bKLM])
                if full:
                    S.op("pe", lambda e, j=j, hs=hs: e.matmul(PO[:, 0:128], lhsT=SCM[j][:], rhs=VVb[:, hs], start=True, stop=False),
                         reads=[bSCM[j], bVVb], writes=[bPO])
                for c in range(8):
                    k = c % 2
                    s_old, s_new = c % 2, (c + 1) % 2
                    S.op("pe", lambda e, c=c, k=k, hs=hs: e.matmul(PKV[k][:, 0:128], lhsT=KLM[:, c, :], rhs=VVb[:, hs], start=True, stop=True),
                         reads=[bKLM, bVVb], writes=[bPKV[k]])
                    if full:
                        S.op("pe", lambda e, c=c, h=h, s_old=s_old: e.matmul(PO[:, 0:128], lhsT=QDM[:, c, :], rhs=STb[:, h, s_old, :],
                                                                            start=False, stop=(c == 7)),
                             reads=[bQDM, bSTb[h][s_old]], writes=[bPO])
                    S.op("dve", lambda e, c=c, k=k, h=h, s_old=s_old, s_new=s_new: e.scalar_tensor_tensor(
                        out=ST[:, h, s_new, :], in0=ST[:, h, s_old, :], scalar=EDEC[:, h * 8 + c:h * 8 + c + 1], in1=PKV[k][:, 0:128], op0=ALU.mult, op1=ALU.add),
                        reads=[bST[h][s_old], bEDEC, bPKV[k]], writes=[bST[h][s_new]])
                    if full:
                        S.op("act", lambda e, h=h, s_new=s_new: e.copy(out=STb[:, h, s_new, :], in_=ST[:, h, s_new, :]), reads=[bST[h][s_new]], writes=[bSTb[h][s_new]])
                if full:
                    S.op("act", lambda e, hs=hs: e.copy(out=R[5][:, hs], in_=PO[:, 0:128]), reads=[bPO], writes=[bR[5]])
            if full:
                S.op("dve", lambda e: e.tensor_tensor(out=R[3][:], in0=R[5][:], in1=R[5][:], op=ALU.mult), reads=[bR[5]], writes=[bR[3]])
                S.op("dve", lambda e: e.reduce_sum(out=ss8[:, 0:8], in_=R[3][:].rearrange("p (h v) -> p h v", h=8), axis=AX.X), reads=[bR[3]], writes=[b_ss8])
                emit_rstd(cx, ss8[:, 0:8], ss8[:, 8:16], 1.0 / 128, b_ss8, b_ss8)
                S.op("dve", lambda e: e.tensor_tensor(out=R[5][:].rearrange("p (h v) -> p h v", h=8), in0=R[5][:].rearrange("p (h v) -> p h v", h=8),
                                                      in1=ss8[:, 8:16].unsqueeze(2).to_broadcast([128, 8, 128]), op=ALU.mult),
                     reads=[bR[5], b_ss8], writes=[bR[5]])
                S.op("pool", lambda e: e.tensor_tensor(out=OGb[:], in0=R[5][:], in1=R[2][:], op=ALU.mult), reads=[bR[5], bR[2]], writes=[b_og])
                nt.transpose(OGb, b_og, ogT[:], b_ogT)
                for nb in range(2):
                    pm, bpm = nextpj()
                    for kc in range(8):
                        S.op("pe", lambda e, pm=pm, nb=nb, kc=kc: e.matmul(pm[:], lhsT=ogT[:, kc, :], rhs=wout[:, kc, nb * 512:(nb + 1) * 512],
                                                                          start=(kc == 0), stop=(kc == 7)),
                             reads=[b_ogT, b_wout], writes=[bpm])
                    S.op("dve", lambda e, pm=pm, x_t=x_t, ti=ti, nb=nb: e.tensor_tensor(out=HM[:, ti, H2[nb]], in0=pm[:], in1=x_t[:, H2[nb]], op=ALU.add),
                         reads=[bpm, bx], writes=[bHM[ti]])
        if (not full) and ntiles % 4 == 0:
            S.dma("pool", snap[ntiles // 4].rearrange("p (h v) -> p h v", h=8), ST[:, :, 0, :], reads=[bST[h][0] for h in range(8)])
        S.barrier()


def attn_consts_host(r):
    j = np.arange(128)
    trin = -(j[:, None] >= j[None, :]).astype(np.float32)
    onesn = -np.ones((128, 128), np.float32)
    ac = np.concatenate([trin, onesn], axis=1).astype(ml_dtypes.bfloat16)
    k = np.arange(16)
    kp = k[None, :, None] * 128 + j[:, None, None]
    qp = 4 * r * 128 + np.arange(512)[None, None, :]
    mask = (kp < qp).astype(np.float32).astype(ml_dtypes.bfloat16)
    return ac, mask


def emit_attn(cx, consts, HM, bHM, m, mod_dram, c_sh, c_ge, c_g, wq_dram, wo_dram, KT_dram, V_dram, ac_dram, mask_dram):
    S = cx.S
    NB = 16 * (m + 1)
    with ExitStack() as es:
        SH = cx.sb(es, [128, 1024]); GE = cx.sb(es, [128, 1024]); b_mod = S.buf("amod")
        S.dma("sp", SH[:], mod_dram[:, c_sh:c_sh + 1024], writes=[b_mod])
        S.dma("sp", GE[:], mod_dram[:, c_ge:c_ge + 1024], writes=[b_mod])
        AC = cx.sb(es, [128, 256], BF16); MASK = cx.sb(es, [128, 16, 512], BF16); b_ac = S.buf("ac")
        S.dma("sp", AC[:], ac_dram, writes=[b_ac])
        S.dma("sp", MASK[:], mask_dram, writes=[b_ac])
        TRIN, ONESN = AC[:, 0:128], AC[:, 128:256]
        wq = cx.sb(es, [128, 8, 1024], BF16); b_wq = S.buf("awq")
        wo = cx.sb(es, [128, 8, 1024], BF16); b_wo = S.buf("awo")
        load_w(cx, wq, wq_dram, b_wq, 1024)
        load_w(cx, wo, wo_dram, b_wo, 1024)
        nt = NormT(cx, es, consts)
        xnT = cx.sb(es, [128, 8, 512], BF16); b_xnT = S.bufs(4, "axnT")
        QT = cx.sb(es, [128, 8, 512], BF16); bQT = S.bufs(8, "QT")
        NKV = 3
        KTc = [cx.sb(es, [128, 2048], BF16) for _ in range(NKV)]; bKT = S.bufs(NKV, "KTc")
        Vc = [cx.sb(es, [128, 16, 128], BF16) for _ in range(NKV)]; bV = S.bufs(NKV, "Vc")
        NR = 3
        E = [cx.sb(es, [128, 512]) for _ in range(NR)]; bE = S.bufs(NR, "E")
        LK = [cx.sb(es, [128, 512], BF16) for _ in range(NR)]; bLK = S.bufs(NR, "LK")
        LKS = [cx.sb(es, [128, 512], BF16) for _ in range(NR)]; bLKS = S.bufs(NR, "LKS")
        A = [cx.sb(es, [128, 512], BF16) for _ in range(NR)]; bA = S.bufs(NR, "A")
        OT = cx.sb(es, [128, 8, 512], BF16); bOT = S.bufs(8, "OT")
        PZ = [cx.ps(es, [128, 512]) for _ in range(3)]; bPZ = S.bufs(3, "PZ")
        PS = [cx.ps(es, [128, 512]) for _ in range(2)]; bPS = S.bufs(2, "PS")
        POUT = [cx.ps(es, [128, 512]) for _ in range(2)]; bPOUT = S.bufs(2, "POUT")
        for tt in range(4):
            nt.normT(HM[:, tt, :], bHM[tt], GE[:], SH[:], b_mod, xnT[:, :, tt * 128:(tt + 1) * 128], b_xnT[tt])
        sc = 1.0 / math.sqrt(128.0)
        for h in range(8):
            pz, bpz = (PZ[h % 3], bPZ[h % 3])
            for kc in range(8):
                S.op("pe", lambda e, h=h, kc=kc, pz=pz: e.matmul(pz[:], lhsT=wq[:, kc, h * 128:(h + 1) * 128], rhs=xnT[:, kc, :], start=(kc == 0), stop=(kc == 7)),
                     reads=[b_wq] + b_xnT, writes=[bpz])
            S.op("act", lambda e, h=h, pz=pz: e.activation(out=QT[:, h, :], in_=pz[:], func=AF.Copy, scale=sc), reads=[bpz], writes=[bQT[h]])
        items = []
        ld = 0
        for h in range(8):
            for ci in range(m, -1, -1):
                slot = ld % NKV
                ld += 1
                for kk in range(15, -1, -1):
                    items.append(dict(h=h, ci=ci, kk=kk, slot=slot, load=(kk == 15), first=(ci == m and kk == 15), last=(ci == 0 and kk == 0),
                                      masked=(ci == m)))
        for i, it in enumerate(items):
            it["i"] = i

        def stageA(it):
            i, h, kk, slot = it["i"], it["h"], it["kk"], it["slot"]
            kt, bkt, vc, bvc = KTc[slot], bKT[slot], Vc[slot], bV[slot]
            if it["load"]:
                S.dma("sp", kt[:], KT_dram[h, :, it["ci"] * 2048:(it["ci"] + 1) * 2048], writes=[bkt])
                S.dma("pool", vc[:], V_dram[h, :, it["ci"] * 16:(it["ci"] + 1) * 16, :], writes=[bvc])
            z, r = i % 3, i % NR
            ks = slice(kk * 128, (kk + 1) * 128)
            S.op("pe", lambda e: e.matmul(PZ[z][:], lhsT=kt[:, ks], rhs=QT[:, h, :], start=True, stop=True), reads=[bkt, bQT[h]], writes=[bPZ[z]])
            S.op("act", lambda e: e.activation(out=E[r][:], in_=PZ[z][:], func=AF.Exp), reads=[bPZ[z]], writes=[bE[r]])
            S.op("act", lambda e: e.activation(out=LK[r][:], in_=E[r][:], func=AF.Ln, bias=1.0, scale=1.0), reads=[bE[r]], writes=[bLK[r]])
            if it["masked"]:
                S.op("dve", lambda e: e.tensor_tensor(out=LK[r][:], in0=LK[r][:], in1=MASK[:, kk, :], op=ALU.mult), reads=[bLK[r], b_ac], writes=[bLK[r]])

        def stageB(it):
            i, h, kk, slot = it["i"], it["h"], it["kk"], it["slot"]
            kt, bkt = KTc[slot], bKT[slot]
            r, p = i % NR, i % 2
            nx = (i + 1) % NR
            first, last = it["first"], it["last"]
            ks = slice(kk * 128, (kk + 1) * 128)
            S.op("pe", lambda e: e.matmul(PS[p][:], lhsT=kt[:, ks], rhs=QT[:, h, :], start=True, stop=False), reads=[bkt, bQT[h]], writes=[bPS[p]])
            S.op("pe", lambda e: e.matmul(PS[p][:], lhsT=TRIN, rhs=LK[r][:], start=False, stop=first), reads=[b_ac, bLK[r]], writes=[bPS[p]])
            if not first:
                S.op("pe", lambda e: e.matmul(PS[p][:], lhsT=ONESN, rhs=LKS[r][:], start=False, stop=True), reads=[b_ac, bLKS[r]], writes=[bPS[p]])
            S.op("act", lambda e: e.activation(out=A[r][:], in_=PS[p][:], func=AF.Exp), reads=[bPS[p]], writes=[bA[r]])
            if it["masked"]:
                S.op("pool", lambda e: e.tensor_tensor(out=A[r][:], in0=A[r][:], in1=MASK[:, kk, :], op=ALU.mult), reads=[bA[r], b_ac], writes=[bA[r]])
            if first:
                S.op("dve", lambda e: e.tensor_copy(out=LKS[nx][:], in_=LK[r][:]), reads=[bLK[r]], writes=[bLKS[nx]])
            elif not last:
                S.op("dve", lambda e: e.tensor_tensor(out=LKS[nx][:], in0=LKS[r][:], in1=LK[r][:], op=ALU.add), reads=[bLK[r], bLKS[r]], writes=[bLKS[nx]])

        def stageC(it):
            i, h, kk, slot = it["i"], it["h"], it["kk"], it["slot"]
            vc, bvc = Vc[slot], bV[slot]
            r = i % NR
            po, bpo = POUT[h % 2], bPOUT[h % 2]
            S.op("pe", lambda e: e.matmul(po[:], lhsT=vc[:, kk, :], rhs=A[r][:], start=it["first"], stop=it["last"]), reads=[bvc, bA[r]], writes=[bpo])
            if it["last"]:
                S.op("dve", lambda e: e.tensor_copy(out=OT[:, h, :], in_=po[:]), reads=[bpo], writes=[bOT[h]])

        n = len(items)
        for k in range(n + 2):
            if k < n:
                stageA(items[k])
            if 0 <= k - 1 < n:
                stageB(items[k - 1])
            if 0 <= k - 2 < n:
                stageC(items[k - 2])
        for tt in range(4):
            for nb in range(2):
                S_ps, b_ps = PZ[nb], bPZ[nb]
                for h in range(8):
                    S.op("pe", lambda e, tt=tt, nb=nb, h=h, S_ps=S_ps: e.matmul(S_ps[:], lhsT=OT[:, h, tt * 128:(tt + 1) * 128], rhs=wo[:, h, nb * 512:(nb + 1) * 512],
                                                                             start=(h == 0), stop=(h == 7)),
                         reads=[bOT[h], b_wo], writes=[b_ps])
                S.op("dve", lambda e, tt=tt, nb=nb, S_ps=S_ps: e.tensor_tensor(out=HM[:, tt, nb * 512:(nb + 1) * 512], in0=HM[:, tt, nb * 512:(nb + 1) * 512], in1=S_ps[:], op=ALU.add),
                     reads=[b_ps, bHM[tt]], writes=[bHM[tt]])
        S.barrier()


def emit_kv(cx, consts, HM, bHM, m, mod_dram, c_sh, c_ge, kvw_dram, KTo, Vo):
    S = cx.S
    with ExitStack() as es:
        SH = cx.sb(es, [128, 1024]); GE = cx.sb(es, [128, 1024]); b_mod = S.buf("kmod")
        S.dma("sp", SH[:], mod_dram[:, c_sh:c_sh + 1024], writes=[b_mod])
        S.dma("sp", GE[:], mod_dram[:, c_ge:c_ge + 1024], writes=[b_mod])
        kvw = cx.sb(es, [128, 8, 2048], BF16); b_kvw = S.buf("kvw")
        load_w(cx, kvw, kvw_dram, b_kvw, 2048)
        nt = NormT(cx, es, consts)
        xnT = cx.sb(es, [128, 8, 512], BF16); b_xnT = S.bufs(4, "kxnT")
        KTs = cx.sb(es, [128, 8, 512], BF16); bKTs = S.buf("KTs")
        Vs = [cx.sb(es, [128, 1024], BF16) for _ in range(2)]; bVs = S.bufs(2, "Vs")
        PK = [cx.ps(es, [128, 512]) for _ in range(4)]; bPK = S.bufs(4, "PK")
        for tt in range(4):
            nt.normT(HM[:, tt, :], bHM[tt], GE[:], SH[:], b_mod, xnT[:, :, tt * 128:(tt + 1) * 128], b_xnT[tt])
        for h in range(8):
            pk, bpk = PK[h % 4], bPK[h % 4]
            for kc in range(8):
                S.op("pe", lambda e, h=h, kc=kc, pk=pk: e.matmul(pk[:], lhsT=kvw[:, kc, h * 128:(h + 1) * 128], rhs=xnT[:, kc, :], start=(kc == 0), stop=(kc == 7)),
                     reads=[b_kvw] + b_xnT, writes=[bpk])
            S.op("act", lambda e, h=h, pk=pk: e.copy(out=KTs[:, h, :], in_=pk[:]), reads=[bpk], writes=[bKTs])
        S.dma("sp", KTo[m].rearrange("h d t -> d h t"), KTs[:], reads=[bKTs])
        for tt in range(4):
            vs, bvs = Vs[tt % 2], bVs[tt % 2]
            for nb in range(2):
                pk, bpk = PK[(tt * 2 + nb) % 4], bPK[(tt * 2 + nb) % 4]
                for kc in range(8):
                    S.op("pe", lambda e, tt=tt, nb=nb, kc=kc, pk=pk: e.matmul(pk[:], lhsT=xnT[:, kc, tt * 128:(tt + 1) * 128],
                                                                             rhs=kvw[:, kc, 1024 + nb * 512:1024 + (nb + 1) * 512], start=(kc == 0), stop=(kc == 7)),
                         reads=[b_kvw, b_xnT[tt]], writes=[bpk])
                S.op("dve", lambda e, nb=nb, pk=pk, vs=vs: e.tensor_copy(out=vs[:, nb * 512:(nb + 1) * 512], in_=pk[:]), reads=[bpk], writes=[bvs])
            S.dma("sp", Vo[m * 512 + tt * 128:m * 512 + (tt + 1) * 128, :], vs[:], reads=[bvs])
        S.barrier()


def emit_store(cx, HM, bHM, m, out):
    S = cx.S
    for tt in range(4):
        S.dma("sp", out[m * 512 + tt * 128:m * 512 + (tt + 1) * 128, :], HM[:, tt, :], reads=[bHM[tt]])


def emit_final(cx, consts, HM, bHM, m, g_dram, out):
    S = cx.S
    with ExitStack() as es:
        G = cx.sb(es, [128, 1024]); bg = S.buf("fg")
        S.dma("pool", G[:], g_dram[0:1, :].partition_broadcast(128), writes=[bg])
        junk = cx.sb(es, [128, 1024], BF16); st = cx.sb(es, [128, 8]); bj, bst = S.buf(), S.buf()
        o = [cx.sb(es, [128, 1024]) for _ in range(2)]; bo = S.bufs(2, "fo")
        for tt in range(4):
            S.op("act", lambda e, tt=tt: e.activation(out=junk[:], in_=HM[:, tt, :], func=AF.Square, accum_out=st[:, 2 * tt:2 * tt + 1]),
                 reads=[bHM[tt]], writes=[bj, bst])
            emit_rstd(cx, st[:, 2 * tt:2 * tt + 1], st[:, 2 * tt + 1:2 * tt + 2], 1.0 / D, bst, bst)
            S.op("dve", lambda e, tt=tt: e.scalar_tensor_tensor(out=o[tt % 2][:], in0=HM[:, tt, :], scalar=st[:, 2 * tt + 1:2 * tt + 2], in1=G[:], op0=ALU.mult, op1=ALU.mult),
                 reads=[bHM[tt], bst, bg], writes=[bo[tt % 2]])
            S.dma("sp", out[m * 512 + tt * 128:m * 512 + (tt + 1) * 128, :], o[tt % 2][:], reads=[bo[tt % 2]])
        S.barrier()


def build_l1(NM, do_peer=True):
    SQ = 2048 * NM
    NQG = 4 * NM
    nc = bass.Bass("TRN2", target_bir_lowering=False)
    with ExitStack() as es:
        cx = Ctx(nc, es)
        S = cx.S
        I = lambda n, s, dt=F32: nc.dram_tensor(n, list(s), dt, kind="ExternalInput").ap()
        O = lambda n, s, dt=F32: nc.dram_tensor(n, list(s), dt, kind="ExternalOutput").ap()
        xb = I("xb", [SQ, D]); xo = I("xo", [NM * 512, D]); cT = I("cT", [128, 8])
        ada_w = I("ada_w", [D, 6 * D]); ada_b = I("ada_b", [1, 6 * D]); kva_w = I("kva_w", [D, 2 * D]); kva_b = I("kva_b", [1, 2 * D])
        gmix = I("gmix", [1, D]); gffn = I("gffn", [1, D]); gkv = I("gkv", [1, D])
        win = I("win", [D, 4 * D]); wout = I("wout", [D, D]); ongT = I("ongT", [128, 8]); lbl = I("lbl", [2, D])
        hcA = I("hcA", [128, 393]); hcB = I("hcB", [1, 1024]); oh = I("oh", [1, 4])
        kvw = I("kvw", [D, 2 * D]); pwq = I("pwq", [D, D]); skT = I("skT", [8, 128, 128])
        uT = I("uT", [D, NEXP]); v = I("v", [NEXP, D])
        h1 = O("h1", [NM * 512, D]); KTo = O("KTo", [NM, 8, 128, 512], BF16); Vo = O("Vo", [NM * 512, D], BF16)
        mod = cx.dram("mod", [128, 8 * D], F32)
        snapt = cx.dram("snap", [NQG, 128, D], F32)
        snap = [snapt[g] for g in range(NQG)]
        uTb = cx.dram("uTb", [D, NEXP], BF16); vb = cx.dram("vb", [NEXP, D], BF16)
        consts = make_consts(cx, es)
        emit_mod(cx, cT, [(ada_w, ada_b, 0, 6 * D), (kva_w, kva_b, 6 * D, 2 * D)], mod)
        emit_geff(cx, mod, 1 * D, gmix)
        emit_geff(cx, mod, 4 * D, gffn)
        emit_geff(cx, mod, 7 * D, gkv)
        if do_peer:
            emit_precast(cx, uT, uTb, D, NEXP)
            emit_precast(cx, v, vb, NEXP, D)
        winb = cx.dram("winb", [D, 4 * D], BF16); woutb = cx.dram("woutb", [D, D], BF16)
        kvwb = cx.dram("kvwb", [D, 2 * D], BF16); pwqb = cx.dram("pwqb", [D, D], BF16)
        emit_precast(cx, win, winb, D, 4 * D)
        emit_precast(cx, kvw, kvwb, D, 2 * D)
        emit_precast(cx, pwq, pwqb, D, D)
        emit_fold_cast(cx, wout, woutb, rowT_dram=ongT, col_dram=mod, col0=2 * D)
        HM = cx.sb(es, [128, 4, D]); bHM = S.bufs(4, "HM")
        emit_hgrn(cx, consts, HM, bHM, "state", xb, 4 * (NQG - 1), snap, mod, 0, D, 2 * D, winb, woutb, ongT, lbl, hcA, hcB)
        for m in range(NM):
            emit_hgrn(cx, consts, HM, bHM, "full", xo, 4, snap, mod, 0, D, 2 * D, winb, woutb, ongT, lbl, hcA, hcB, onehot_dram=oh, m=m)
            if do_peer:
                emit_peer(cx, consts, HM, bHM, mod, 3 * D, 4 * D, 5 * D, pwqb, skT, uTb, vb)
            emit_store(cx, HM, bHM, m, h1)
            emit_kv(cx, consts, HM, bHM, m, mod, 6 * D, 7 * D, kvwb, KTo, Vo)
        S.barrier()
        S.emit()
    return nc


def build_l2(NM, do_peer=True):
    SQ = 2048 * NM
    nc = bass.Bass("TRN2", target_bir_lowering=False)
    with ExitStack() as es:
        cx = Ctx(nc, es)
        S = cx.S
        I = lambda n, s, dt=F32: nc.dram_tensor(n, list(s), dt, kind="ExternalInput").ap()
        O = lambda n, s, dt=F32: nc.dram_tensor(n, list(s), dt, kind="ExternalOutput").ap()
        h1 = I("h1", [NM * 512, D]); cT = I("cT", [128, 8])
        ada_w = I("ada_w", [D, 6 * D]); ada_b = I("ada_b", [1, 6 * D])
        gmix = I("gmix", [1, D]); gffn = I("gffn", [1, D]); gfin = I("gfin", [1, D])
        sbwq = I("sbwq", [D, D]); sbwo = I("sbwo", [D, D])
        KT = I("KT", [8, 128, SQ], BF16); V = I("V", [8, 128, SQ // 128, 128], BF16)
        ac = I("ac", [128, 256], BF16); mask = I("mask", [128, 16, 512], BF16)
        pwq = I("pwq", [D, D]); skT = I("skT", [8, 128, 128]); uT = I("uT", [D, NEXP]); v = I("v", [NEXP, D])
        out = O("out", [NM * 512, D])
        mod = cx.dram("mod", [128, 6 * D], F32)
        uTb = cx.dram("uTb", [D, NEXP], BF16); vb = cx.dram("vb", [NEXP, D], BF16)
        consts = make_consts(cx, es)
        emit_mod(cx, cT, [(ada_w, ada_b, 0, 6 * D)], mod)
        emit_geff(cx, mod, 1 * D, gmix)
        emit_geff(cx, mod, 4 * D, gffn)
        if do_peer:
            emit_precast(cx, uT, uTb, D, NEXP)
            emit_precast(cx, v, vb, NEXP, D)
        sbwqb = cx.dram("sbwqb", [D, D], BF16); sbwob = cx.dram("sbwob", [D, D], BF16); pwqb = cx.dram("pwqb", [D, D], BF16)
        emit_precast(cx, sbwq, sbwqb, D, D)
        emit_precast(cx, pwq, pwqb, D, D)
        emit_fold_cast(cx, sbwo, sbwob, rowT_dram=None, col_dram=mod, col0=2 * D)
        HM = cx.sb(es, [128, 4, D]); bHM = S.bufs(4, "HM")
        for m in range(NM):
            for tt in range(4):
                S.dma("sp", HM[:, tt, :], h1[m * 512 + tt * 128:m * 512 + (tt + 1) * 128, :], writes=[bHM[tt]])
            emit_attn(cx, consts, HM, bHM, m, mod, 0, D, 2 * D, sbwqb, sbwob, KT, V, ac, mask)
            if do_peer:
                emit_peer(cx, consts, HM, bHM, mod, 3 * D, 4 * D, 5 * D, pwqb, skT, uTb, vb)
            emit_final(cx, consts, HM, bHM, m, gfin, out)
        S.barrier()
        S.emit()
    return nc


def run_model(inp, NM, do_peer=True, runner=None):
    f32 = lambda a: np.ascontiguousarray(np.asarray(a, dtype=np.float32))
    x = f32(inp["x"]); c = f32(inp["c"])
    B = x.shape[0]
    SQ = 2048 * NM
    assert x.shape == (B, SQ, D) and B == 2
    ncores = 8
    if runner is None:
        runner = lambda nc, maps: run_bass_kernel_spmd(nc, maps, core_ids=list(range(len(maps)))).results
    hcA, hcB = hgrn_consts_host()
    row = lambda a: f32(a).reshape(1, -1)
    colT = lambda a: np.ascontiguousarray(f32(a).reshape(8, 128).T)
    own = lambda b, r: np.concatenate([np.arange((4 * m + r) * 512, (4 * m + r + 1) * 512) for m in range(NM)])

    def peer_w(l):
        sk = f32(inp["peer_subkeys"][l]).reshape(8, 128, 128)
        return {"pwq": f32(inp["peer_w_q"][l]), "skT": np.ascontiguousarray(sk.transpose(0, 2, 1)),
                "uT": np.ascontiguousarray(f32(inp["peer_u"][l]).T), "v": f32(inp["peer_v"][l])}

    pw0 = peer_w(0)
    shared1 = {"ada_w": f32(inp["ada_w"][0]), "ada_b": row(inp["ada_b"][0]), "kva_w": f32(inp["kv_ada_w"]), "kva_b": row(inp["kv_ada_b"]),
               "gmix": row(inp["norm_mix_g"][0]), "gffn": row(inp["norm_ffn_g"][0]), "gkv": row(inp["kv_norm_g"]),
               "win": f32(inp["hgrn_w_in"][0]), "wout": f32(inp["hgrn_w_out"][0]), "ongT": colT(inp["hgrn_onorm_g"][0]),
               "lbl": f32(inp["hgrn_lb_logits"]), "hcA": hcA, "hcB": hcB, "kvw": f32(inp["kv_w"]), **pw0}
    maps = []
    for core in range(ncores):
        b, r = core // 4, core % 4
        oh = np.zeros((1, 4), np.float32); oh[0, r] = 1.0
        maps.append({"xb": x[b], "xo": np.ascontiguousarray(x[b][own(b, r)]), "cT": colT(c[b]), "oh": oh, **shared1})
    nc1 = build_l1(NM, do_peer)
    res1 = runner(nc1, maps)
    del maps, shared1, pw0
    KTf = np.zeros((B, 8, 128, SQ), ml_dtypes.bfloat16)
    Vf = np.zeros((B, SQ, D), ml_dtypes.bfloat16)
    for core in range(ncores):
        b, r = core // 4, core % 4
        kto = np.asarray(res1[core]["KTo"]); vo = np.asarray(res1[core]["Vo"])
        for m in range(NM):
            g = 4 * m + r
            KTf[b, :, :, g * 512:(g + 1) * 512] = kto[m]
            Vf[b, g * 512:(g + 1) * 512] = vo[m * 512:(m + 1) * 512]
    Vl = np.ascontiguousarray(Vf.reshape(B, SQ // 128, 128, 8, 128).transpose(0, 3, 2, 1, 4))
    pw1 = peer_w(1)
    shared2 = {"ada_w": f32(inp["ada_w"][1]), "ada_b": row(inp["ada_b"][1]), "gmix": row(inp["norm_mix_g"][1]), "gffn": row(inp["norm_ffn_g"][1]),
               "gfin": row(inp["final_norm_g"]), "sbwq": f32(inp["sb_w_q"][0]), "sbwo": f32(inp["sb_w_out"][0]), **pw1}
    maps = []
    for core in range(ncores):
        b, r = core // 4, core % 4
        ac, mask = attn_consts_host(r)
        maps.append({"h1": np.asarray(res1[core]["h1"]), "cT": colT(c[b]), "KT": KTf[b], "V": Vl[b], "ac": ac, "mask": mask, **shared2})
    nc2 = build_l2(NM, do_peer)
    res2 = runner(nc2, maps)
    out = np.zeros((B, SQ, D), np.float32)
    for core in range(ncores):
        b, r = core // 4, core % 4
        out[b, own(b, r)] = np.asarray(res2[core]["out"])
    return out


def kernel(**inputs):
    return run_model(inputs, 8)
```

```python
from contextlib import ExitStack
import math
import numpy as np
import ml_dtypes
import concourse.bass as bass
import concourse.mybir as mybir
from concourse.bass_utils import run_bass_kernel_spmd

F32 = mybir.dt.float32
BF16 = mybir.dt.bfloat16
ALU = mybir.AluOpType
AF = mybir.ActivationFunctionType
AX = mybir.AxisListType


class Buf:
    __slots__ = ("name", "w", "r")

    def __init__(self, name):
        self.name = name
        self.w = None
        self.r = []


class Sched:
    ENGS = ("pe", "act", "dve", "pool", "sp")
    NDMA = 8
    ROT = 20000

    def __init__(self, nc, es):
        self.nc = nc
        self.streams = {e: [] for e in self.ENGS}
        self.sem = {}
        self.cnt = {}
        for e in self.ENGS:
            self.sem[e] = es.enter_context(nc.semaphore("s_" + e))
            self.cnt[e] = 0
        self.dsem = {}
        self.dcnt = {}
        for e in ("sp", "act", "pool"):
            self.dsem[e] = [es.enter_context(nc.semaphore("d_%s%d" % (e, i))) for i in range(self.NDMA)]
            self.dcnt[e] = 0
        self.waited = {e: {} for e in self.ENGS}
        self.nbuf = 0
        self.es = es
        self.nrot = 0

    def buf(self, name=None):
        self.nbuf += 1
        return Buf(name or "b%d" % self.nbuf)

    def bufs(self, n, name="b"):
        return [self.buf("%s%d" % (name, i)) for i in range(n)]

    def _need(self, eng, reads, writes, same_ok):
        need = {}

        def add(tok):
            if tok is None:
                return
            sem, val, src = tok
            if same_ok and src == eng:
                return
            k = id(sem)
            if k not in need or need[k][1] < val:
                need[k] = (sem, val)

        for b in reads:
            add(b.w)
        for b in writes:
            add(b.w)
            for t in b.r:
                add(t)
        out = []
        wd = self.waited[eng]
        for k, (sem, val) in need.items():
            if wd.get(k, 0) >= val:
                continue
            wd[k] = val
            out.append((sem, val))
        return out

    def op(self, eng, fn, reads=(), writes=()):
        waits = self._need(eng, reads, writes, same_ok=(eng == "pe"))
        self.cnt[eng] += 1
        tok = (self.sem[eng], self.cnt[eng], eng)
        self.streams[eng].append((waits, fn, (self.sem[eng], 1)))
        for b in reads:
            b.r.append(tok)
        for b in writes:
            b.w = tok
            b.r = []
        return tok

    def dma(self, q, out, in_, reads=(), writes=(), **kw):
        j = self.dcnt[q]
        self.dcnt[q] += 1
        sem = self.dsem[q][j % self.NDMA]
        val = 16 * (j // self.NDMA + 1)
        waits = self._need(q, reads, writes, same_ok=False)
        if j >= self.NDMA:
            prev = val - 16
            wd = self.waited[q]
            if wd.get(id(sem), 0) < prev:
                wd[id(sem)] = prev
                waits.append((sem, prev))
        tok = (sem, val, "dma_" + q)
        self.streams[q].append((waits, lambda e: e.dma_start(out=out, in_=in_, **kw), (sem, 16)))
        for b in reads:
            b.r.append(tok)
        for b in writes:
            b.w = tok
            b.r = []
        return tok

    def coll(self, kind, groups, src, dst, reads=(), writes=()):
        q = "pool"
        j = self.dcnt[q]
        self.dcnt[q] += 1
        sem = self.dsem[q][j % self.NDMA]
        val = 16 * (j // self.NDMA + 1)
        waits = self._need(q, reads, writes, same_ok=False)
        if j >= self.NDMA:
            prev = val - 16
            wd = self.waited[q]
            if wd.get(id(sem), 0) < prev:
                wd[id(sem)] = prev
                waits.append((sem, prev))
        tok = (sem, val, "dma_" + q)
        self.streams[q].append((waits, lambda e: e.collective_compute(kind, ALU.bypass, groups, [src], [dst]), (sem, 16)))
        for b in reads:
            b.r.append(tok)
        for b in writes:
            b.w = tok
            b.r = []
        return tok

    def wait_all(self, eng, bufs):
        waits = self._need(eng, bufs, (), same_ok=False)
        self.streams[eng].append((waits, None, None))

    def barrier(self):
        toks = []
        for e in self.ENGS:
            if self.cnt[e]:
                toks.append((self.sem[e], self.cnt[e]))
        for q in self.dsem:
            j = self.dcnt[q]
            for s in range(min(j, self.NDMA)):
                last = ((j - 1 - s) // self.NDMA) if j - 1 >= s else -1
                uses = (j - s + self.NDMA - 1) // self.NDMA
                toks.append((self.dsem[q][s], 16 * uses))
        for e in self.ENGS:
            waits = []
            wd = self.waited[e]
            for sem, val in toks:
                if wd.get(id(sem), 0) >= val:
                    continue
                wd[id(sem)] = val
                waits.append((sem, val))
            if waits:
                self.streams[e].append((waits, None, None))
        for e in self.ENGS:
            if self.cnt[e] > self.ROT:
                self.sem[e] = self.es.enter_context(self.nc.semaphore("s_%s_%d" % (e, self.nrot)))
                self.nrot += 1
                self.cnt[e] = 0
        for q in self.dsem:
            if 16 * (self.dcnt[q] // self.NDMA + 1) > self.ROT:
                self.dsem[q] = [self.es.enter_context(self.nc.semaphore("d_%s%d_%d" % (q, i, self.nrot))) for i in range(self.NDMA)]
                self.nrot += 1
                self.dcnt[q] = 0

    def scatter(self, out, idx_ap, in_, reads=(), writes=()):
        q = "pool"
        j = self.dcnt[q]
        self.dcnt[q] += 1
        sem = self.dsem[q][j % self.NDMA]
        val = 16 * (j // self.NDMA + 1)
        waits = self._need(q, reads, writes, same_ok=False)
        if j >= self.NDMA:
            prev = val - 16
            wd = self.waited[q]
            if wd.get(id(sem), 0) < prev:
                wd[id(sem)] = prev
                waits.append((sem, prev))
        tok = (sem, val, "dma_" + q)
        self.streams[q].append((waits, lambda e: e.indirect_dma_start(out=out, out_offset=bass.IndirectOffsetOnAxis(ap=idx_ap, axis=0),
                                                                      in_=in_, in_offset=None, bounds_check=out.shape[0] - 1, oob_is_err=False), (sem, 16)))
        for b in reads:
            b.r.append(tok)
        for b in writes:
            b.w = tok
            b.r = []
        return tok

    def core_barrier(self):
        self.barrier()
        self.emit()
        self.streams = {e: [] for e in self.ENGS}
        self.nc.all_core_barrier()

    def emit(self):
        nc = self.nc
        handles = {"pe": "tensor", "act": "scalar", "dve": "vector", "pool": "gpsimd", "sp": "sync"}
        with nc.Block() as block:
            for e in self.ENGS:
                stream = self.streams[e]

                def body(eng, stream=stream):
                    for waits, fn, inc in stream:
                        for sem, val in waits:
                            eng.wait_ge(sem, val)
                        if fn is not None:
                            ins = fn(eng)
                            ins.then_inc(inc[0], inc[1])

                getattr(block, handles[e])(body)


D = 1024
NEXP = 16384
EPS = 1e-6


class Ctx:
    def __init__(self, nc, es):
        self.nc = nc
        self.es = es
        self.S = Sched(nc, es)
        self.n = 0

    def sb(self, es, shape, dt=F32, name=None):
        self.n += 1
        return es.enter_context(self.nc.sbuf_tensor(name or "t%d" % self.n, list(shape), dt))

    def ps(self, es, shape, dt=F32, name=None):
        self.n += 1
        return es.enter_context(self.nc.psum_tensor(name or "p%d" % self.n, list(shape), dt))

    def dram(self, name, shape, dt, kind="Internal"):
        return self.nc.dram_tensor(name, list(shape), dt, kind=kind).ap()


def make_consts(cx, es):
    S = cx.S
    c = {}
    identf = cx.sb(es, [128, 128], F32)
    identb = cx.sb(es, [128, 128], BF16)
    b = S.buf("ident")
    S.op("pool", lambda e: e.memset(identf[:], 1.0), writes=[b])
    S.op("pool", lambda e: e.affine_select(out=identf[:], in_=identf[:], pattern=[[-1, 128]],
                                           compare_op=ALU.is_equal, fill=0.0, base=0, channel_multiplier=1),
         reads=[b], writes=[b])
    S.op("dve", lambda e: e.tensor_copy(out=identb[:], in_=identf[:]), reads=[b], writes=[b])
    c["identf"], c["identb"], c["b_ident"] = identf, identb, b
    return c


def emit_mod(cx, cT_ap, specs, out_dram):
    S = cx.S
    with ExitStack() as es:
        cs = cx.sb(es, [128, 8])
        sg = cx.sb(es, [128, 8])
        CA = cx.sb(es, [128, 8, 128])
        wst = [cx.sb(es, [128, 2048]) for _ in range(2)]
        bwst = S.bufs(2, "wst")
        bias = cx.sb(es, [128, 2048])
        res = cx.sb(es, [128, 2048])
        pm = cx.ps(es, [128, 4, 512])
        b_cs, b_CA, b_bias, b_res, b_pm = S.buf(), S.buf(), S.buf(), S.buf(), S.buf()
        S.dma("sp", cs[:], cT_ap, writes=[b_cs])
        S.op("act", lambda e: e.activation(out=sg[:], in_=cs[:], func=AF.Sigmoid), reads=[b_cs], writes=[b_CA])
        S.op("dve", lambda e: e.tensor_tensor(out=cs[:], in0=cs[:], in1=sg[:], op=ALU.mult), reads=[b_cs, b_CA], writes=[b_cs])
        S.op("dve", lambda e: e.tensor_copy(out=CA[:], in_=cs[:].unsqueeze(2).to_broadcast([128, 8, 128])),
             reads=[b_cs], writes=[b_CA])
        it = 0
        for (W, B, col0, ncols) in specs:
            for cb in range(0, ncols, 2048):
                S.dma("pool", bias[:], B[0:1, cb:cb + 2048].partition_broadcast(128), writes=[b_bias])
                for kc in range(8):
                    w = wst[it % 2]
                    bw = bwst[it % 2]
                    it += 1
                    S.dma("sp", w[:], W[kc * 128:(kc + 1) * 128, cb:cb + 2048], writes=[bw])
                    for nb in range(4):
                        S.op("pe", lambda e, w=w, nb=nb, kc=kc: e.matmul(pm[:, nb, :], lhsT=CA[:, kc, :], rhs=w[:, nb * 512:(nb + 1) * 512],
                                                                        start=(kc == 0), stop=(kc == 7)),
                             reads=[b_CA, bw], writes=[b_pm])
                S.op("dve", lambda e: e.tensor_tensor(out=res[:], in0=pm[:].rearrange("p a b -> p (a b)"), in1=bias[:], op=ALU.add),
                     reads=[b_pm, b_bias], writes=[b_res])
                S.dma("sp", out_dram[:, col0 + cb:col0 + cb + 2048], res[:], reads=[b_res])
        S.barrier()


def emit_rstd(cx, ss_ap, rstd_ap, inv_n, b_ss, b_rstd):
    S = cx.S
    S.op("dve", lambda e: e.tensor_scalar(out=rstd_ap, in0=ss_ap, scalar1=inv_n, scalar2=EPS, op0=ALU.mult, op1=ALU.add),
         reads=[b_ss], writes=[b_rstd])
    S.op("act", lambda e: e.activation(out=rstd_ap, in_=rstd_ap, func=AF.Ln), reads=[b_rstd], writes=[b_rstd])
    S.op("act", lambda e: e.activation(out=rstd_ap, in_=rstd_ap, func=AF.Exp, scale=-0.5), reads=[b_rstd], writes=[b_rstd])


class NormT:
    def __init__(self, cx, es, consts, pt=None, b_pt=None):
        self.cx = cx
        self.c = consts
        S = cx.S
        self.junk = cx.sb(es, [128, 1024], BF16)
        self.st = cx.sb(es, [128, 4])
        self.tmp = cx.sb(es, [128, 1024])
        self.hb = cx.sb(es, [128, 1024], BF16)
        self.pt = pt if pt is not None else cx.ps(es, [128, 8, 128], BF16)
        self.b_junk, self.b_st, self.b_tmp, self.b_hb = S.buf(), S.buf(), S.buf(), S.buf()
        self.b_pt = b_pt if b_pt is not None else S.buf()

    def norm(self, x_ap, b_x, geff, shift, b_mod, out_ap, b_out, out_eng="pool"):
        S = self.cx.S
        st, tmp = self.st, self.tmp
        S.op("act", lambda e: e.activation(out=self.junk[:], in_=x_ap, func=AF.Square, accum_out=st[:, 0:1]),
             reads=[b_x], writes=[self.b_junk, self.b_st])
        emit_rstd(self.cx, st[:, 0:1], st[:, 1:2], 1.0 / D, self.b_st, self.b_st)
        S.op("dve", lambda e: e.scalar_tensor_tensor(out=tmp[:], in0=x_ap, scalar=st[:, 1:2], in1=geff, op0=ALU.mult, op1=ALU.mult),
             reads=[b_x, self.b_st, b_mod], writes=[self.b_tmp])
        S.op(out_eng, lambda e: e.tensor_tensor(out=out_ap, in0=tmp[:], in1=shift, op=ALU.add),
             reads=[self.b_tmp, b_mod], writes=[b_out])

    def transpose(self, in_bf, b_in, outT_ap, b_out):
        S = self.cx.S
        for kc in range(8):
            S.op("pe", lambda e, kc=kc: e.transpose(self.pt[:, kc, :], in_bf[:, kc * 128:(kc + 1) * 128], self.c["identb"][:]),
                 reads=[b_in, self.c["b_ident"]], writes=[self.b_pt])
        S.op("act", lambda e: e.copy(out=outT_ap, in_=self.pt[:, :, :]), reads=[self.b_pt], writes=[b_out])

    def normT(self, x_ap, b_x, geff, shift, b_mod, outT_ap, b_out):
        self.norm(x_ap, b_x, geff, shift, b_mod, self.hb[:], self.b_hb)
        self.transpose(self.hb, self.b_hb, outT_ap, b_out)


def load_cast(cx, es_tmp, q, dst_ap_fn, src_ap_fn, nchunks, shape, b_dst, cast_eng="pool"):
    S = cx.S
    stg = [cx.sb(es_tmp, shape) for _ in range(2)]
    bst = S.bufs(2, "stg")
    for i in range(nchunks):
        s, b = stg[i % 2], bst[i % 2]
        S.dma(q, s[:], src_ap_fn(i), writes=[b])
        S.op(cast_eng, lambda e, s=s, i=i: e.tensor_copy(out=dst_ap_fn(i), in_=s[:]), reads=[b], writes=[b_dst])


def emit_geff(cx, mod_dram, col, g_dram):
    S = cx.S
    with ExitStack() as es:
        a = cx.sb(es, [128, 1024])
        g = cx.sb(es, [128, 1024])
        ba, bg = S.buf(), S.buf()
        S.dma("sp", a[:], mod_dram[:, col:col + 1024], writes=[ba])
        S.dma("pool", g[:], g_dram[0:1, :].partition_broadcast(128), writes=[bg])
        S.op("dve", lambda e: e.scalar_tensor_tensor(out=a[:], in0=a[:], scalar=1.0, in1=g[:], op0=ALU.add, op1=ALU.mult),
             reads=[ba, bg], writes=[ba])
        S.dma("sp", mod_dram[:, col:col + 1024], a[:], reads=[ba])
        S.barrier()


def emit_precast(cx, src, dst, rows, cols):
    S = cx.S
    with ExitStack() as es:
        CW = 4096
        st = [cx.sb(es, [128, CW]) for _ in range(2)]
        ob = [cx.sb(es, [128, CW], BF16) for _ in range(2)]
        bs, bo = S.bufs(2), S.bufs(2)
        srcv = src.rearrange("(a p) c -> p a c", p=128)
        dstv = dst.rearrange("(a p) c -> p a c", p=128)
        i = 0
        per = max(1, CW // cols)
        cw = min(CW, cols)
        for a0 in range(0, rows // 128, per):
            for c0 in range(0, cols, cw):
                s, o = st[i % 2], ob[i % 2]
                sv = s[:].rearrange("p (a c) -> p a c", a=per)
                ov = o[:].rearrange("p (a c) -> p a c", a=per)
                S.dma("sp" if i % 2 == 0 else "act", sv, srcv[:, a0:a0 + per, c0:c0 + cw], writes=[bs[i % 2]])
                eng = ("pool", "dve")[i % 2]
                S.op(eng, lambda e, s=s, o=o: e.tensor_copy(out=o[:], in_=s[:]), reads=[bs[i % 2]], writes=[bo[i % 2]])
                S.dma("pool", dstv[:, a0:a0 + per, c0:c0 + cw], ov, reads=[bo[i % 2]])
                i += 1
        S.barrier()


def emit_fold_cast(cx, src, dst, rowT_dram=None, col_dram=None, col0=0):
    S = cx.S
    with ExitStack() as es:
        st = [cx.sb(es, [128, 1024]) for _ in range(2)]; ob = [cx.sb(es, [128, 1024], BF16) for _ in range(2)]
        bs, bo = S.bufs(2), S.bufs(2)
        bc = S.buf()
        if rowT_dram is not None:
            rT = cx.sb(es, [128, 8]); S.dma("sp", rT[:], rowT_dram, writes=[bc])
        if col_dram is not None:
            cB = cx.sb(es, [128, 1024]); S.dma("sp", cB[:], col_dram[:, col0:col0 + 1024], writes=[bc])
        for kc in range(8):
            s, o = st[kc % 2], ob[kc % 2]
            S.dma("sp", s[:], src[kc * 128:(kc + 1) * 128, :], writes=[bs[kc % 2]])
            if rowT_dram is not None:
                S.op("dve", lambda e, s=s, kc=kc: e.tensor_scalar(out=s[:], in0=s[:], scalar1=rT[:, kc:kc + 1], scalar2=None, op0=ALU.mult),
                     reads=[bs[kc % 2], bc], writes=[bs[kc % 2]])
            if col_dram is not None:
                S.op("dve", lambda e, s=s, o=o: e.tensor_tensor(out=o[:], in0=s[:], in1=cB[:], op=ALU.mult), reads=[bs[kc % 2], bc], writes=[bo[kc % 2]])
            else:
                S.op("dve", lambda e, s=s, o=o: e.tensor_copy(out=o[:], in_=s[:]), reads=[bs[kc % 2]], writes=[bo[kc % 2]])
            S.dma("act", dst[kc * 128:(kc + 1) * 128, :], o[:], reads=[bo[kc % 2]])
        S.barrier()


def load_w(cx, dst, src_bf, b_dst, ncol):
    S = cx.S
    v = src_bf.rearrange("(kc p) n -> p kc n", p=128)
    S.dma("sp", dst[:, 0:4, :], v[:, 0:4, :], writes=[b_dst])
    S.dma("act", dst[:, 4:8, :], v[:, 4:8, :], writes=[b_dst])


PEER_XENG = ("dve", "dve", "dve", "dve")


def emit_peer(cx, consts, HM, bHM, mod_dram, c_sh, c_ge, c_g, wq_dram, skT_dram, uTb, vb):
    S = cx.S
    GA = 4
    NG = 128 // GA
    with ExitStack() as es:
        G2 = cx.sb(es, [128, 1024]); b_g2 = S.buf("g2")
        S.dma("sp", G2[:], mod_dram[:, c_g:c_g + 1024], writes=[b_g2])
        xnT = cx.sb(es, [128, 8, 512], BF16); b_xnT = S.bufs(4, "xnT")
        QA = cx.sb(es, [128, 4, 1024]); bQA = S.bufs(4, "QA")
        PSA = cx.ps(es, [128, 4, 512]); bPSA = S.bufs(4, "PSA")
        Pexp = cx.sb(es, [128, 4, 8, 128]); bP = S.bufs(4, "Pexp")
        TH = cx.sb(es, [128, 4, 4]); RZ = cx.sb(es, [128, 4, 4]); P1n = cx.sb(es, [128, 4, 4, 128]); bTH = S.bufs(4, "TH")
        with ExitStack() as es1:
            SH = cx.sb(es1, [128, 1024]); GE = cx.sb(es1, [128, 1024]); b_mod = S.buf("mod")
            S.dma("sp", SH[:], mod_dram[:, c_sh:c_sh + 1024], writes=[b_mod])
            S.dma("sp", GE[:], mod_dram[:, c_ge:c_ge + 1024], writes=[b_mod])
            wq = cx.sb(es1, [128, 8, 1024], BF16); b_wq = S.buf("wq")
            load_w(cx, wq, wq_dram, b_wq, 1024)
            skT = cx.sb(es1, [128, 8, 128]); b_sk = S.buf("sk")
            S.dma("sp", skT[:], skT_dram.rearrange("h d n -> d h n"), writes=[b_sk])
            nt = NormT(cx, es1, consts)
            top = cx.sb(es1, [128, 8, 16]); work = cx.sb(es1, [128, 128]); cand = cx.sb(es1, [128, 256]); work2 = cx.sb(es1, [128, 256])
            c24 = cx.sb(es1, [128, 24]); mx = cx.sb(es1, [128, 8]); zs = cx.sb(es1, [128, 4]); th4 = cx.sb(es1, [128, 4])
            b_sm = S.buf("small")
            for tt in range(4):
                nt.normT(HM[:, tt, :], bHM[tt], GE[:], SH[:], b_mod, xnT[:, :, tt * 128:(tt + 1) * 128], b_xnT[tt])
            for hp in range(8):
                bank = hp % 4
                for kc in range(8):
                    S.op("pe", lambda e, hp=hp, kc=kc, bank=bank: e.matmul(PSA[:, bank, :], lhsT=wq[:, kc, hp * 128:(hp + 1) * 128], rhs=xnT[:, kc, :],
                                                                           start=(kc == 0), stop=(kc == 7)),
                         reads=[b_wq] + b_xnT, writes=[bPSA[bank]])
                S.op("act" if hp % 2 else "dve", (lambda e, hp=hp, bank=bank: e.copy(out=QA[:, hp // 2, (hp % 2) * 512:(hp % 2 + 1) * 512], in_=PSA[:, bank, :])) if hp % 2 else
                     (lambda e, hp=hp, bank=bank: e.tensor_copy(out=QA[:, hp // 2, (hp % 2) * 512:(hp % 2 + 1) * 512], in_=PSA[:, bank, :])),
                     reads=[bPSA[bank]], writes=[bQA[hp // 2]])
            for tt in range(4):
                pb = 2 * (tt % 2)
                for hp in range(8):
                    S.op("pe", lambda e, hp=hp, tt=tt, pb=pb: e.matmul(PSA[:, pb + hp // 4, (hp % 4) * 128:(hp % 4 + 1) * 128],
                                                                      lhsT=QA[:, hp // 2, (hp % 2) * 512 + tt * 128:(hp % 2) * 512 + (tt + 1) * 128],
                                                                      rhs=skT[:, hp, :], start=True, stop=True),
                         reads=[bQA[hp // 2], b_sk], writes=[bPSA[pb + hp // 4]])
                scv = PSA[:, pb:pb + 2, :].rearrange("p a (h n) -> p (a h) n", n=128)
                S.op("dve", lambda e, scv=scv: e.reduce_max(out=mx[:], in_=scv, axis=AX.X), reads=[bPSA[pb], bPSA[pb + 1]], writes=[b_sm])
                S.op("dve", lambda e: e.tensor_scalar(out=mx[:], in0=mx[:], scalar1=-1.0, scalar2=None, op0=ALU.mult), reads=[b_sm], writes=[b_sm])
                for hp in range(8):
                    S.op("act", lambda e, hp=hp, tt=tt, pb=pb: e.activation(out=Pexp[:, tt, hp, :], in_=PSA[:, pb + hp // 4, (hp % 4) * 128:(hp % 4 + 1) * 128],
                                                                           func=AF.Exp, bias=mx[:, hp:hp + 1], scale=1.0),
                         reads=[bPSA[pb + hp // 4], b_sm], writes=[bP[tt]])
                for hp in range(8):
                    S.op("dve", lambda e, hp=hp, tt=tt: e.max(out=top[:, hp, 0:8], in_=Pexp[:, tt, hp, :]), reads=[bP[tt]], writes=[b_sm])
                    S.op("dve", lambda e, hp=hp, tt=tt: e.match_replace(out=work[:], in_to_replace=top[:, hp, 0:8], in_values=Pexp[:, tt, hp, :], imm_value=-1.0),
                         reads=[bP[tt], b_sm], writes=[b_sm])
                    S.op("dve", lambda e, hp=hp: e.max(out=top[:, hp, 8:16], in_=work[:]), reads=[b_sm], writes=[b_sm])
                for h in range(4):
                    S.op("dve", lambda e, h=h: e.tensor_tensor(out=cand[:].rearrange("p (a b) -> p a b", a=16),
                                                               in0=top[:, 2 * h, :].unsqueeze(2).to_broadcast([128, 16, 16]),
                                                               in1=top[:, 2 * h + 1, :].unsqueeze(1).to_broadcast([128, 16, 16]), op=ALU.mult),
                         reads=[b_sm], writes=[b_sm])
                    S.op("dve", lambda e: e.max(out=c24[:, 0:8], in_=cand[:]), reads=[b_sm], writes=[b_sm])
                    S.op("dve", lambda e: e.match_replace(out=work2[:], in_to_replace=c24[:, 0:8], in_values=cand[:], imm_value=-1.0), reads=[b_sm], writes=[b_sm])
                    S.op("dve", lambda e: e.max(out=c24[:, 8:16], in_=work2[:]), reads=[b_sm], writes=[b_sm])
                    S.op("dve", lambda e: e.match_replace(out=cand[:], in_to_replace=c24[:, 8:16], in_values=work2[:], imm_value=-1.0), reads=[b_sm], writes=[b_sm])
                    S.op("dve", lambda e: e.max(out=c24[:, 16:24], in_=cand[:]), reads=[b_sm], writes=[b_sm])
                    S.op("dve", lambda e, h=h: e.tensor_tensor(out=th4[:, h:h + 1], in0=c24[:, 15:16], in1=c24[:, 16:17], op=ALU.add), reads=[b_sm], writes=[b_sm])
                    S.op("dve", lambda e, h=h: e.reduce_sum(out=zs[:, h:h + 1], in_=c24[:, 0:16], axis=AX.X), reads=[b_sm], writes=[b_sm])
                S.op("dve", lambda e, tt=tt: e.reciprocal(out=RZ[:, tt, :], in_=zs[:]), reads=[b_sm], writes=[bTH[tt]])
                S.op("dve", lambda e, tt=tt: e.scalar_tensor_tensor(out=TH[:, tt, :], in0=th4[:], scalar=0.5, in1=RZ[:, tt, :], op0=ALU.mult, op1=ALU.mult),
                     reads=[b_sm, bTH[tt]], writes=[bTH[tt]])
                for h in range(4):
                    S.op("dve", lambda e, tt=tt, h=h: e.tensor_scalar(out=P1n[:, tt, h, :], in0=Pexp[:, tt, 2 * h, :], scalar1=RZ[:, tt, h:h + 1], scalar2=None, op0=ALU.mult),
                         reads=[bP[tt], bTH[tt]], writes=[bTH[tt]])
            S.barrier()
        with ExitStack() as es3:
            PH = cx.ps(es3, [128, 2, 512]); bPH = S.bufs(2, "PH")
            PW = [cx.ps(es3, [128, 512]) for _ in range(2)]; bPW = S.bufs(2, "PW")
            UG = [cx.sb(es3, [128, 8, GA * 128], BF16) for _ in range(2)]; bUG = S.bufs(2, "UG")
            NV = 3
            VG = [cx.sb(es3, [128, GA, 1024], BF16) for _ in range(NV)]; bVG = S.bufs(NV, "VG")
            NX = 6
            Xn = [cx.sb(es3, [128, GA, 128]) for _ in range(NX)]; bXn = S.bufs(NX, "Xn")
            Wh = [[cx.sb(es3, [128, GA, 128], BF16) for _ in range(16)] for _ in range(2)]; bWh = [S.bufs(16, "Wh%d_" % k) for k in range(2)]
            G = [cx.sb(es3, [128, 512], BF16) for _ in range(2 * GA)]; bG = S.bufs(2 * GA, "G")
            WgT = [cx.sb(es3, [128, GA, 512], BF16) for _ in range(2)]; bWg = S.bufs(2, "WgT")
            uv = uTb.rearrange("(kc p) n -> p kc n", p=128)
            vv = vb.rearrange("(a p) d -> p a d", p=128)
            cnt = {"x": 0, "ph": 0, "pw": 0, "fl": 0}
            XENG = PEER_XENG

            def stage1(gi):
                ug, bu = UG[gi % 2], bUG[gi % 2]
                S.dma("sp", ug[:, 0:4, :], uv[:, 0:4, gi * GA * 128:(gi + 1) * GA * 128], writes=[bu])
                S.dma("act", ug[:, 4:8, :], uv[:, 4:8, gi * GA * 128:(gi + 1) * GA * 128], writes=[bu])
                for ac in range(GA):
                    j = cnt["ph"] % 2
                    cnt["ph"] += 1
                    g, bg = G[(gi % 2) * GA + ac], bG[(gi % 2) * GA + ac]
                    for kc in range(8):
                        S.op("pe", lambda e, ac=ac, kc=kc, j=j, ug=ug: e.matmul(PH[:, j, :], lhsT=ug[:, kc, ac * 128:(ac + 1) * 128], rhs=xnT[:, kc, :],
                                                                              start=(kc == 0), stop=(kc == 7)),
                             reads=[bu] + b_xnT, writes=[bPH[j]])
                    S.op("act", lambda e, j=j, g=g: e.activation(out=g[:], in_=PH[:, j, :], func=AF.Gelu), reads=[bPH[j]], writes=[bg])
                for tt in range(4):
                    for h in range(4):
                        x, bx = Xn[cnt["x"] % NX], bXn[cnt["x"] % NX]
                        cnt["x"] += 1
                        if True:
                            S.op(XENG[h], lambda e, tt=tt, h=h, x=x, gi=gi: e.tensor_tensor(
                                out=x[:], in0=P1n[:, tt, h, gi * GA:(gi + 1) * GA].unsqueeze(2).to_broadcast([128, GA, 128]),
                                in1=Pexp[:, tt, 2 * h + 1, :].unsqueeze(1).to_broadcast([128, GA, 128]), op=ALU.mult),
                                reads=[bP[tt], bTH[tt]], writes=[bx])
                        else:
                            for a in range(GA):
                                S.op("act", lambda e, tt=tt, h=h, a=a, x=x, gi=gi: e.activation(out=x[:, a, :], in_=Pexp[:, tt, 2 * h + 1, :], func=AF.Copy,
                                                                                             scale=P1n[:, tt, h, gi * GA + a:gi * GA + a + 1]),
                                     reads=[bP[tt], bTH[tt]], writes=[bx])
                        w, bw = Wh[gi % 2][tt * 4 + h], bWh[gi % 2][tt * 4 + h]
                        S.op("dve", lambda e, tt=tt, h=h, x=x, w=w: e.scalar_tensor_tensor(out=w[:], in0=x[:], scalar=TH[:, tt, h:h + 1], in1=x[:],
                                                                                          op0=ALU.is_ge, op1=ALU.mult),
                             reads=[bx, bTH[tt]], writes=[bw])

            def stage2(gi):
                vg, bv = VG[gi % NV], bVG[gi % NV]
                S.dma("sp", vg[:, 0:2, :], vv[:, gi * GA:gi * GA + 2, :], writes=[bv])
                S.dma("act", vg[:, 2:4, :], vv[:, gi * GA + 2:(gi + 1) * GA, :], writes=[bv])
                wg, bwg = WgT[gi % 2], bWg[gi % 2]
                for ac in range(GA):
                    j = cnt["pw"] % 2
                    cnt["pw"] += 1
                    g, bg = G[(gi % 2) * GA + ac], bG[(gi % 2) * GA + ac]
                    for tt in range(4):
                        for h in range(4):
                            w, bw = Wh[gi % 2][tt * 4 + h], bWh[gi % 2][tt * 4 + h]
                            S.op("pe", lambda e, ac=ac, tt=tt, j=j, w=w, h=h: e.matmul(PW[j][:, tt * 128:(tt + 1) * 128], lhsT=w[:, ac, :], rhs=consts["identb"][:],
                                                                                     start=(h == 0), stop=(h == 3)),
                                 reads=[bw, consts["b_ident"]], writes=[bPW[j]])
                    S.op("dve", lambda e, ac=ac, j=j, wg=wg, g=g: e.tensor_tensor(out=wg[:, ac, :], in0=g[:], in1=PW[j][:], op=ALU.mult),
                         reads=[bg, bPW[j]], writes=[bwg])

            def stage3(gi):
                vg, bv = VG[gi % NV], bVG[gi % NV]
                wg, bwg = WgT[gi % 2], bWg[gi % 2]
                for tt in range(4):
                    pb = 2 * (tt % 2)
                    for ac in range(GA):
                        for half in range(2):
                            S.op("pe", lambda e, ac=ac, tt=tt, half=half, pb=pb, wg=wg, vg=vg: e.matmul(
                                PSA[:, pb + half, :], lhsT=wg[:, ac, tt * 128:(tt + 1) * 128], rhs=vg[:, ac, half * 512:(half + 1) * 512],
                                start=(ac == 0), stop=(ac == GA - 1)),
                                reads=[bwg, bv], writes=[bPSA[pb + half]])
                    src = PSA[:, pb:pb + 2, :].rearrange("p a b -> p (a b)")
                    if gi == 0:
                        S.op("act", lambda e, tt=tt, src=src: e.copy(out=QA[:, tt, :], in_=src), reads=[bPSA[pb], bPSA[pb + 1]], writes=[bQA[tt]])
                    else:
                        S.op("dve", lambda e, tt=tt, src=src: e.tensor_tensor(out=QA[:, tt, :], in0=QA[:, tt, :], in1=src, op=ALU.add),
                             reads=[bPSA[pb], bPSA[pb + 1], bQA[tt]], writes=[bQA[tt]])

            for k in range(NG + 2):
                if k < NG:
                    stage1(k)
                if 0 <= k - 1 < NG:
                    stage2(k - 1)
                if 0 <= k - 2 < NG:
                    stage3(k - 2)
            for tt in range(4):
                S.op("pool", lambda e, tt=tt: e.tensor_tensor(out=QA[:, tt, :], in0=QA[:, tt, :], in1=G2[:], op=ALU.mult), reads=[bQA[tt], b_g2], writes=[bQA[tt]])
                S.op("dve", lambda e, tt=tt: e.tensor_tensor(out=HM[:, tt, :], in0=HM[:, tt, :], in1=QA[:, tt, :], op=ALU.add), reads=[bQA[tt], bHM[tt]], writes=[bHM[tt]])
            S.barrier()


def hgrn_consts_host():
    t = np.arange(128)
    ch = t // 16
    same = ch[:, None] == ch[None, :]
    BT = (same & (t[:, None] <= t[None, :])).astype(np.float32)
    RT = (same & (t[:, None] > t[None, :])).astype(np.float32)
    CI = (ch[:, None] == np.arange(8)[None, :]).astype(np.float32)
    RTF = (t[:, None] > t[None, :]).astype(np.float32)
    hcA = np.concatenate([BT, RT, CI, RTF, np.ones((128, 1), np.float32)], axis=1)
    hcB = np.ascontiguousarray(CI.T).reshape(1, 1024)
    return hcA, hcB


def emit_hgrn(cx, consts, HM, bHM, mode, x_dram, ntiles, snap, mod_dram, c_sh, c_ge, c_g,
              win_dram, wout_dram, ongT_dram, lbl_dram, hcA_dram, hcB_dram, onehot_dram=None, m=0):
    S = cx.S
    full = (mode == "full")
    with ExitStack() as es:
        hcA = cx.sb(es, [128, 393]); CHM = cx.sb(es, [128, 8, 128]); b_hc = S.buf("hc")
        S.dma("sp", hcA[:], hcA_dram, writes=[b_hc])
        S.dma("pool", CHM[:].rearrange("p a b -> p (a b)"), hcB_dram[0:1, :].partition_broadcast(128), writes=[b_hc])
        BT, RT, CI = hcA[:, 0:128], hcA[:, 128:256], hcA[:, 256:264]
        RTF, ONE = hcA[:, 264:392], hcA[:, 392:393]
        LB = cx.sb(es, [128, 1024]); OML = cx.sb(es, [128, 1024]); GE = cx.sb(es, [128, 1024]); SH = cx.sb(es, [128, 1024])
        b_mod = S.buf("hmod")
        S.dma("sp", SH[:], mod_dram[:, c_sh:c_sh + 1024], writes=[b_mod])
        S.dma("sp", GE[:], mod_dram[:, c_ge:c_ge + 1024], writes=[b_mod])
        S.dma("pool", LB[:], lbl_dram[0:1, :].partition_broadcast(128), writes=[b_mod])
        S.dma("pool", OML[:], lbl_dram[1:2, :].partition_broadcast(128), writes=[b_mod])
        S.op("dve", lambda e: e.tensor_tensor(out=LB[:], in0=LB[:], in1=OML[:], op=ALU.subtract), reads=[b_mod], writes=[b_mod])
        S.op("act", lambda e: e.activation(out=LB[:], in_=LB[:], func=AF.Sigmoid), reads=[b_mod], writes=[b_mod])
        S.op("dve", lambda e: e.tensor_scalar(out=OML[:], in0=LB[:], scalar1=-1.0, scalar2=1.0, op0=ALU.mult, op1=ALU.add), reads=[b_mod], writes=[b_mod])
        win = cx.sb(es, [128, 8, 4096], BF16); b_win = S.buf("win")
        wout = cx.sb(es, [128, 8, 1024], BF16); b_wout = S.buf("wout")
        load_w(cx, win, win_dram, b_win, 4096)
        if full:
            load_w(cx, wout, wout_dram, b_wout, 1024)
        nt = NormT(cx, es, consts)
        xt = [cx.sb(es, [128, 1024]) for _ in range(2)]; b_xt = S.bufs(2, "xt")
        hnT = cx.sb(es, [128, 8, 128], BF16); b_hnT = S.buf("hnT")
        R = [cx.sb(es, [128, 1024]) for _ in range(6)]; bR = S.bufs(6, "R")
        OGb = cx.sb(es, [128, 1024], BF16); b_og = S.buf("og")
        VVb = cx.sb(es, [128, 1024], BF16); bVVb = S.buf("VVb")
        KLb = cx.sb(es, [128, 1024], BF16); bKLb = S.buf("KLb")
        QDb = cx.sb(es, [128, 1024], BF16); bQDb = S.buf("QDb")
        KDb = cx.sb(es, [128, 1024], BF16); bKDb = S.buf("KDb")
        STb = cx.sb(es, [128, 8, 2, 128], BF16); bSTb = [S.bufs(2, "STb%d_" % h) for h in range(8)]
        ogT = cx.sb(es, [128, 8, 128], BF16); b_ogT = S.buf("ogT")
        QKT = [cx.sb(es, [128, 2, 128], BF16) for _ in range(2)]; bQKT = S.bufs(2, "QKT")
        SCM = [cx.sb(es, [128, 128], BF16) for _ in range(2)]; bSCM = S.bufs(2, "SCM")
        QDM = [cx.sb(es, [128, 8, 128], BF16) for _ in range(2)]; bQDM = S.bufs(2, "QDM")
        KLM = [cx.sb(es, [128, 8, 128], BF16) for _ in range(2)]; bKLM = S.bufs(2, "KLM")
        ST = cx.sb(es, [128, 8, 2, 128]); bST = [S.bufs(2, "ST%d_" % h) for h in range(8)]
        EDEC = cx.sb(es, [128, 64]); bEDEC = S.buf("EDEC")
        ss8 = cx.sb(es, [128, 16]); b_ss8 = S.buf("ss8")
        PJ = [cx.ps(es, [128, 512]) for _ in range(2)]; bPJ = S.bufs(2, "PJ")
        PKV = [cx.ps(es, [128, 512]) for _ in range(2)]; bPKV = S.bufs(2, "PKV")
        PO = [cx.ps(es, [128, 512]) for _ in range(2)]; bPO = S.bufs(2, "PO")
        PC = cx.ps(es, [128, 512]); bPC = S.buf("PC")
        pj_i = [0]

        def nextpj():
            k = pj_i[0] % 2
            pj_i[0] += 1
            return PJ[k], bPJ[k]

        def proj(j, nb):
            pj, b = nextpj()
            for kc in range(8):
                S.op("pe", lambda e, pj=pj, kc=kc: e.matmul(pj[:], lhsT=hnT[:, kc, :],
                                                            rhs=win[:, kc, j * 1024 + nb * 512:j * 1024 + (nb + 1) * 512],
                                                            start=(kc == 0), stop=(kc == 7)),
                     reads=[b_hnT, b_win], writes=[b])
            return pj, b

        if full:
            oh = cx.sb(es, [128, 4]); b_oh = S.buf("oh")
            S.dma("pool", oh[:], onehot_dram[0:1, :].partition_broadcast(128), writes=[b_oh])
            stv = ST[:, :, 0, :]
            allst = [bST[h][0] for h in range(8)]
            for j in range(4):
                S.dma("sp", R[5][:], snap[4 * m + j], writes=[bR[5]])
                r5 = R[5][:].rearrange("p (h v) -> p h v", h=8)
                if j == 0:
                    S.op("dve", lambda e, j=j, r5=r5: e.tensor_scalar(out=stv, in0=r5, scalar1=oh[:, j:j + 1], scalar2=None, op0=ALU.mult),
                         reads=[bR[5], b_oh], writes=allst)
                else:
                    S.op("dve", lambda e, j=j, r5=r5: e.scalar_tensor_tensor(out=stv, in0=r5, scalar=oh[:, j:j + 1], in1=stv, op0=ALU.mult, op1=ALU.add),
                         reads=[bR[5], b_oh] + allst, writes=allst)
            S.op("act", lambda e: e.copy(out=STb[:, :, 0, :], in_=ST[:, :, 0, :]), reads=allst, writes=[bSTb[h][0] for h in range(8)])
        else:
            S.op("pool", lambda e: e.memset(ST[:].rearrange("p a b c -> p (a b c)"), 0.0), writes=[bST[h][s] for h in range(8) for s in range(2)])

        for ti in range(ntiles):
            x_t, bx = xt[ti % 2], b_xt[ti % 2]
            if (not full) and ti % 4 == 0:
                S.dma("pool", snap[ti // 4].rearrange("p (h v) -> p h v", h=8), ST[:, :, 0, :], reads=[bST[h][0] for h in range(8)])
            row0 = (m * 4 + ti) * 128 if full else ti * 128
            S.dma("sp", x_t[:], x_dram[row0:row0 + 128, :], writes=[bx])
            nt.normT(x_t[:], bx, GE[:], SH[:], b_mod, hnT[:], b_hnT)
            H2 = [slice(0, 512), slice(512, 1024)]
            for nb in range(2):
                pf, bpf = proj(1, nb)
                S.op("act", lambda e, pf=pf, nb=nb: e.activation(out=R[0][:, H2[nb]], in_=pf[:], func=AF.Sigmoid), reads=[bpf], writes=[bR[0]])
            S.op("dve", lambda e: e.tensor_tensor(out=R[0][:], in0=R[0][:], in1=OML[:], op=ALU.mult), reads=[bR[0], b_mod], writes=[bR[0]])
            S.op("pool", lambda e: e.tensor_tensor(out=R[0][:], in0=R[0][:], in1=LB[:], op=ALU.add), reads=[bR[0], b_mod], writes=[bR[0]])
            S.op("act", lambda e: e.activation(out=R[1][:], in_=R[0][:], func=AF.Ln), reads=[bR[0]], writes=[bR[1]])
            S.op("pool", lambda e: e.tensor_scalar(out=R[2][:], in0=R[0][:], scalar1=-1.0, scalar2=1.0, op0=ALU.mult, op1=ALU.add), reads=[bR[0]], writes=[bR[2]])
            for nb in range(2 if full else 0):
                pc, bpc = nextpj()
                S.op("pe", lambda e, pc=pc, nb=nb: e.matmul(pc[:], lhsT=BT, rhs=R[1][:, H2[nb]], start=True, stop=True),
                     reads=[b_hc, bR[1]], writes=[bpc])
                if full:
                    S.op("act", lambda e, pc=pc, nb=nb: e.activation(out=R[0][:, H2[nb]], in_=pc[:], func=AF.Exp), reads=[bpc], writes=[bR[0]])
                    S.op("act", lambda e, pc=pc, nb=nb: e.activation(out=R[3][:, H2[nb]], in_=pc[:], func=AF.Exp, scale=-1.0), reads=[bpc], writes=[bR[3]])
            for nb in range(2):
                pr, bpr = nextpj()
                S.op("pe", lambda e, pr=pr, nb=nb: e.matmul(pr[:], lhsT=(RT if full else RTF), rhs=R[1][:, H2[nb]], start=True, stop=True),
                     reads=[b_hc, bR[1]], writes=[bpr])
                S.op("act", lambda e, pr=pr, nb=nb: e.activation(out=R[4][:, H2[nb]], in_=pr[:], func=AF.Exp), reads=[bpr], writes=[bR[4]])
            for h in range(8):
                S.op("pe", lambda e, h=h: e.matmul(PC[:, 384 + h * 8:384 + (h + 1) * 8], lhsT=R[1][:, h * 128:(h + 1) * 128], rhs=CI, start=True, stop=True),
                     reads=[b_hc, bR[1]], writes=[bPC]) if full else \
                    S.op("pe", lambda e, h=h: e.matmul(PC[:, 384 + h:385 + h], lhsT=R[1][:, h * 128:(h + 1) * 128], rhs=ONE, start=True, stop=True),
                         reads=[b_hc, bR[1]], writes=[bPC])
            if full:
                S.op("act", lambda e: e.activation(out=EDEC[:], in_=PC[:, 384:448], func=AF.Exp), reads=[bPC], writes=[bEDEC])
            else:
                S.op("act", lambda e: e.activation(out=EDEC[:, 0:8], in_=PC[:, 384:392], func=AF.Exp), reads=[bPC], writes=[bEDEC])
            S.op("dve", lambda e: e.tensor_tensor(out=KLb[:], in0=R[4][:], in1=R[2][:], op=ALU.mult), reads=[bR[4], bR[2]], writes=[bKLb])
            if full:
                S.op("dve", lambda e: e.tensor_tensor(out=KDb[:], in0=R[3][:], in1=R[2][:], op=ALU.mult), reads=[bR[3], bR[2]], writes=[bKDb])
            for nb in range(2):
                pi, bpi = proj(2, nb)
                S.op("act", lambda e, pi=pi, nb=nb: e.copy(out=VVb[:, H2[nb]], in_=pi[:]), reads=[bpi], writes=[bVVb])
            if full:
                for nb in range(2):
                    pq, bpq = proj(0, nb)
                    S.op("act", lambda e, pq=pq, nb=nb: e.activation(out=R[2][:, H2[nb]], in_=pq[:], func=AF.Silu), reads=[bpq], writes=[bR[2]])
                S.op("dve", lambda e: e.tensor_tensor(out=QDb[:], in0=R[0][:], in1=R[2][:], op=ALU.mult), reads=[bR[0], bR[2]], writes=[bQDb])
                for nb in range(2):
                    pg, bpg = proj(3, nb)
                    S.op("act", lambda e, pg=pg, nb=nb: e.activation(out=R[2][:, H2[nb]], in_=pg[:], func=AF.Silu), reads=[bpg], writes=[bR[2]])
            if not full:
                for h in range(8):
                    hs = slice(h * 128, (h + 1) * 128)
                    k = h % 2
                    S.op("pe", lambda e, k=k, hs=hs: e.matmul(PKV[k][:, 0:128], lhsT=KLb[:, hs], rhs=VVb[:, hs], start=True, stop=True),
                         reads=[bKLb, bVVb], writes=[bPKV[k]])
                    S.op("dve", lambda e, k=k, h=h: e.scalar_tensor_tensor(
                        out=ST[:, h, 0, :], in0=ST[:, h, 0, :], scalar=EDEC[:, h:h + 1], in1=PKV[k][:, 0:128], op0=ALU.mult, op1=ALU.add),
                        reads=[bST[h][0], bEDEC, bPKV[k]], writes=[bST[h][0]])
            else:
                for hp in range(4):
                    for j in range(2):
                        h = 2 * hp + j
                        hs = slice(h * 128, (h + 1) * 128)
                        S.op("pe", lambda e, hs=hs: e.transpose(nt.pt[:, 0, :], QDb[:, hs], consts["identb"][:]), reads=[bQDb, consts["b_ident"]], writes=[nt.b_pt])
                        S.op("pe", lambda e, hs=hs: e.transpose(nt.pt[:, 1, :], KDb[:, hs], consts["identb"][:]), reads=[bKDb, consts["b_ident"]], writes=[nt.b_pt])
                        S.op("act", lambda e, j=j: e.copy(out=QKT[j][:], in_=nt.pt[:, 0:2, :]), reads=[nt.b_pt], writes=[bQKT[j]])
                        S.op("pe", lambda e, j=j: e.matmul(PC[:, 256:384], lhsT=QKT[j][:, 1, :], rhs=QKT[j][:, 0, :], start=True, stop=True), reads=[bQKT[j]], writes=[bPC])
                        S.op("dve", lambda e, j=j: e.tensor_tensor(out=SCM[j][:], in0=PC[:, 256:384], in1=BT, op=ALU.mult), reads=[bPC, b_hc], writes=[bSCM[j]])
                        S.op("pool", lambda e, j=j: e.tensor_tensor(out=QDM[j][:], in0=QKT[j][:, 0, :].unsqueeze(1).to_broadcast([128, 8, 128]), in1=CHM[:], op=ALU.mult),
                             reads=[bQKT[j], b_hc], writes=[bQDM[j]])
                        S.op("pool", lambda e, j=j, hs=hs: e.tensor_tensor(out=KLM[j][:], in0=KLb[:, hs].unsqueeze(1).to_broadcast([128, 8, 128]),
                                                                           in1=CI.unsqueeze(2).to_broadcast([128, 8, 128]), op=ALU.mult),
                             reads=[bKLb, b_hc], writes=[bKLM[j]])
                    for j in range(2):
                        h = 2 * hp + j
                        hs = slice(h * 128, (h + 1) * 128)
                        S.op("pe", lambda e, j=j, hs=hs: e.matmul(PO[j][:, 0:128], lhsT=SCM[j][:], rhs=VVb[:, hs], start=True, stop=False),
                             reads=[bSCM[j], bVVb], writes=[bPO[j]])
                    for c in range(8):
                        s_old, s_new = c % 2, (c + 1) % 2
                        for j in range(2):
                            h = 2 * hp + j
                            hs = slice(h * 128, (h + 1) * 128)
                            S.op("pe", lambda e, c=c, j=j, hs=hs: e.matmul(PKV[j][:, 0:128], lhsT=KLM[j][:, c, :], rhs=VVb[:, hs], start=True, stop=True),
                                 reads=[bKLM[j], bVVb], writes=[bPKV[j]])
                            S.op("pe", lambda e, c=c, h=h, j=j, s_old=s_old: e.matmul(PO[j][:, 0:128], lhsT=QDM[j][:, c, :], rhs=STb[:, h, s_old, :],
                                                                                     start=False, stop=(c == 7)),
                                 reads=[bQDM[j], bSTb[h][s_old]], writes=[bPO[j]])
                            S.op("dve", lambda e, c=c, j=j, h=h, s_old=s_old, s_new=s_new: e.scalar_tensor_tensor(
                                out=ST[:, h, s_new, :], in0=ST[:, h, s_old, :], scalar=EDEC[:, h * 8 + c:h * 8 + c + 1], in1=PKV[j][:, 0:128], op0=ALU.mult, op1=ALU.add),
                                reads=[bST[h][s_old], bEDEC, bPKV[j]], writes=[bST[h][s_new]])
                            S.op("act", lambda e, h=h, s_new=s_new: e.copy(out=STb[:, h, s_new, :], in_=ST[:, h, s_new, :]), reads=[bST[h][s_new]], writes=[bSTb[h][s_new]])
                    for j in range(2):
                        h = 2 * hp + j
                        S.op("act", lambda e, h=h, j=j: e.copy(out=R[5][:, h * 128:(h + 1) * 128], in_=PO[j][:, 0:128]), reads=[bPO[j]], writes=[bR[5]])
            if full:
                S.op("dve", lambda e: e.tensor_tensor(out=R[3][:], in0=R[5][:], in1=R[5][:], op=ALU.mult), reads=[bR[5]], writes=[bR[3]])
                S.op("dve", lambda e: e.reduce_sum(out=ss8[:, 0:8], in_=R[3][:].rearrange("p (h v) -> p h v", h=8), axis=AX.X), reads=[bR[3]], writes=[b_ss8])
                emit_rstd(cx, ss8[:, 0:8], ss8[:, 8:16], 1.0 / 128, b_ss8, b_ss8)
                S.op("dve", lambda e: e.tensor_tensor(out=R[5][:].rearrange("p (h v) -> p h v", h=8), in0=R[5][:].rearrange("p (h v) -> p h v", h=8),
                                                      in1=ss8[:, 8:16].unsqueeze(2).to_broadcast([128, 8, 128]), op=ALU.mult),
                     reads=[bR[5], b_ss8], writes=[bR[5]])
                S.op("pool", lambda e: e.tensor_tensor(out=OGb[:], in0=R[5][:], in1=R[2][:], op=ALU.mult), reads=[bR[5], bR[2]], writes=[b_og])
                nt.transpose(OGb, b_og, ogT[:], b_ogT)
                for nb in range(2):
                    pm, bpm = nextpj()
                    for kc in range(8):
                        S.op("pe", lambda e, pm=pm, nb=nb, kc=kc: e.matmul(pm[:], lhsT=ogT[:, kc, :], rhs=wout[:, kc, nb * 512:(nb + 1) * 512],
                                                                          start=(kc == 0), stop=(kc == 7)),
                             reads=[b_ogT, b_wout], writes=[bpm])
                    S.op("dve", lambda e, pm=pm, x_t=x_t, ti=ti, nb=nb: e.tensor_tensor(out=HM[:, ti, H2[nb]], in0=pm[:], in1=x_t[:, H2[nb]], op=ALU.add),
                         reads=[bpm, bx], writes=[bHM[ti]])
        if (not full) and ntiles % 4 == 0:
            S.dma("pool", snap[ntiles // 4].rearrange("p (h v) -> p h v", h=8), ST[:, :, 0, :], reads=[bST[h][0] for h in range(8)])
        S.barrier()


def attn_consts_host(r):
    j = np.arange(128)
    trin = -(j[:, None] >= j[None, :]).astype(np.float32)
    onesn = -np.ones((128, 128), np.float32)
    ac = np.concatenate([trin, onesn], axis=1).astype(ml_dtypes.bfloat16)
    k = np.arange(16)
    kp = k[None, :, None] * 128 + j[:, None, None]
    qp = 4 * r * 128 + np.arange(512)[None, None, :]
    mask = (kp < qp).astype(np.float32).astype(ml_dtypes.bfloat16)
    return ac, mask


def emit_attn(cx, consts, HM, bHM, m, mod_dram, c_sh, c_ge, c_g, wq_dram, wo_dram, KT_dram, V_dram, ac_dram, mask_dram):
    S = cx.S
    NB = 16 * (m + 1)
    with ExitStack() as es:
        SH = cx.sb(es, [128, 1024]); GE = cx.sb(es, [128, 1024]); b_mod = S.buf("amod")
        S.dma("sp", SH[:], mod_dram[:, c_sh:c_sh + 1024], writes=[b_mod])
        S.dma("sp", GE[:], mod_dram[:, c_ge:c_ge + 1024], writes=[b_mod])
        AC = cx.sb(es, [128, 256], BF16); MASK = cx.sb(es, [128, 16, 512], BF16); b_ac = S.buf("ac")
        S.dma("sp", AC[:], ac_dram, writes=[b_ac])
        S.dma("sp", MASK[:], mask_dram, writes=[b_ac])
        TRIN, ONESN = AC[:, 0:128], AC[:, 128:256]
        wq = cx.sb(es, [128, 8, 1024], BF16); b_wq = S.buf("awq")
        wo = cx.sb(es, [128, 8, 1024], BF16); b_wo = S.buf("awo")
        load_w(cx, wq, wq_dram, b_wq, 1024)
        load_w(cx, wo, wo_dram, b_wo, 1024)
        nt = NormT(cx, es, consts)
        xnT = cx.sb(es, [128, 8, 512], BF16); b_xnT = S.bufs(4, "axnT")
        QT = cx.sb(es, [128, 8, 512], BF16); bQT = S.bufs(8, "QT")
        NKV = 3
        KTc = [cx.sb(es, [128, 2048], BF16) for _ in range(NKV)]; bKT = S.bufs(NKV, "KTc")
        Vc = [cx.sb(es, [128, 16, 128], BF16) for _ in range(NKV)]; bV = S.bufs(NKV, "Vc")
        NR = 3
        E = [cx.sb(es, [128, 512]) for _ in range(NR)]; bE = S.bufs(NR, "E")
        LK = [cx.sb(es, [128, 512], BF16) for _ in range(NR)]; bLK = S.bufs(NR, "LK")
        LKS = [cx.sb(es, [128, 512], BF16) for _ in range(NR)]; bLKS = S.bufs(NR, "LKS")
        A = [cx.sb(es, [128, 512], BF16) for _ in range(NR)]; bA = S.bufs(NR, "A")
        OT = cx.sb(es, [128, 8, 512], BF16); bOT = S.bufs(8, "OT")
        PZ = [cx.ps(es, [128, 512]) for _ in range(3)]; bPZ = S.bufs(3, "PZ")
        PS = [cx.ps(es, [128, 512]) for _ in range(2)]; bPS = S.bufs(2, "PS")
        POUT = [cx.ps(es, [128, 512]) for _ in range(2)]; bPOUT = S.bufs(2, "POUT")
        for tt in range(4):
            nt.normT(HM[:, tt, :], bHM[tt], GE[:], SH[:], b_mod, xnT[:, :, tt * 128:(tt + 1) * 128], b_xnT[tt])
        sc = 1.0 / math.sqrt(128.0)
        for h in range(8):
            pz, bpz = (PZ[h % 3], bPZ[h % 3])
            for kc in range(8):
                S.op("pe", lambda e, h=h, kc=kc, pz=pz: e.matmul(pz[:], lhsT=wq[:, kc, h * 128:(h + 1) * 128], rhs=xnT[:, kc, :], start=(kc == 0), stop=(kc == 7)),
                     reads=[b_wq] + b_xnT, writes=[bpz])
            S.op("act", lambda e, h=h, pz=pz: e.activation(out=QT[:, h, :], in_=pz[:], func=AF.Copy, scale=sc), reads=[bpz], writes=[bQT[h]])
        items = []
        ld = 0
        for h in range(8):
            for ci in range(m, -1, -1):
                slot = ld % NKV
                ld += 1
                for kk in range(15, -1, -1):
                    items.append(dict(h=h, ci=ci, kk=kk, slot=slot, load=(kk == 15), first=(ci == m and kk == 15), last=(ci == 0 and kk == 0),
                                      masked=(ci == m)))
        for i, it in enumerate(items):
            it["i"] = i

        def stageA(it):
            i, h, kk, slot = it["i"], it["h"], it["kk"], it["slot"]
            kt, bkt, vc, bvc = KTc[slot], bKT[slot], Vc[slot], bV[slot]
            if it["load"]:
                S.dma("sp", kt[:], KT_dram[h, :, it["ci"] * 2048:(it["ci"] + 1) * 2048], writes=[bkt])
                S.dma("pool", vc[:], V_dram[h, :, it["ci"] * 16:(it["ci"] + 1) * 16, :], writes=[bvc])
            z, r = i % 3, i % NR
            ks = slice(kk * 128, (kk + 1) * 128)
            S.op("pe", lambda e: e.matmul(PZ[z][:], lhsT=kt[:, ks], rhs=QT[:, h, :], start=True, stop=True), reads=[bkt, bQT[h]], writes=[bPZ[z]])
            S.op("act", lambda e: e.activation(out=E[r][:], in_=PZ[z][:], func=AF.Exp), reads=[bPZ[z]], writes=[bE[r]])
            S.op("act", lambda e: e.activation(out=LK[r][:], in_=E[r][:], func=AF.Ln, bias=1.0, scale=1.0), reads=[bE[r]], writes=[bLK[r]])
            if it["masked"]:
                S.op("dve", lambda e: e.tensor_tensor(out=LK[r][:], in0=LK[r][:], in1=MASK[:, kk, :], op=ALU.mult), reads=[bLK[r], b_ac], writes=[bLK[r]])

        def stageB(it):
            i, h, kk, slot = it["i"], it["h"], it["kk"], it["slot"]
            kt, bkt = KTc[slot], bKT[slot]
            r, p = i % NR, i % 2
            nx = (i + 1) % NR
            first, last = it["first"], it["last"]
            ks = slice(kk * 128, (kk + 1) * 128)
            S.op("pe", lambda e: e.matmul(PS[p][:], lhsT=kt[:, ks], rhs=QT[:, h, :], start=True, stop=False), reads=[bkt, bQT[h]], writes=[bPS[p]])
            S.op("pe", lambda e: e.matmul(PS[p][:], lhsT=TRIN, rhs=LK[r][:], start=False, stop=first), reads=[b_ac, bLK[r]], writes=[bPS[p]])
            if not first:
                S.op("pe", lambda e: e.matmul(PS[p][:], lhsT=ONESN, rhs=LKS[r][:], start=False, stop=True), reads=[b_ac, bLKS[r]], writes=[bPS[p]])
            S.op("act", lambda e: e.activation(out=A[r][:], in_=PS[p][:], func=AF.Exp), reads=[bPS[p]], writes=[bA[r]])
            if it["masked"]:
                S.op("pool", lambda e: e.tensor_tensor(out=A[r][:], in0=A[r][:], in1=MASK[:, kk, :], op=ALU.mult), reads=[bA[r], b_ac], writes=[bA[r]])
            if first:
                S.op("dve", lambda e: e.tensor_copy(out=LKS[nx][:], in_=LK[r][:]), reads=[bLK[r]], writes=[bLKS[nx]])
            elif not last:
                S.op("dve", lambda e: e.tensor_tensor(out=LKS[nx][:], in0=LKS[r][:], in1=LK[r][:], op=ALU.add), reads=[bLK[r], bLKS[r]], writes=[bLKS[nx]])

        def stageC(it):
            i, h, kk, slot = it["i"], it["h"], it["kk"], it["slot"]
            vc, bvc = Vc[slot], bV[slot]
            r = i % NR
            po, bpo = POUT[h % 2], bPOUT[h % 2]
            S.op("pe", lambda e: e.matmul(po[:], lhsT=vc[:, kk, :], rhs=A[r][:], start=it["first"], stop=it["last"]), reads=[bvc, bA[r]], writes=[bpo])
            if it["last"]:
                S.op("dve", lambda e: e.tensor_copy(out=OT[:, h, :], in_=po[:]), reads=[bpo], writes=[bOT[h]])

        n = len(items)
        for k in range(n + 2):
            if k < n:
                stageA(items[k])
            if 0 <= k - 1 < n:
                stageB(items[k - 1])
            if 0 <= k - 2 < n:
                stageC(items[k - 2])
        for tt in range(4):
            for nb in range(2):
                S_ps, b_ps = PZ[nb], bPZ[nb]
                for h in range(8):
                    S.op("pe", lambda e, tt=tt, nb=nb, h=h, S_ps=S_ps: e.matmul(S_ps[:], lhsT=OT[:, h, tt * 128:(tt + 1) * 128], rhs=wo[:, h, nb * 512:(nb + 1) * 512],
                                                                             start=(h == 0), stop=(h == 7)),
                         reads=[bOT[h], b_wo], writes=[b_ps])
                S.op("dve", lambda e, tt=tt, nb=nb, S_ps=S_ps: e.tensor_tensor(out=HM[:, tt, nb * 512:(nb + 1) * 512], in0=HM[:, tt, nb * 512:(nb + 1) * 512], in1=S_ps[:], op=ALU.add),
                     reads=[b_ps, bHM[tt]], writes=[bHM[tt]])
        S.barrier()


def emit_kv(cx, consts, HM, bHM, m, mod_dram, c_sh, c_ge, kvw_dram, KTo, Vo):
    S = cx.S
    with ExitStack() as es:
        SH = cx.sb(es, [128, 1024]); GE = cx.sb(es, [128, 1024]); b_mod = S.buf("kmod")
        S.dma("sp", SH[:], mod_dram[:, c_sh:c_sh + 1024], writes=[b_mod])
        S.dma("sp", GE[:], mod_dram[:, c_ge:c_ge + 1024], writes=[b_mod])
        kvw = cx.sb(es, [128, 8, 2048], BF16); b_kvw = S.buf("kvw")
        load_w(cx, kvw, kvw_dram, b_kvw, 2048)
        nt = NormT(cx, es, consts)
        xnT = cx.sb(es, [128, 8, 512], BF16); b_xnT = S.bufs(4, "kxnT")
        KTs = cx.sb(es, [128, 8, 512], BF16); bKTs = S.buf("KTs")
        Vs = [cx.sb(es, [128, 1024], BF16) for _ in range(2)]; bVs = S.bufs(2, "Vs")
        PK = [cx.ps(es, [128, 512]) for _ in range(4)]; bPK = S.bufs(4, "PK")
        for tt in range(4):
            nt.normT(HM[:, tt, :], bHM[tt], GE[:], SH[:], b_mod, xnT[:, :, tt * 128:(tt + 1) * 128], b_xnT[tt])
        for h in range(8):
            pk, bpk = PK[h % 4], bPK[h % 4]
            for kc in range(8):
                S.op("pe", lambda e, h=h, kc=kc, pk=pk: e.matmul(pk[:], lhsT=kvw[:, kc, h * 128:(h + 1) * 128], rhs=xnT[:, kc, :], start=(kc == 0), stop=(kc == 7)),
                     reads=[b_kvw] + b_xnT, writes=[bpk])
            S.op("act", lambda e, h=h, pk=pk: e.copy(out=KTs[:, h, :], in_=pk[:]), reads=[bpk], writes=[bKTs])
        S.dma("sp", KTo[m].rearrange("h d t -> d h t"), KTs[:], reads=[bKTs])
        for tt in range(4):
            vs, bvs = Vs[tt % 2], bVs[tt % 2]
            for nb in range(2):
                pk, bpk = PK[(tt * 2 + nb) % 4], bPK[(tt * 2 + nb) % 4]
                for kc in range(8):
                    S.op("pe", lambda e, tt=tt, nb=nb, kc=kc, pk=pk: e.matmul(pk[:], lhsT=xnT[:, kc, tt * 128:(tt + 1) * 128],
                                                                             rhs=kvw[:, kc, 1024 + nb * 512:1024 + (nb + 1) * 512], start=(kc == 0), stop=(kc == 7)),
                         reads=[b_kvw, b_xnT[tt]], writes=[bpk])
                S.op("dve", lambda e, nb=nb, pk=pk, vs=vs: e.tensor_copy(out=vs[:, nb * 512:(nb + 1) * 512], in_=pk[:]), reads=[bpk], writes=[bvs])
            S.dma("sp", Vo[m * 512 + tt * 128:m * 512 + (tt + 1) * 128, :], vs[:], reads=[bvs])
        S.barrier()


def emit_store(cx, HM, bHM, m, out):
    S = cx.S
    for tt in range(4):
        S.dma("sp", out[m * 512 + tt * 128:m * 512 + (tt + 1) * 128, :], HM[:, tt, :], reads=[bHM[tt]])


def emit_final(cx, consts, HM, bHM, m, g_dram, out):
    S = cx.S
    with ExitStack() as es:
        G = cx.sb(es, [128, 1024]); bg = S.buf("fg")
        S.dma("pool", G[:], g_dram[0:1, :].partition_broadcast(128), writes=[bg])
        junk = cx.sb(es, [128, 1024], BF16); st = cx.sb(es, [128, 8]); bj, bst = S.buf(), S.buf()
        o = [cx.sb(es, [128, 1024]) for _ in range(2)]; bo = S.bufs(2, "fo")
        for tt in range(4):
            S.op("act", lambda e, tt=tt: e.activation(out=junk[:], in_=HM[:, tt, :], func=AF.Square, accum_out=st[:, 2 * tt:2 * tt + 1]),
                 reads=[bHM[tt]], writes=[bj, bst])
            emit_rstd(cx, st[:, 2 * tt:2 * tt + 1], st[:, 2 * tt + 1:2 * tt + 2], 1.0 / D, bst, bst)
            S.op("dve", lambda e, tt=tt: e.scalar_tensor_tensor(out=o[tt % 2][:], in0=HM[:, tt, :], scalar=st[:, 2 * tt + 1:2 * tt + 2], in1=G[:], op0=ALU.mult, op1=ALU.mult),
                 reads=[bHM[tt], bst, bg], writes=[bo[tt % 2]])
            S.dma("sp", out[m * 512 + tt * 128:m * 512 + (tt + 1) * 128, :], o[tt % 2][:], reads=[bo[tt % 2]])
        S.barrier()


def build_l1(NM, do_peer=True):
    SQ = 2048 * NM
    NQG = 4 * NM
    nc = bass.Bass("TRN2", target_bir_lowering=False)
    with ExitStack() as es:
        cx = Ctx(nc, es)
        S = cx.S
        I = lambda n, s, dt=F32: nc.dram_tensor(n, list(s), dt, kind="ExternalInput").ap()
        O = lambda n, s, dt=F32: nc.dram_tensor(n, list(s), dt, kind="ExternalOutput").ap()
        xb = I("xb", [SQ, D]); xo = I("xo", [NM * 512, D]); cT = I("cT", [128, 8])
        ada_w = I("ada_w", [D, 6 * D]); ada_b = I("ada_b", [1, 6 * D]); kva_w = I("kva_w", [D, 2 * D]); kva_b = I("kva_b", [1, 2 * D])
        gmix = I("gmix", [1, D]); gffn = I("gffn", [1, D]); gkv = I("gkv", [1, D])
        win = I("win", [D, 4 * D]); wout = I("wout", [D, D]); ongT = I("ongT", [128, 8]); lbl = I("lbl", [2, D])
        hcA = I("hcA", [128, 393]); hcB = I("hcB", [1, 1024]); oh = I("oh", [1, 4])
        kvw = I("kvw", [D, 2 * D]); pwq = I("pwq", [D, D]); skT = I("skT", [8, 128, 128])
        uT = I("uT", [D, NEXP]); v = I("v", [NEXP, D])
        h1 = O("h1", [NM * 512, D]); KTo = O("KTo", [NM, 8, 128, 512], BF16); Vo = O("Vo", [NM * 512, D], BF16)
        mod = cx.dram("mod", [128, 8 * D], F32)
        snapt = cx.dram("snap", [NQG, 128, D], F32)
        snap = [snapt[g] for g in range(NQG)]
        uTb = cx.dram("uTb", [D, NEXP], BF16); vb = cx.dram("vb", [NEXP, D], BF16)
        consts = make_consts(cx, es)
        emit_mod(cx, cT, [(ada_w, ada_b, 0, 6 * D), (kva_w, kva_b, 6 * D, 2 * D)], mod)
        emit_geff(cx, mod, 1 * D, gmix)
        emit_geff(cx, mod, 4 * D, gffn)
        emit_geff(cx, mod, 7 * D, gkv)
        if do_peer:
            emit_precast(cx, uT, uTb, D, NEXP)
            emit_precast(cx, v, vb, NEXP, D)
        winb = cx.dram("winb", [D, 4 * D], BF16); woutb = cx.dram("woutb", [D, D], BF16)
        kvwb = cx.dram("kvwb", [D, 2 * D], BF16); pwqb = cx.dram("pwqb", [D, D], BF16)
        emit_precast(cx, win, winb, D, 4 * D)
        emit_precast(cx, kvw, kvwb, D, 2 * D)
        emit_precast(cx, pwq, pwqb, D, D)
        emit_fold_cast(cx, wout, woutb, rowT_dram=ongT, col_dram=mod, col0=2 * D)
        HM = cx.sb(es, [128, 4, D]); bHM = S.bufs(4, "HM")
        emit_hgrn(cx, consts, HM, bHM, "state", xb, 4 * (NQG - 1), snap, mod, 0, D, 2 * D, winb, woutb, ongT, lbl, hcA, hcB)
        for m in range(NM):
            emit_hgrn(cx, consts, HM, bHM, "full", xo, 4, snap, mod, 0, D, 2 * D, winb, woutb, ongT, lbl, hcA, hcB, onehot_dram=oh, m=m)
            if do_peer:
                emit_peer(cx, consts, HM, bHM, mod, 3 * D, 4 * D, 5 * D, pwqb, skT, uTb, vb)
            emit_store(cx, HM, bHM, m, h1)
            emit_kv(cx, consts, HM, bHM, m, mod, 6 * D, 7 * D, kvwb, KTo, Vo)
        S.barrier()
        S.emit()
    return nc


def build_l2(NM, do_peer=True):
    SQ = 2048 * NM
    nc = bass.Bass("TRN2", target_bir_lowering=False)
    with ExitStack() as es:
        cx = Ctx(nc, es)
        S = cx.S
        I = lambda n, s, dt=F32: nc.dram_tensor(n, list(s), dt, kind="ExternalInput").ap()
        O = lambda n, s, dt=F32: nc.dram_tensor(n, list(s), dt, kind="ExternalOutput").ap()
        h1 = I("h1", [NM * 512, D]); cT = I("cT", [128, 8])
        ada_w = I("ada_w", [D, 6 * D]); ada_b = I("ada_b", [1, 6 * D])
        gmix = I("gmix", [1, D]); gffn = I("gffn", [1, D]); gfin = I("gfin", [1, D])
        sbwq = I("sbwq", [D, D]); sbwo = I("sbwo", [D, D])
        KT = I("KT", [8, 128, SQ], BF16); V = I("V", [8, 128, SQ // 128, 128], BF16)
        ac = I("ac", [128, 256], BF16); mask = I("mask", [128, 16, 512], BF16)
        pwq = I("pwq", [D, D]); skT = I("skT", [8, 128, 128]); uT = I("uT", [D, NEXP]); v = I("v", [NEXP, D])
        out = O("out", [NM * 512, D])
        mod = cx.dram("mod", [128, 6 * D], F32)
        uTb = cx.dram("uTb", [D, NEXP], BF16); vb = cx.dram("vb", [NEXP, D], BF16)
        consts = make_consts(cx, es)
        emit_mod(cx, cT, [(ada_w, ada_b, 0, 6 * D)], mod)
        emit_geff(cx, mod, 1 * D, gmix)
        emit_geff(cx, mod, 4 * D, gffn)
        if do_peer:
            emit_precast(cx, uT, uTb, D, NEXP)
            emit_precast(cx, v, vb, NEXP, D)
        sbwqb = cx.dram("sbwqb", [D, D], BF16); sbwob = cx.dram("sbwob", [D, D], BF16); pwqb = cx.dram("pwqb", [D, D], BF16)
        emit_precast(cx, sbwq, sbwqb, D, D)
        emit_precast(cx, pwq, pwqb, D, D)
        emit_fold_cast(cx, sbwo, sbwob, rowT_dram=None, col_dram=mod, col0=2 * D)
        HM = cx.sb(es, [128, 4, D]); bHM = S.bufs(4, "HM")
        for m in range(NM):
            for tt in range(4):
                S.dma("sp", HM[:, tt, :], h1[m * 512 + tt * 128:m * 512 + (tt + 1) * 128, :], writes=[bHM[tt]])
            emit_attn(cx, consts, HM, bHM, m, mod, 0, D, 2 * D, sbwqb, sbwob, KT, V, ac, mask)
            if do_peer:
                emit_peer(cx, consts, HM, bHM, mod, 3 * D, 4 * D, 5 * D, pwqb, skT, uTb, vb)
            emit_final(cx, consts, HM, bHM, m, gfin, out)
        S.barrier()
        S.emit()
    return nc


def run_model(inp, NM, do_peer=True, runner=None):
    f32 = lambda a: np.ascontiguousarray(np.asarray(a, dtype=np.float32))
    x = f32(inp["x"]); c = f32(inp["c"])
    B = x.shape[0]
    SQ = 2048 * NM
    assert x.shape == (B, SQ, D) and B == 2
    ncores = 8
    if runner is None:
        runner = lambda nc, maps: run_bass_kernel_spmd(nc, maps, core_ids=list(range(len(maps)))).results
    hcA, hcB = hgrn_consts_host()
    row = lambda a: f32(a).reshape(1, -1)
    colT = lambda a: np.ascontiguousarray(f32(a).reshape(8, 128).T)
    own = lambda b, r: np.concatenate([np.arange((4 * m + r) * 512, (4 * m + r + 1) * 512) for m in range(NM)])

    def peer_w(l):
        sk = f32(inp["peer_subkeys"][l]).reshape(8, 128, 128)
        return {"pwq": f32(inp["peer_w_q"][l]), "skT": np.ascontiguousarray(sk.transpose(0, 2, 1)),
                "uT": np.ascontiguousarray(f32(inp["peer_u"][l]).T), "v": f32(inp["peer_v"][l])}

    pw0 = peer_w(0)
    shared1 = {"ada_w": f32(inp["ada_w"][0]), "ada_b": row(inp["ada_b"][0]), "kva_w": f32(inp["kv_ada_w"]), "kva_b": row(inp["kv_ada_b"]),
               "gmix": row(inp["norm_mix_g"][0]), "gffn": row(inp["norm_ffn_g"][0]), "gkv": row(inp["kv_norm_g"]),
               "win": f32(inp["hgrn_w_in"][0]), "wout": f32(inp["hgrn_w_out"][0]), "ongT": colT(inp["hgrn_onorm_g"][0]),
               "lbl": f32(inp["hgrn_lb_logits"]), "hcA": hcA, "hcB": hcB, "kvw": f32(inp["kv_w"]), **pw0}
    maps = []
    for core in range(ncores):
        b, r = core // 4, core % 4
        oh = np.zeros((1, 4), np.float32); oh[0, r] = 1.0
        maps.append({"xb": x[b], "xo": np.ascontiguousarray(x[b][own(b, r)]), "cT": colT(c[b]), "oh": oh, **shared1})
    nc1 = build_l1(NM, do_peer)
    res1 = runner(nc1, maps)
    del maps, shared1, pw0
    KTf = np.zeros((B, 8, 128, SQ), ml_dtypes.bfloat16)
    Vf = np.zeros((B, SQ, D), ml_dtypes.bfloat16)
    for core in range(ncores):
        b, r = core // 4, core % 4
        kto = np.asarray(res1[core]["KTo"]); vo = np.asarray(res1[core]["Vo"])
        for m in range(NM):
            g = 4 * m + r
            KTf[b, :, :, g * 512:(g + 1) * 512] = kto[m]
            Vf[b, g * 512:(g + 1) * 512] = vo[m * 512:(m + 1) * 512]
    Vl = np.ascontiguousarray(Vf.reshape(B, SQ // 128, 128, 8, 128).transpose(0, 3, 2, 1, 4))
    pw1 = peer_w(1)
    shared2 = {"ada_w": f32(inp["ada_w"][1]), "ada_b": row(inp["ada_b"][1]), "gmix": row(inp["norm_mix_g"][1]), "gffn": row(inp["norm_ffn_g"][1]),
               "gfin": row(inp["final_norm_g"]), "sbwq": f32(inp["sb_w_q"][0]), "sbwo": f32(inp["sb_w_out"][0]), **pw1}
    maps = []
    for core in range(ncores):
        b, r = core // 4, core % 4
        ac, mask = attn_consts_host(r)
        maps.append({"h1": np.asarray(res1[core]["h1"]), "cT": colT(c[b]), "KT": KTf[b], "V": Vl[b], "ac": ac, "mask": mask, **shared2})
    nc2 = build_l2(NM, do_peer)
    res2 = runner(nc2, maps)
    out = np.zeros((B, SQ, D), np.float32)
    for core in range(ncores):
        b, r = core // 4, core % 4
        out[b, own(b, r)] = np.asarray(res2[core]["out"])
    return out


def kernel(**inputs):
    return run_model(inputs, 8)
```
